# Optimizing a Trainium2 kernel written in Bass

```python
import jax, jax.numpy as jnp
from jax import lax
import numpy as np

D_MODEL = 1024
BATCH = 16
SEQ = 2048
DEPTH = 4

CHUNK = 64
D_MIX = D_MODEL
M_WIDTH = D_MIX // 2
M_HEADS = 4
M_HEAD_DIM = M_WIDTH // M_HEADS
M_CONV = 4
F_WIDTH = D_MIX // 4
F_HEADS = 4
F_HEAD_DIM = F_WIDTH // F_HEADS
Q_BLOCK = 128
C_WIDTH = D_MIX - M_WIDTH - F_WIDTH
C_GROUPS = 4
C_KERNEL = 31
RMS_EPS = 1e-6
LN_EPS = 1e-5

IN_WIDTHS = (M_WIDTH, M_WIDTH, M_WIDTH, M_WIDTH, M_WIDTH, M_HEADS, M_HEADS,
             F_WIDTH, F_WIDTH, F_WIDTH, F_WIDTH, F_HEADS,
             C_WIDTH, C_WIDTH, C_WIDTH)
D_IN = 5 * M_WIDTH + 2 * M_HEADS + 4 * F_WIDTH + F_HEADS + 3 * C_WIDTH

kernel_name = "hybrid_mlstm_fox_conformer_adaln"


def _col_starts():
    starts, acc = [], 0
    for w in IN_WIDTHS:
        starts.append(acc)
        acc += w
    return starts


def _split_cols(u):
    return jnp.split(u, _col_starts()[1:], axis=-1)


def rmsnorm(x, g):
    xf = x.astype(jnp.float32)
    y = xf * lax.rsqrt(jnp.mean(xf * xf, axis=-1, keepdims=True) + RMS_EPS)
    return (y * g.astype(jnp.float32)).astype(x.dtype)


def causal_depthwise_conv(x, w, b):
    width, ch = w.shape
    y = lax.conv_general_dilated(x, w[:, None, :].astype(x.dtype), window_strides=(1,),
                                 padding=((width - 1, 0),),
                                 dimension_numbers=("NWC", "WIO", "NWC"),
                                 feature_group_count=ch)
    return y + b


def mlstm_group(q, k, v, i_pre, f_pre, o_pre, hn_g):
    B, S, _ = q.shape
    nc = S // CHUNK
    f32 = jnp.float32

    def heads(t):
        return t.astype(f32).reshape(B, nc, CHUNK, M_HEADS, M_HEAD_DIM).transpose(1, 0, 3, 2, 4)

    def gates(g):
        return g.astype(f32).reshape(B, nc, CHUNK, M_HEADS).transpose(1, 0, 3, 2)

    qc, kc, vc = heads(q), heads(k) * (M_HEAD_DIM ** -0.5), heads(v)
    li = gates(i_pre)
    lf = jax.nn.log_sigmoid(gates(f_pre))
    causal = jnp.tril(jnp.ones((CHUNK, CHUNK), dtype=bool))

    def step(carry, inp):
        C, n, m = carry
        qt, kt, vt, lit, lft = inp
        b = jnp.cumsum(lft, axis=-1)
        dlog = b[..., :, None] - b[..., None, :] + lit[..., None, :]
        dlog = jnp.where(causal, dlog, -jnp.inf)
        m_inter = b + m[..., None]
        m_t = jnp.maximum(m_inter, jnp.max(dlog, axis=-1))
        s_mat = jnp.einsum("bhtd,bhsd->bhts", qt, kt) * jnp.exp(dlog - m_t[..., None])
        inter = jnp.exp(m_inter - m_t)
        num = (jnp.einsum("bhts,bhsd->bhtd", s_mat, vt)
               + inter[..., None] * jnp.einsum("bhtk,bhvk->bhtv", qt, C))
        den = s_mat.sum(-1) + inter * jnp.einsum("bhtk,bhk->bht", qt, n)
        h = num / jnp.maximum(jnp.abs(den), jnp.exp(-m_t))[..., None]
        b_last = b[..., -1]
        g = b_last[..., None] - b + lit
        m_new = jnp.maximum(b_last + m, jnp.max(g, axis=-1))
        w = jnp.exp(g - m_new[..., None])
        decay = jnp.exp(b_last + m - m_new)
        C_new = decay[..., None, None] * C + jnp.einsum("bhsv,bhsk->bhvk", vt * w[..., None], kt)
        n_new = decay[..., None] * n + jnp.einsum("bhs,bhsk->bhk", w, kt)
        return (C_new, n_new, m_new), h

    init = (jnp.zeros((B, M_HEADS, M_HEAD_DIM, M_HEAD_DIM), f32),
            jnp.zeros((B, M_HEADS, M_HEAD_DIM), f32),
            jnp.zeros((B, M_HEADS), f32))
    _, hc = lax.scan(step, init, (qc, kc, vc, li, lf))
    h = hc.transpose(1, 0, 3, 2, 4).reshape(B, S, M_HEADS, M_HEAD_DIM)
    h = jax.nn.sigmoid(o_pre.astype(f32)).reshape(B, S, M_HEADS, M_HEAD_DIM) * h
    mu = jnp.mean(h, axis=-1, keepdims=True)
    var = jnp.mean(jnp.square(h - mu), axis=-1, keepdims=True)
    h = (h - mu) * lax.rsqrt(var + LN_EPS) * hn_g.astype(f32).reshape(M_HEADS, M_HEAD_DIM)
    return h.reshape(B, S, M_WIDTH).astype(q.dtype)


def fox_group(q, k, v, f_pre):
    B, S, _ = q.shape
    qh = q.reshape(B, S, F_HEADS, F_HEAD_DIM)
    kh = k.reshape(B, S, F_HEADS, F_HEAD_DIM)
    vh = v.reshape(B, S, F_HEADS, F_HEAD_DIM)
    cum = jnp.cumsum(jax.nn.log_sigmoid(f_pre.astype(jnp.float32)), axis=1).transpose(0, 2, 1)
    scale = F_HEAD_DIM ** -0.5
    outs = []
    for blk in range(S // Q_BLOCK):
        qs, qe = blk * Q_BLOCK, (blk + 1) * Q_BLOCK
        logits = jnp.einsum("bqhd,bkhd->bhqk", qh[:, qs:qe], kh[:, :qe]).astype(jnp.float32) * scale
        logits = logits + cum[:, :, qs:qe, None] - cum[:, :, None, :qe]
        mask = jnp.arange(qe)[None, :] <= jnp.arange(qs, qe)[:, None]
        p = jax.nn.softmax(jnp.where(mask, logits, -jnp.inf), axis=-1)
        outs.append(jnp.einsum("bhqk,bkhd->bqhd", p.astype(v.dtype), vh[:, :qe]))
    return jnp.concatenate(outs, axis=1).reshape(B, S, F_WIDTH)


def conv_group(a, g, dw_w, dw_b, ln_g, ln_b):
    B, S, _ = a.shape
    u = a * jax.nn.sigmoid(g)
    u = causal_depthwise_conv(u, dw_w, dw_b)
    uf = u.astype(jnp.float32).reshape(B, S, C_GROUPS, C_WIDTH // C_GROUPS)
    mu = jnp.mean(uf, axis=-1, keepdims=True)
    var = jnp.mean(jnp.square(uf - mu), axis=-1, keepdims=True)
    uf = ((uf - mu) * lax.rsqrt(var + LN_EPS)).reshape(B, S, C_WIDTH)
    uf = uf * ln_g.astype(jnp.float32) + ln_b.astype(jnp.float32)
    return jax.nn.silu(uf).astype(a.dtype)


def setup_inputs(seed: int = 0) -> dict:
    key = jax.random.key(seed)
    ks = jax.random.split(key, 16)
    n = jax.random.normal
    f32 = jnp.float32
    starts = _col_starts()
    b_in = 0.02 * n(ks[6], (DEPTH, D_IN), f32)
    mf0 = starts[6]
    ff0 = starts[11]
    b_in = b_in.at[:, mf0:mf0 + M_HEADS].add(jnp.linspace(3.0, 6.0, M_HEADS))
    b_in = b_in.at[:, ff0:ff0 + F_HEADS].add(1.0)
    return {
        "x": n(ks[0], (BATCH, SEQ, D_MODEL), f32),
        "c": n(ks[1], (BATCH, D_MODEL), f32),
        "norm_g": 1.0 + 0.02 * n(ks[2], (DEPTH, D_MODEL), f32),
        "w_ada": n(ks[3], (DEPTH, D_MODEL, 3 * D_MODEL), f32) * D_MODEL ** -0.5,
        "b_ada": 0.02 * n(ks[4], (DEPTH, 3 * D_MODEL), f32),
        "w_in": n(ks[5], (DEPTH, D_MODEL, D_IN), f32) * D_MODEL ** -0.5,
        "b_in": b_in,
        "m_conv_w": n(ks[7], (DEPTH, M_CONV, 2 * M_WIDTH), f32) * M_CONV ** -0.5,
        "m_conv_b": 0.02 * n(ks[8], (DEPTH, 2 * M_WIDTH), f32),
        "m_hn_g": 1.0 + 0.02 * n(ks[9], (DEPTH, M_WIDTH), f32),
        "c_dw_w": n(ks[10], (DEPTH, C_KERNEL, C_WIDTH), f32) * C_KERNEL ** -0.5,
        "c_dw_b": 0.02 * n(ks[11], (DEPTH, C_WIDTH), f32),
        "c_ln_g": 1.0 + 0.02 * n(ks[12], (DEPTH, C_WIDTH), f32),
        "c_ln_b": 0.02 * n(ks[13], (DEPTH, C_WIDTH), f32),
        "w_out": n(ks[14], (DEPTH, D_MIX, D_MODEL), f32) * D_MIX ** -0.5,
        "final_g": 1.0 + 0.02 * n(ks[15], (D_MODEL,), f32),
    }


def reference(x, c, norm_g, w_ada, b_ada, w_in, b_in, m_conv_w, m_conv_b, m_hn_g,
              c_dw_w, c_dw_b, c_ln_g, c_ln_b, w_out, final_g):
    c_act = jax.nn.silu(c)
    for l in range(DEPTH):
        mod = c_act @ w_ada[l] + b_ada[l]
        shift, scale, gate = jnp.split(mod, 3, axis=-1)
        h = rmsnorm(x, norm_g[l]) * (1.0 + scale[:, None, :]) + shift[:, None, :]
        u = h @ w_in[l] + b_in[l]
        (mq, mk, mv, mo, mz, mi, mf, fq, fk, fv, fz, ff, ca, cg, cz) = _split_cols(u)
        qk = jax.nn.silu(causal_depthwise_conv(jnp.concatenate([mq, mk], axis=-1),
                                               m_conv_w[l], m_conv_b[l]))
        mq, mk = jnp.split(qk, 2, axis=-1)
        ya = mlstm_group(mq, mk, mv, mi, mf, mo, m_hn_g[l]) * jax.nn.silu(mz)
        yb = fox_group(fq, fk, fv, ff) * jax.nn.silu(fz)
        yc = conv_group(ca, cg, c_dw_w[l], c_dw_b[l], c_ln_g[l], c_ln_b[l]) * jax.nn.silu(cz)
        y = jnp.concatenate([ya, yb, yc], axis=-1) @ w_out[l]
        x = x + gate[:, None, :] * y
    return rmsnorm(x, final_g)
```

```python
import math
import numpy as np
import concourse.bass as bass
import concourse.mybir as mybir
from concourse.bass_utils import run_bass_kernel_spmd
from contextlib import ExitStack

AF = mybir.ActivationFunctionType
ALU = mybir.AluOpType
F32 = mybir.dt.float32
BF16 = mybir.dt.bfloat16

ENGS = ["pe", "act", "dve", "pool", "sp"]
SEM_EPOCH = 30000
SAME_ENG_SYNC = True
STOP_AFTER = None

S = 2048
D = 1024
NHALF = 1024
DIN = 4364
C_MQ, C_MK, C_MV, C_MO, C_MZ, C_MI, C_MF, C_FQ, C_FK, C_FV, C_FZ, C_FF, C_CA, C_CG, C_CZ = (
    0, 512, 1024, 1536, 2048, 2560, 2564, 2568, 2824, 3080, 3336, 3592, 3596, 3852, 4108)
V_NG, V_BA, V_BQ, V_BK, V_BZ, V_BFQ, V_BFK, V_BCA, V_BCG, V_BCZ, V_CB, V_HG, V_DWB, V_LNG, V_LNB, V_CW, V_DW, NV = (
    0, 8, 32, 36, 40, 44, 46, 48, 50, 52, 54, 62, 66, 68, 70, 72, 104, 166)


class Op:
    __slots__ = ("eng", "idx", "fn", "dma", "deps", "signal", "sig", "dcount", "waits")

    def __init__(self, eng, idx, fn, dma):
        self.eng = eng
        self.idx = idx
        self.fn = fn
        self.dma = dma
        self.deps = None
        self.signal = False
        self.sig = None
        self.dcount = None
        self.waits = []


class _Call:
    __slots__ = ("name", "a", "kw")

    def __init__(self, name, a, kw):
        self.name = name
        self.a = a
        self.kw = kw

    def __call__(self, e):
        return getattr(e, self.name)(*self.a, **self.kw)


class _Rec:
    def __getattr__(self, name):
        return lambda *a, **kw: _Call(name, a, kw)


_REC = _Rec()


class Prog:
    def __init__(self, nc, stack):
        self.nc = nc
        self.stack = stack
        self.ops = {e: [] for e in ENGS}
        self.reg = {}
        self.dma_cnt = {}
        self.dma_sem = {}
        self.pend = {e: None for e in ENGS}

    def barrier(self):
        snap_c = {e: len(self.ops[e]) - 1 for e in ENGS if e != "sp" and len(self.ops[e]) > 0}
        snap_c = {e: i for e, i in snap_c.items()}
        snap_d = {k: c for k, c in self.dma_cnt.items() if not (isinstance(k, tuple) and k[0] == "wb")}
        for e in ENGS:
            self.pend[e] = (dict(snap_c), dict(snap_d))

    def op(self, eng, fn, r=(), w=(), dma=None):
        call = fn(_REC)
        assert isinstance(call, _Call), "op lambda must return e.<instr>(...)"
        fn = call
        o = Op(eng, len(self.ops[eng]), fn, dma)
        deps = []
        for k in r:
            st = self.reg.get(k)
            if st is not None and st[0] is not None:
                deps.append(st[0])
        for k in w:
            st = self.reg.get(k)
            if st is not None:
                if st[0] is not None:
                    deps.append(st[0])
                deps.extend(st[1])
        cdeps = {}
        ddeps = {}
        for d in deps:
            if d.dma is not None:
                ddeps[d.dma] = self.dma_cnt[d.dma]
            else:
                if d.eng == eng and (eng == "pe" or not SAME_ENG_SYNC):
                    continue
                if cdeps.get(d.eng, -1) < d.idx:
                    cdeps[d.eng] = d.idx
        pb = self.pend[eng]
        if pb is not None:
            self.pend[eng] = None
            for e2, i2 in pb[0].items():
                if e2 == eng:
                    continue
                tgt = self.ops[e2][i2]
                j = i2
                while j >= 0 and self.ops[e2][j].dma is not None:
                    j -= 1
                if j >= 0 and cdeps.get(e2, -1) < j:
                    cdeps[e2] = j
            for k, c in pb[1].items():
                if ddeps.get(k, 0) < c:
                    ddeps[k] = c
        o.deps = (cdeps, ddeps)
        for k in r:
            st = self.reg.get(k)
            if st is None:
                st = [None, []]
                self.reg[k] = st
            st[1].append(o)
        for k in w:
            self.reg[k] = [o, []]
        if dma is not None:
            self.dma_cnt[dma] = self.dma_cnt.get(dma, 0) + 16
            o.dcount = self.dma_cnt[dma]
        self.ops[eng].append(o)
        return o

    def finalize_and_emit(self):
        nc = self.nc
        for e in ENGS:
            for o in self.ops[e]:
                for de, di in o.deps[0].items():
                    self.ops[de][di].signal = True
        nsig = {}
        for e in ENGS:
            c = 0
            for o in self.ops[e]:
                if o.signal and o.dma is None:
                    o.sig = c
                    c += 1
            nsig[e] = c
        sems = {}
        for e in ENGS:
            n = (nsig[e] + SEM_EPOCH - 1) // SEM_EPOCH
            sems[e] = [self.stack.enter_context(nc.semaphore(f"s_{e}_{i}")) for i in range(n)]
        for k in self.dma_cnt:
            self.dma_sem[k] = self.stack.enter_context(nc.semaphore(f"d_{len(self.dma_sem)}"))
        for e in ENGS:
            seen = {}
            dseen = {}
            for o in self.ops[e]:
                for de, di in o.deps[0].items():
                    s = self.ops[de][di].sig
                    if seen.get(de, -1) >= s:
                        continue
                    seen[de] = s
                    o.waits.append((sems[de][s // SEM_EPOCH], s % SEM_EPOCH + 1))
                for k, cnt in o.deps[1].items():
                    if dseen.get(k, 0) >= cnt:
                        continue
                    dseen[k] = cnt
                    o.waits.append((self.dma_sem[k], cnt))
        blk = self.stack.enter_context(nc.Block())
        prog = self

        def emit(e, name):
            for o in prog.ops[name]:
                for (s, v) in o.waits:
                    e.wait_ge(s, v)
                ins = o.fn(e)
                if o.dma is not None:
                    ins.then_inc(prog.dma_sem[o.dma], 16)
                elif o.signal:
                    ins.then_inc(sems[name][o.sig // SEM_EPOCH], 1)
            for k, cnt in prog.dma_cnt.items():
                if any(o.dma == k for o in prog.ops[name]):
                    e.wait_ge(prog.dma_sem[k], cnt)

        @blk.tensor
        def _(e):
            emit(e, "pe")

        @blk.scalar
        def _(e):
            emit(e, "act")

        @blk.vector
        def _(e):
            emit(e, "dve")

        @blk.gpsimd
        def _(e):
            emit(e, "pool")

        @blk.sync
        def _(e):
            emit(e, "sp")


def build(L, NS, final_norm, dbg=False):
    nc = bass.Bass("TRN2", target_bir_lowering=False)

    def din(n, s, d=F32):
        return nc.dram_tensor(n, s, d, kind="ExternalInput").ap()

    x_d = din("x", [NS, S, D])
    cT_d = din("cT", [128, 8, NS])
    vecs_d = din("vecs", [L, 128, NV])
    vec64_d = din("vec64", [L, 64, 12])
    gb_d = din("gb", [L, 4, 3])
    wada_d = din("w_ada", [L, D, 3 * D])
    win_d = din("w_in", [L, D, DIN])
    bin_d = din("b_in", [L, 1, DIN])
    wout_d = din("w_out", [L, D, D])
    fg_d = din("final_g", [1, D])
    out_d = nc.dram_tensor("out", [NS, S, D], F32, kind="ExternalOutput").ap()
    cq_scr = nc.dram_tensor("cq_scr", [4, NHALF], BF16).ap()
    if dbg:
        d_h = nc.dram_tensor("d_h", [128, 8, NHALF], BF16, kind="ExternalOutput").ap()
        d_ya = nc.dram_tensor("d_ya", [128, 4, NHALF], BF16, kind="ExternalOutput").ap()
        d_yb = nc.dram_tensor("d_yb", [128, 2, NHALF], BF16, kind="ExternalOutput").ap()
        d_yc = nc.dram_tensor("d_yc", [128, 2, NHALF], BF16, kind="ExternalOutput").ap()
        d_q = nc.dram_tensor("d_q", [128, 2, NHALF], BF16, kind="ExternalOutput").ap()
        d_gt = nc.dram_tensor("d_gt", [128, 64], F32, kind="ExternalOutput").ap()
        d_O = nc.dram_tensor("d_O", [128, 512], F32, kind="ExternalOutput").ap()
        d_pt = nc.dram_tensor("d_pt", [128, 512], BF16, kind="ExternalOutput").ap()
        d_fq = nc.dram_tensor("d_fq", [128, 4, NHALF], BF16, kind="ExternalOutput").ap()
        d_fk = nc.dram_tensor("d_fk", [128, 4, S], BF16, kind="ExternalOutput").ap()
        d_fz = nc.dram_tensor("d_fz", [64, 4, NHALF], BF16, kind="ExternalOutput").ap()
        d_V = nc.dram_tensor("d_V", [128, 16 * 4 * 65], BF16, kind="ExternalOutput").ap()
        d_cs = nc.dram_tensor("d_cs", [128, 64], F32, kind="ExternalOutput").ap()

    with ExitStack() as st:
        P = Prog(nc, st)
        A = P.op

        def sb(n, s, d):
            return st.enter_context(nc.sbuf_tensor(n, s, d))

        xT = sb("xT", [128, 8, S], F32)
        hT = sb("hT", [128, 8, NHALF], BF16)
        yTa = sb("yTa", [128, 4, NHALF], BF16)
        yTb = sb("yTb", [128, 2, NHALF], BF16)
        yTc = sb("yTc", [128, 2, NHALF], BF16)
        wbuf = [sb(f"wbuf{i}", [128, 8, 512], BF16) for i in range(2)]
        fkT = sb("fkT", [128, 2, S], BF16)
        Vext = sb("Vext", [128, 16, 4, 65], BF16)
        ubuf = sb("ubuf", [128, 2, 30 + NHALF], BF16)
        Cst = sb("Cst", [128, 4, 130], F32)
        cumspT = sb("cumspT", [128, 16, 4], F32)
        cumq_bf = sb("cumq_bf", [4, NHALF], BF16)
        carry = sb("carry", [4, 2], F32)
        wtm = sb("wtm", [128, 8, 2, 4], F32)
        dbc = sb("dbc", [128, 4, 8], F32)
        halo = sb("halo", [128, 8, 4], BF16)
        ident_f = sb("ident_f", [128, 128], F32)
        ident_b = sb("ident_b", [128, 128], BF16)
        ones_b = sb("ones_b", [128, 128], BF16)
        maskT = sb("maskT", [128, 128], BF16)
        blk64 = sb("blk64", [128, 128], BF16)
        negmask = sb("negmask", [128, 128], BF16)
        selF = sb("selF", [4, 4, 128], F32)
        selB = sb("selB", [4, 4, 128], BF16)
        onesF = sb("onesF", [128, 64], F32)
        cst = sb("cst", [128, 8], F32)
        vecs = sb("vecs_sb", [128, NV], F32)
        vec64 = sb("vec64_sb", [64, 12], F32)
        gb = sb("gb_sb", [4, 4], F32)
        cact = sb("cact", [128, 8, NS], BF16)
        cin = sb("cin", [128, 8, NS], F32)
        modv = sb("modv", [128, L, 24, NS], F32)
        Gv = sb("Gv", [128, 8], F32)
        SCRB = 36 * 1024
        scr = sb("scr", [128, SCRB // 2], BF16)
        pb = [st.enter_context(nc.psum_tensor(f"pb{i}", [128, 512], F32)) for i in range(8)]

        def carve(off, npart, shape, dt):
            n = 1
            for v in shape:
                n *= v
            nb = n * (4 if dt == F32 else 2)
            assert off % 4 == 0 and off + nb <= SCRB, (off, nb)
            ap = scr[0:npart, off // 2:(off + nb) // 2]
            if dt == F32:
                ap = ap.bitcast(F32)
            if len(shape) == 2:
                ap = ap.rearrange("p (a b) -> p a b", b=shape[1])
            elif len(shape) == 3:
                ap = ap.rearrange("p (a b c) -> p a b c", b=shape[1], c=shape[2])
            return ap

        def pbb(i):
            return pb[i][:].bitcast(BF16)

        CE6, CE5, C1, CLN, C0 = 0, 1, 2, 3, 4
        for i, v in enumerate([1e-6, 1e-5, 1.0, -0.5 * math.log(128.0), 0.0]):
            A("dve", lambda e, i=i, v=v: e.memset(cst[:, i:i + 1], v), w=["cst"], r=["cst"])
        A("pool", lambda e: e.memset(ident_f[:], 0.0), w=["ident_f"])
        A("pool", lambda e: e.affine_select(out=ident_f[:], in_=ident_f[:], pattern=[[-1, 128]],
                                            compare_op=ALU.not_equal, fill=1.0, base=0, channel_multiplier=1),
          r=["ident_f"], w=["ident_f"])
        A("dve", lambda e: e.tensor_copy(out=ident_b[:], in_=ident_f[:]), r=["ident_f"], w=["ident_b"])
        A("dve", lambda e: e.memset(ones_b[:], 1.0), w=["ones_b"])
        A("pool", lambda e: e.memset(maskT[:], 1.0), w=["maskT"])
        A("pool", lambda e: e.affine_select(out=maskT[:], in_=maskT[:], pattern=[[1, 128]],
                                            compare_op=ALU.is_ge, fill=0.0, base=0, channel_multiplier=-1),
          r=["maskT"], w=["maskT"])
        A("pool", lambda e: e.memset(negmask[:], 0.0), w=["negmask"])
        A("pool", lambda e: e.affine_select(out=negmask[:], in_=negmask[:], pattern=[[1, 128]],
                                            compare_op=ALU.is_ge, fill=-30000.0, base=0, channel_multiplier=-1),
          r=["negmask"], w=["negmask"])
        A("dve", lambda e: e.memset(blk64[:], 0.0), w=["blk64"])
        A("dve", lambda e: e.memset(blk64[0:64, 0:64], 1.0 / 64), r=["blk64"], w=["blk64"])
        A("dve", lambda e: e.memset(blk64[64:128, 64:128], 1.0 / 64), r=["blk64"], w=["blk64"])
        A("pool", lambda e: e.memset(selF[:], 0.0), w=["selF"])
        A("pool", lambda e: e.affine_select(out=selF[:], in_=selF[:], pattern=[[1, 4], [0, 128]],
                                            compare_op=ALU.not_equal, fill=1.0, base=0, channel_multiplier=-1),
          r=["selF"], w=["selF"])
        A("dve", lambda e: e.tensor_copy(out=selB[:], in_=selF[:]), r=["selF"], w=["selB"])
        A("dve", lambda e: e.memset(onesF[:], 1.0), w=["onesF"])
        A("dve", lambda e: e.memset(halo[:], 0.0), w=["halo"])
        A("sp", lambda e: e.dma_start(out=cin[:], in_=cT_d[:, :, :]), w=["cin"], dma="cin")
        A("act", lambda e: e.activation(out=cact[:], in_=cin[:], func=AF.Silu), r=["cin"], w=["cact"])

        wstate = {"n": 0}

        def wload(src, segs):
            slot = wstate["n"] % 2
            wstate["n"] += 1
            offs = []
            o = 0
            for (c0, ncol) in segs:
                A("pool", lambda e, o=o, c0=c0, ncol=ncol, slot=slot: e.dma_start(
                    out=wbuf[slot][:, :, o:o + ncol],
                    in_=src[:, c0:c0 + ncol].rearrange("(k p) n -> p k n", p=128)),
                  w=[("wb", slot)], r=[("wb", slot)], dma=("wb", slot))
                offs.append(o)
                o += ncol
            return slot, offs

        class Jobs:
            def __init__(self, lst):
                self.lst = lst
                self.i = 0
                self.loaded = None

            def get(self):
                if self.loaded is None:
                    self.loaded = wload(*self.lst[self.i])
                cur = self.loaded
                self.i += 1
                self.loaded = wload(*self.lst[self.i]) if self.i < len(self.lst) else None
                return cur

        def load_x(s):
            P.barrier()
            xin = [carve(i * 4096, 128, [D], F32) for i in range(2)]
            for t in range(16):
                A("sp", lambda e, t=t: e.dma_start(out=xin[t % 2], in_=x_d[s, t * 128:(t + 1) * 128, :]),
                  w=[("xin", t % 2)], dma=("xin", t % 2))
                for hb in range(2):
                    bank = 2 * (t % 2) + hb
                    for c4 in range(4):
                        c = hb * 4 + c4
                        A("pe", lambda e, t=t, c=c, c4=c4, bank=bank: e.transpose(
                            out=pb[bank][:, c4 * 128:(c4 + 1) * 128], in_=xin[t % 2][:, c * 128:(c + 1) * 128],
                            identity=ident_f[:]), r=[("xin", t % 2), "ident_f"], w=[("pb", bank)])
                    eng = "act" if hb == 0 else "dve"
                    if eng == "act":
                        A("act", lambda e, t=t, hb=hb, bank=bank: e.copy(
                            out=xT[:, hb * 4:hb * 4 + 4, t * 128:(t + 1) * 128],
                            in_=pb[bank][:].rearrange("p (a b) -> p a b", b=128)),
                          r=[("pb", bank)], w=[("xT", t // 4)])
                    else:
                        A("dve", lambda e, t=t, hb=hb, bank=bank: e.tensor_copy(
                            out=xT[:, hb * 4:hb * 4 + 4, t * 128:(t + 1) * 128],
                            in_=pb[bank][:].rearrange("p (a b) -> p a b", b=128)),
                          r=[("pb", bank)], w=[("xT", t // 4)])

        def store_x(s):
            P.barrier()
            xo = [carve(i * 4096, 128, [D], F32) for i in range(2)]
            fgb = carve(8192, 128, [D], F32)
            junk = carve(12288, 128, [D], F32)
            stat = carve(16384, 128, [8], F32)
            if final_norm:
                A("sp", lambda e: e.dma_start(out=fgb, in_=fg_d.partition_broadcast(128)), w=["fgb"], dma="fgb")
            for t in range(16):
                for hb in range(2):
                    bank = 2 * (t % 2) + hb
                    for c4 in range(4):
                        c = hb * 4 + c4
                        A("pe", lambda e, t=t, c=c, c4=c4, bank=bank: e.transpose(
                            out=pb[bank][:, c4 * 128:(c4 + 1) * 128], in_=xT[:, c, t * 128:(t + 1) * 128],
                            identity=ident_f[:]), r=[("xT", t // 4), "ident_f"], w=[("pb", bank)])
                    if hb == 0:
                        A("act", lambda e, t=t, bank=bank: e.copy(out=xo[t % 2][:, 0:512], in_=pb[bank][:]),
                          r=[("pb", bank)], w=[("xo", t % 2)])
                    else:
                        A("dve", lambda e, t=t, bank=bank: e.tensor_copy(out=xo[t % 2][:, 512:1024], in_=pb[bank][:]),
                          r=[("pb", bank)], w=[("xo", t % 2)])
                if final_norm:
                    A("act", lambda e, t=t: e.activation(out=junk, in_=xo[t % 2], func=AF.Square,
                                                          accum_out=stat[:, 0:1]),
                      r=[("xo", t % 2)], w=["junk", "stat"])
                    A("act", lambda e: e.activation(out=stat[:, 1:2], in_=stat[:, 0:1], func=AF.Ln,
                                                    scale=1.0 / D, bias=cst[:, CE6:CE6 + 1]),
                      r=["stat", "cst"], w=["stat"])
                    A("act", lambda e: e.activation(out=stat[:, 2:3], in_=stat[:, 1:2], func=AF.Exp, scale=-0.5),
                      r=["stat"], w=["stat"])
                    A("dve", lambda e, t=t: e.scalar_tensor_tensor(out=xo[t % 2], in0=xo[t % 2], scalar=stat[:, 2:3],
                                                                   in1=fgb, op0=ALU.mult, op1=ALU.mult),
                      r=[("xo", t % 2), "stat", "fgb"], w=[("xo", t % 2)])
                A("sp", lambda e, t=t: e.dma_start(out=out_d[s, t * 128:(t + 1) * 128, :], in_=xo[t % 2]),
                  r=[("xo", t % 2)], dma=("xo", t % 2))

        def ada(l):
            P.barrier()
            jobs = Jobs([(wada_d[l], [(g * 512, 512)]) for g in range(6)])
            for g in range(6):
                slot, offs = jobs.get()
                for fc4 in range(4):
                    fc = g * 4 + fc4
                    for k in range(8):
                        A("pe", lambda e, slot=slot, fc4=fc4, fc=fc, k=k: e.matmul(
                            pb[4][:, fc * NS:(fc + 1) * NS], lhsT=wbuf[slot][:, k, fc4 * 128:(fc4 + 1) * 128],
                            rhs=cact[:, k, :], start=(k == 0), stop=(k == 7)),
                          r=[("wb", slot), "cact"], w=[("pb", 4)])
            A("dve", lambda e: e.tensor_tensor(
                out=modv[:, l, :, :], in0=pb[4][:, 0:24 * NS].rearrange("p (a b) -> p a b", b=NS),
                in1=vecs[:, V_BA:V_BA + 24].unsqueeze(2).to_broadcast([128, 24, NS]), op=ALU.add),
              r=[("pb", 4), "vecs"], w=["modv"])

        def load_vecs(l):
            P.barrier()
            A("sp", lambda e: e.dma_start(out=vecs[:], in_=vecs_d[l, :, :]), w=["vecs"], dma="vecs")
            A("sp", lambda e: e.dma_start(out=vec64[:], in_=vec64_d[l, :, :]), w=["vec64"], dma="vec64")
            A("sp", lambda e: e.dma_start(out=gb[:, 0:3], in_=gb_d[l, :, :]), w=["gb"], dma="gb")

        acc_state = {"n": 0}

        def next_acc():
            b = acc_state["n"] % 2
            acc_state["n"] += 1
            return b

        def proj_fm(slot, col0, M, evac, tts=(0, 1)):
            for tt in tts:
                bank = next_acc()
                for k in range(8):
                    A("pe", lambda e, k=k, tt=tt, bank=bank: e.matmul(
                        pb[bank][0:M, :], lhsT=wbuf[slot][:, k, col0:col0 + M], rhs=hT[:, k, tt * 512:(tt + 1) * 512],
                        start=(k == 0), stop=(k == 7)),
                      r=[("wb", slot), ("hT", tt)], w=[("pb", bank)])
                evac(bank, tt)

        def proj_tm(slot, col0, N, evac):
            for t8 in range(8):
                bank = next_acc()
                for k in range(8):
                    A("pe", lambda e, k=k, t8=t8, bank=bank: e.matmul(
                        pb[bank][:, 0:N], lhsT=hT[:, k, t8 * 128:(t8 + 1) * 128], rhs=wbuf[slot][:, k, col0:col0 + N],
                        start=(k == 0), stop=(k == 7)),
                      r=[("wb", slot), ("hT", t8 // 4)], w=[("pb", bank)])
                evac(bank, t8)

        def layer(l, s, first_layer_dbg):
            load_vecs(l)
            A("dve", lambda e: e.scalar_tensor_tensor(out=Gv[:], in0=modv[:, l, 8:16, s], scalar=1.0,
                                                      in1=vecs[:, V_NG:V_NG + 8], op0=ALU.add, op1=ALU.mult),
              r=["modv", "vecs"], w=["Gv"])
            A("dve", lambda e: e.memset(Cst[:], 0.0), w=["Cst"], r=["Cst"])
            A("dve", lambda e: e.memset(carry[:], 0.0), w=["carry"], r=["carry"])
            A("dve", lambda e: e.memset(halo[:], 0.0), w=["halo"], r=["halo"])
            A("dve", lambda e: e.memset(Vext[:, :, :, 64:65], 1.0), w=["Vext"], r=["Vext"])
            win = win_d[l]
            wout = wout_d[l]
            for half in range(2):
                t0 = half * NHALF
                joblist = [(win, [(C_MI, 8), (C_FF, 4)])]
                for hp in range(2):
                    joblist.append((win, [(C_MQ + hp * 256, 256), (C_MK + hp * 256, 256)]))
                    joblist.append((win, [(C_MZ + hp * 256, 256)]))
                    joblist.append((win, [(C_MV + hp * 256, 256), (C_MO + hp * 256, 256)]))
                joblist.append((win, [(C_FQ, 256), (C_FK, 256)]))
                joblist.append((win, [(C_FV, 256), (C_FZ, 256)]))
                joblist.append((win, [(C_CA, 256), (C_CG, 256)]))
                joblist.append((win, [(C_CZ, 256)]))
                joblist.append((wout, [(0, 512)]))
                joblist.append((wout, [(512, 512)]))
                jobs = Jobs(joblist)

                P.barrier()
                sq = carve(0, 128, [8, 512], BF16)
                tmpf = [carve(8192 + i * 2048, 128, [512], F32) for i in range(2)]
                lnv = carve(12288, 128, [512], F32)
                for tt in range(2):
                    tok = slice(t0 + tt * 512, t0 + (tt + 1) * 512)
                    for c in range(8):
                        A("act", lambda e, c=c, tok=tok: e.activation(out=sq[:, c, :], in_=xT[:, c, tok], func=AF.Square),
                          r=[("xT", (t0 + tt * 512) // 512)], w=[("sq", c)])
                        A("pe", lambda e, c=c: e.matmul(pb[4][:], lhsT=ones_b[:], rhs=sq[:, c, :],
                                                        start=(c == 0), stop=(c == 7)),
                          r=[("sq", c), "ones_b"], w=[("pb", 4)])
                    A("act", lambda e: e.activation(out=lnv, in_=pb[4][:], func=AF.Ln, scale=1.0 / D,
                                                    bias=cst[:, CE6:CE6 + 1]), r=[("pb", 4), "cst"], w=["lnv"])
                    A("act", lambda e: e.activation(out=pb[5][:], in_=lnv, func=AF.Exp, scale=-0.5),
                      r=["lnv"], w=[("pb", 5)])
                    for c in range(8):
                        A("dve", lambda e, c=c, tok=tok: e.scalar_tensor_tensor(
                            out=tmpf[c % 2], in0=xT[:, c, tok], scalar=Gv[:, c:c + 1], in1=pb[5][:],
                            op0=ALU.mult, op1=ALU.mult),
                          r=[("xT", (t0 + tt * 512) // 512), "Gv", ("pb", 5)], w=[("tmpf", c % 2)])
                        A("act", lambda e, c=c, tt=tt: e.activation(
                            out=hT[:, c, tt * 512:(tt + 1) * 512], in_=tmpf[c % 2], func=AF.Identity,
                            bias=modv[:, l, c, s:s + 1], scale=1.0),
                          r=[("tmpf", c % 2), "modv"], w=[("hT", tt)])
                if dbg and first_layer_dbg and half == 0:
                    A("sp", lambda e: e.dma_start(out=d_h[:, :, :], in_=hT[:]), r=[("hT", 0), ("hT", 1)], dma="dbg_h")

                if STOP_AFTER == "H":
                    continue
                P.barrier()
                g1 = carve(0, 4, [NHALF], F32)
                g2 = carve(4096, 4, [NHALF], F32)
                g3 = carve(8192, 4, [NHALF], F32)
                g4 = carve(12288, 4, [NHALF], F32)
                g5 = carve(16384, 4, [NHALF], F32)
                msk = carve(20480, 4, [NHALF], F32)
                dsm = carve(24576, 4, [8], F32)
                slot, offs = jobs.get()

                gname = {id(g1): "g1", id(g2): "g2", id(g3): "g3", id(g4): "g4", id(g5): "g5"}

                def gate_proj(gi, dst):
                    def ev(bank, tt):
                        A("act", lambda e, bank=bank, tt=tt: e.activation(
                            out=dst[:, tt * 512:(tt + 1) * 512], in_=pb[bank][0:4, :], func=AF.Identity,
                            bias=gb[:, gi:gi + 1], scale=1.0), r=[("pb", bank), "gb"], w=[("g", gname[id(dst)], tt)])
                    proj_fm(slot, 4 * gi, 4, ev)

                gate_proj(0, g1)
                gate_proj(1, g2)
                gate_proj(2, g4)
                gk = lambda t: [("g", gname[id(t)], 0), ("g", gname[id(t)], 1)]
                for gt in (g2, g4):
                    A("act", lambda e, gt=gt: e.activation(out=gt, in_=gt, func=AF.Exp, scale=-1.0), r=gk(gt), w=gk(gt))
                    A("act", lambda e, gt=gt: e.activation(out=gt, in_=gt, func=AF.Ln, bias=cst[0:4, C1:C1 + 1], scale=1.0),
                      r=gk(gt) + ["cst"], w=gk(gt))
                A("dve", lambda e: e.memset(msk, 1.0), w=["msk"])
                A("dve", lambda e: e.memset(msk.rearrange("p (a b) -> p a b", b=128)[:, :, 0:1], 0.0), r=["msk"], w=["msk"])
                A("dve", lambda e: e.tensor_tensor_scan(out=g3, data0=msk, data1=g2, initial=0.0,
                                                        op0=ALU.mult, op1=ALU.add), r=["msk"] + gk(g2), w=gk(g3))
                g3v = g3.rearrange("p (a b) -> p a b", b=128)
                A("act", lambda e: e.activation(out=dsm, in_=g3v[:, :, 127], func=AF.Exp, scale=-1.0), r=gk(g3), w=["dsm"])
                A("dve", lambda e: e.tensor_tensor(out=g2.rearrange("p (a b) -> p a b", b=128), in0=g3v,
                                                   in1=g3v[:, :, 127:128].to_broadcast([4, 8, 128]), op=ALU.subtract),
                  r=gk(g3), w=gk(g2))
                A("dve", lambda e: e.tensor_tensor(out=g1, in0=g1, in1=g2, op=ALU.add), r=gk(g1) + gk(g2), w=gk(g1))
                A("act", lambda e: e.activation(out=g1, in_=g1, func=AF.Exp, bias=cst[0:4, CLN:CLN + 1], scale=1.0),
                  r=gk(g1) + ["cst"], w=gk(g1))
                A("act", lambda e: e.activation(out=g2, in_=g2, func=AF.Exp), r=gk(g2), w=gk(g2))
                A("dve", lambda e: e.memset(msk, 1.0), r=["msk"] + gk(g3), w=["msk"])
                A("dve", lambda e: e.tensor_tensor_scan(out=g5, data0=msk, data1=g4, initial=carry[:, 0:1],
                                                        op0=ALU.mult, op1=ALU.add),
                  r=["msk", "carry"] + gk(g4), w=gk(g5))
                A("dve", lambda e: e.tensor_copy(out=carry[:, 0:1], in_=g5[:, NHALF - 1:NHALF]), r=gk(g5), w=["carry"])
                A("act", lambda e: e.activation(out=cumq_bf[:], in_=g5, func=AF.Identity, scale=-1.0),
                  r=gk(g5), w=["cumq_bf"])
                for t8 in range(8):
                    for qi, gt in enumerate((g1, g2)):
                        A("pe", lambda e, t8=t8, qi=qi, gt=gt: e.transpose(
                            out=pb[6][:, (t8 * 2 + qi) * 4:(t8 * 2 + qi) * 4 + 4], in_=gt[:, t8 * 128:(t8 + 1) * 128],
                            identity=ident_f[0:4, 0:4]), r=gk(gt) + ["ident_f"], w=[("pb", 6)])
                    A("pe", lambda e, t8=t8: e.transpose(
                        out=pb[7][:, t8 * 4:t8 * 4 + 4], in_=g5[:, t8 * 128:(t8 + 1) * 128],
                        identity=ident_f[0:4, 0:4]), r=gk(g5) + ["ident_f"], w=[("pb", 7)])
                A("dve", lambda e: e.tensor_copy(out=wtm[:].rearrange("p a b c -> p (a b c)"), in_=pb[6][:, 0:64]),
                  r=[("pb", 6)], w=["wtm"])
                A("dve", lambda e: e.tensor_copy(out=cumspT[:, half * 8:half * 8 + 8, :].rearrange("p a b -> p (a b)"),
                                                 in_=pb[7][:, 0:32]), r=[("pb", 7)], w=["cumspT"])
                for h in range(4):
                    A("pe", lambda e, h=h: e.matmul(pb[5][:, h * 8:(h + 1) * 8], lhsT=selF[:, h, :], rhs=dsm,
                                                    start=True, stop=True), r=["selF", "dsm"], w=[("pb", 5)])
                A("dve", lambda e: e.tensor_copy(out=dbc[:].rearrange("p a b -> p (a b)"), in_=pb[5][:, 0:32]),
                  r=[("pb", 5)], w=["dbc"])
                if dbg and first_layer_dbg and half == 0:
                    A("sp", lambda e: e.dma_start(out=d_gt[:, :], in_=wtm[:].rearrange("p a b c -> p (a b c)")),
                      r=["wtm"], dma="dbg_gt")

                if STOP_AFTER == "G":
                    continue
                for hp in range(2):
                    P.barrier()
                    qT = carve(0, 128, [2, NHALF], BF16)
                    kT = carve(4096, 128, [2, NHALF], BF16)
                    zT = carve(8192, 128, [2, NHALF], BF16)
                    vext = carve(12288, 128, [8, 2, 130], BF16)
                    osb = carve(16448, 128, [8, 256], BF16)
                    pre = [carve(20544 + i * 2064, 128, [3 + NHALF + 5], BF16) for i in range(2)]
                    dg4 = carve(24672, 128, [16, 128], BF16)
                    bbc = carve(28768, 128, [512], F32)
                    tbase = 30816
                    kp = [carve(tbase + i * 256, 128, [128], BF16) for i in range(2)]
                    spb = [carve(tbase + 512 + i * 256, 128, [128], BF16) for i in range(2)]
                    cbf = [carve(tbase + 1024 + i * 264, 128, [130], BF16) for i in range(2)]
                    hhf = [carve(tbase + 1552 + i * 512, 128, [128], F32) for i in range(2)]
                    hnb = [carve(tbase + 2576 + i * 256, 128, [128], BF16) for i in range(2)]
                    osig = carve(tbase + 3088, 128, [256], F32)
                    stt = [carve(tbase + 4112 + i * 64, 128, [16], F32) for i in range(2)]
                    for ci in range(4):
                        chunk = (0 if ci < 2 else 4) + hp * 2 + (ci % 2)
                        for j in range(4):
                            A("dve", lambda e, ci=ci, j=j, chunk=chunk: e.tensor_scalar(
                                out=dg4[:, ci * 4 + j, :], in0=ident_b[:],
                                scalar1=vecs[:, V_CW + j * 8 + chunk:V_CW + j * 8 + chunk + 1], scalar2=None,
                                op0=ALU.mult), r=["ident_b", "vecs"], w=[("dg4", ci)])
                    A("sp", lambda e, hp=hp: e.dma_start(out=bbc[:, 0:256],
                                                         in_=bin_d[l, :, C_MV + hp * 256:C_MV + hp * 256 + 256].partition_broadcast(128)),
                      w=["bbc"], r=["bbc"], dma="bbc")
                    A("sp", lambda e, hp=hp: e.dma_start(out=bbc[:, 256:512],
                                                         in_=bin_d[l, :, C_MO + hp * 256:C_MO + hp * 256 + 256].partition_broadcast(128)),
                      w=["bbc"], r=["bbc"], dma="bbc")
                    A("dve", lambda e: e.memset(vext, 1.0), w=["vext"], r=["vext"])
                    slot, offs = jobs.get()
                    for ci in range(4):
                        chunk = (0 if ci < 2 else 4) + hp * 2 + (ci % 2)
                        pr = pre[ci % 2]
                        dstT = qT if ci < 2 else kT
                        bcol = (V_BQ if ci < 2 else V_BK) + hp * 2 + (ci % 2)
                        A("act", lambda e, pr=pr, chunk=chunk: e.copy(out=pr[:, 0:3], in_=halo[:, chunk, 0:3]),
                          r=["halo"], w=[("pre", ci % 2, 0)])

                        def ev(bank, tt, pr=pr, bcol=bcol, ci=ci):
                            A("act", lambda e: e.activation(out=pr[:, 3 + tt * 512:3 + (tt + 1) * 512], in_=pb[bank][:],
                                                            func=AF.Identity, bias=vecs[:, bcol:bcol + 1], scale=1.0),
                              r=[("pb", bank), "vecs"], w=[("pre", ci % 2, tt)])
                        proj_fm(slot, ci * 128, 128, ev)
                        A("act", lambda e, pr=pr, chunk=chunk: e.copy(out=halo[:, chunk, 0:3], in_=pr[:, NHALF:NHALF + 3]),
                          r=[("pre", ci % 2, 1)], w=["halo"])
                        for tt in range(2):
                            bank = 2 + (tt % 2)
                            for j in range(4):
                                A("pe", lambda e, j=j, tt=tt, bank=bank, pr=pr, ci=ci: e.matmul(
                                    pb[bank][:], lhsT=dg4[:, ci * 4 + j, :], rhs=pr[:, tt * 512 + j:tt * 512 + j + 512],
                                    start=(j == 0), stop=(j == 3)),
                                  r=[("dg4", ci), ("pre", ci % 2, 0), ("pre", ci % 2, 1)], w=[("pb", bank)])
                            A("act", lambda e, tt=tt, bank=bank, dstT=dstT, ci=ci, chunk=chunk: e.activation(
                                out=dstT[:, ci % 2, tt * 512:(tt + 1) * 512], in_=pb[bank][:], func=AF.Silu,
                                bias=vecs[:, V_CB + chunk:V_CB + chunk + 1], scale=1.0),
                              r=[("pb", bank), "vecs"], w=[("qk", ci, tt)])
                    if dbg and first_layer_dbg and half == 0 and hp == 0:
                        A("sp", lambda e: e.dma_start(out=d_q[:, :, :], in_=qT), r=[("qk", 0, 0), ("qk", 0, 1), ("qk", 1, 0), ("qk", 1, 1)], dma="dbg_q")
                    slot, offs = jobs.get()
                    for i in range(2):
                        def ev(bank, tt, i=i):
                            A("act", lambda e: e.activation(out=zT[:, i, tt * 512:(tt + 1) * 512], in_=pb[bank][:],
                                                            func=AF.Silu, bias=vecs[:, V_BZ + hp * 2 + i:V_BZ + hp * 2 + i + 1],
                                                            scale=1.0), r=[("pb", bank), "vecs"], w=[("zT", i, tt)])
                        proj_fm(slot, i * 128, 128, ev)
                    slot, offs = jobs.get()

                    def ev_vo(bank, t8):
                        A("dve", lambda e: e.tensor_tensor(
                            out=vext[:, t8, :, 0:128], in0=pb[bank][:, 0:256].rearrange("p (a b) -> p a b", b=128),
                            in1=bbc[:, 0:256].rearrange("p (a b) -> p a b", b=128), op=ALU.add),
                          r=[("pb", bank), "bbc", "vext"], w=[("vext", t8)])
                        A("dve", lambda e: e.tensor_tensor(out=osig, in0=pb[bank][:, 256:512], in1=bbc[:, 256:512], op=ALU.add),
                          r=[("pb", bank), "bbc"], w=["osig"])
                        A("act", lambda e: e.activation(out=osb[:, t8, :], in_=osig, func=AF.Sigmoid),
                          r=["osig"], w=[("osb", t8)])
                    proj_tm(slot, 0, 512, ev_vo)
                    P.barrier()
                    it = 0
                    for hh in range(2):
                        h = hp * 2 + hh
                        for c8 in range(8):
                            sset = it % 2
                            it += 1
                            bT, bSt, bN, bC, bT2 = sset, 2 + sset, 4 + sset, 6, 7
                            tok = slice(c8 * 128, (c8 + 1) * 128)
                            wcol = wtm[:, c8, 0, h:h + 1]
                            fcol = wtm[:, c8, 1, h:h + 1]
                            dcol = dbc[:, h, c8:c8 + 1]
                            qk_r = [("qk", hh, c8 // 4), ("qk", 2 + hh, c8 // 4)]
                            tpv = pbb(bT)[:, 0:128]
                            stv = pb[bSt][:, 0:128]
                            tp2v = pbb(bT2)[:, 0:128]
                            A("pe", lambda e, hh=hh, tok=tok, tpv=tpv: e.transpose(out=tpv, in_=kT[:, hh, tok], identity=ident_b[:]),
                              r=qk_r + ["ident_b"], w=[("pb", bT)])
                            A("act", lambda e, tpv=tpv, wcol=wcol, sset=sset: e.activation(
                                out=kp[sset], in_=tpv, func=AF.Identity, scale=wcol),
                              r=[("pb", bT), "wtm"], w=[("kp", sset)])
                            A("pe", lambda e, hh=hh, tok=tok, stv=stv: e.matmul(stv, lhsT=kT[:, hh, tok], rhs=qT[:, hh, tok],
                                                                               start=True, stop=True),
                              r=qk_r, w=[("pb", bSt)])
                            A("dve", lambda e, stv=stv, wcol=wcol, sset=sset: e.scalar_tensor_tensor(
                                out=spb[sset], in0=stv, scalar=wcol, in1=maskT[:], op0=ALU.mult, op1=ALU.mult),
                              r=[("pb", bSt), "wtm", "maskT"], w=[("spb", sset)])
                            A("dve", lambda e, h=h, dcol=dcol, sset=sset: e.tensor_scalar(
                                out=cbf[sset], in0=Cst[:, h, :], scalar1=dcol, scalar2=None, op0=ALU.mult),
                              r=["Cst", "dbc"], w=[("cbf", sset)])
                            A("pe", lambda e, sset=sset, c8=c8, hh=hh, bN=bN: e.matmul(
                                pb[bN][:, 0:130], lhsT=spb[sset], rhs=vext[:, c8, hh, :], start=True, stop=False),
                              r=[("spb", sset), ("vext", c8), "vext"], w=[("pb", bN)])
                            A("pe", lambda e, sset=sset, hh=hh, tok=tok, bN=bN: e.matmul(
                                pb[bN][:, 0:130], lhsT=qT[:, hh, tok], rhs=cbf[sset], start=False, stop=True),
                              r=qk_r + [("cbf", sset)], w=[("pb", bN)])
                            A("pe", lambda e, sset=sset, c8=c8, hh=hh, bC=bC: e.matmul(
                                pb[bC][:, 0:130], lhsT=kp[sset], rhs=vext[:, c8, hh, :], start=True, stop=True),
                              r=[("kp", sset), ("vext", c8), "vext"], w=[("pb", bC)])
                            A("dve", lambda e, h=h, dcol=dcol, bC=bC: e.scalar_tensor_tensor(
                                out=Cst[:, h, :], in0=Cst[:, h, :], scalar=dcol, in1=pb[bC][:, 0:130],
                                op0=ALU.mult, op1=ALU.add), r=["Cst", "dbc", ("pb", bC)], w=["Cst"])
                            sv = stt[sset]
                            A("dve", lambda e, bN=bN, fcol=fcol, sv=sv: e.tensor_scalar(
                                out=sv[:, 12:13], in0=pb[bN][:, 128:129], scalar1=-1.0, scalar2=fcol,
                                op0=ALU.mult, op1=ALU.max), r=[("pb", bN), "wtm"], w=[("stt", sset)])
                            A("dve", lambda e, bN=bN, sv=sv: e.tensor_tensor(
                                out=sv[:, 0:1], in0=sv[:, 12:13], in1=pb[bN][:, 128:129], op=ALU.max),
                              r=[("pb", bN), ("stt", sset)], w=[("stt", sset)])
                            A("dve", lambda e, sv=sv: e.reciprocal(out=sv[:, 1:2], in_=sv[:, 0:1]),
                              r=[("stt", sset)], w=[("stt", sset)])
                            A("dve", lambda e, bN=bN, sv=sv, sset=sset, c8=c8, hh=hh: e.scalar_tensor_tensor(
                                out=hhf[sset], in0=pb[bN][:, 0:128], scalar=sv[:, 1:2], in1=osb[:, c8, hh * 128:(hh + 1) * 128],
                                op0=ALU.mult, op1=ALU.mult), r=[("pb", bN), ("stt", sset), ("osb", c8)], w=[("hhf", sset)])
                            A("dve", lambda e, sv=sv, sset=sset: e.bn_stats(out=sv[:, 2:8], in_=hhf[sset]),
                              r=[("hhf", sset)], w=[("stt", sset)])
                            A("dve", lambda e, sv=sv: e.bn_aggr(out=sv[:, 8:10], in_=sv[:, 2:8]),
                              r=[("stt", sset)], w=[("stt", sset)])
                            A("act", lambda e, sv=sv: e.activation(out=sv[:, 10:11], in_=sv[:, 9:10], func=AF.Ln,
                                                                   bias=cst[:, CE5:CE5 + 1], scale=1.0),
                              r=[("stt", sset), "cst"], w=[("stt", sset)])
                            A("act", lambda e, sv=sv: e.activation(out=sv[:, 11:12], in_=sv[:, 10:11], func=AF.Exp, scale=-0.5),
                              r=[("stt", sset)], w=[("stt", sset)])
                            A("dve", lambda e, sv=sv, sset=sset: e.tensor_scalar(
                                out=hnb[sset], in0=hhf[sset], scalar1=sv[:, 8:9], scalar2=sv[:, 11:12],
                                op0=ALU.subtract, op1=ALU.mult), r=[("hhf", sset), ("stt", sset)], w=[("hnb", sset)])
                            A("pe", lambda e, sset=sset, tp2v=tp2v: e.transpose(out=tp2v, in_=hnb[sset], identity=ident_b[:]),
                              r=[("hnb", sset), "ident_b"], w=[("pb", bT2)])
                            A("dve", lambda e, tp2v=tp2v, h=h, hh=hh, tok=tok: e.scalar_tensor_tensor(
                                out=yTa[:, h, tok], in0=tp2v, scalar=vecs[:, V_HG + h:V_HG + h + 1], in1=zT[:, hh, tok],
                                op0=ALU.mult, op1=ALU.mult),
                              r=[("pb", bT2), "vecs", ("zT", hh, c8 // 4)], w=[("yTa", h)])
                if dbg and first_layer_dbg and half == 0:
                    A("sp", lambda e: e.dma_start(out=d_ya[:, :, :], in_=yTa[:]), r=[("yTa", i) for i in range(4)], dma="dbg_ya")

                if STOP_AFTER == "A":
                    continue
                P.barrier()
                fqT = carve(0, 128, [2, NHALF], BF16)
                fzT = carve(8192, 64, [4, NHALF], BF16)
                ptb = [carve(16384 + i * 1024, 128, [512], BF16) for i in range(2)]
                rden = carve(18432, 128, [512], F32)
                t1 = carve(20480, 64, [512], F32)
                ytmp = carve(22528, 64, [512], BF16)
                bbf = carve(23552, 128, [256], F32)
                A("sp", lambda e: e.dma_start(out=bbf, in_=bin_d[l, :, C_FV:C_FV + 256].partition_broadcast(128)),
                  w=["bbf"], dma="bbf")
                slot, offs = jobs.get()
                for i in range(2):
                    def evq(bank, tt, i=i):
                        A("dve", lambda e: e.tensor_scalar(out=fqT[:, i, tt * 512:(tt + 1) * 512], in0=pb[bank][:],
                                                           scalar1=vecs[:, V_BFQ + i:V_BFQ + i + 1], scalar2=0.125,
                                                           op0=ALU.add, op1=ALU.mult),
                          r=[("pb", bank), "vecs"], w=[("fqT", tt)])
                    proj_fm(slot, i * 128, 128, evq)

                    def evk(bank, tt, i=i):
                        A("act", lambda e: e.activation(out=fkT[:, i, t0 + tt * 512:t0 + (tt + 1) * 512], in_=pb[bank][:],
                                                        func=AF.Identity, bias=vecs[:, V_BFK + i:V_BFK + i + 1], scale=1.0),
                          r=[("pb", bank), "vecs"], w=["fkT"])
                    proj_fm(slot, 256 + i * 128, 128, evk)
                slot, offs = jobs.get()

                def ev_fv(bank, t8):
                    A("dve", lambda e: e.tensor_tensor(
                        out=Vext[:, half * 8 + t8, :, 0:64], in0=pb[bank][:, 0:256].rearrange("p (a b) -> p a b", b=64),
                        in1=bbf.rearrange("p (a b) -> p a b", b=64), op=ALU.add),
                      r=[("pb", bank), "bbf"], w=["Vext"])
                proj_tm(slot, 0, 256, ev_fv)
                for h in range(4):
                    def evz(bank, tt, h=h):
                        A("act", lambda e: e.activation(out=fzT[:, h, tt * 512:(tt + 1) * 512], in_=pb[bank][0:64, :],
                                                        func=AF.Silu, bias=vec64[:, h:h + 1], scale=1.0),
                          r=[("pb", bank), "vec64"], w=[("fzT", tt)])
                    proj_fm(slot, 256 + h * 64, 64, evz)
                P.barrier()
                nS = 0
                nO = 0
                for h in range(4):
                    kc, pbase = h // 2, (h % 2) * 64
                    for Q in range(2):
                        q0t = (t0 + Q * 512) // 128
                        nkb = q0t + 4
                        bO = 2 + (nO % 2)
                        nO += 1
                        for kb in range(nkb):
                            j = kb - q0t
                            nc0 = max(0, j) * 128
                            N = 512 - nc0
                            bS = nS % 2
                            pt = ptb[nS % 2]
                            nS += 1
                            qs = slice(Q * 512 + nc0, Q * 512 + 512)
                            regs = [(0, N, False)] if j < 0 else ([(0, 128, True)] + ([(128, N, False)] if N > 128 else []))
                            for (ra, rb, dg) in regs:
                                qsr = slice(Q * 512 + nc0 + ra, Q * 512 + nc0 + rb)
                                A("pe", lambda e, ra=ra, rb=rb, qsr=qsr: e.matmul(
                                    pb[bS][:, ra:rb], lhsT=fkT[pbase:pbase + 64, kc, kb * 128:(kb + 1) * 128],
                                    rhs=fqT[pbase:pbase + 64, kc, qsr], start=True, stop=False),
                                  r=["fkT", ("fqT", Q)], w=[("pb", bS)])
                                A("pe", lambda e, ra=ra, rb=rb, qsr=qsr, dg=dg: e.matmul(
                                    pb[bS][:, ra:rb], lhsT=selB[:, h, :], rhs=cumq_bf[:, qsr], start=False, stop=(not dg)),
                                  r=["selB", "cumq_bf"], w=[("pb", bS)])
                                if dg:
                                    A("pe", lambda e, ra=ra, rb=rb: e.matmul(
                                        pb[bS][:, ra:rb], lhsT=ident_b[:], rhs=negmask[:], start=False, stop=True),
                                      r=["ident_b", "negmask"], w=[("pb", bS)])
                            A("act", lambda e: e.activation(
                                out=pt[:, 0:N], in_=pb[bS][:, 0:N], func=AF.Exp, bias=cumspT[:, kb, h:h + 1], scale=1.0),
                              r=[("pb", bS), "cumspT"], w=[("pt", bS)])
                            A("pe", lambda e: e.matmul(
                                pb[bO][0:65, nc0:512], lhsT=Vext[:, kb, h, :], rhs=pt[:, 0:N],
                                start=(kb == 0), stop=(kb == nkb - 1)),
                              r=["Vext", ("pt", bS)], w=[("pb", bO)])
                        A("act", lambda e: e.copy(out=rden[0:1, :], in_=pb[bO][64:65, :]),
                          r=[("pb", bO)], w=["rden"])
                        A("dve", lambda e: e.reciprocal(out=rden[0:1, :], in_=rden[0:1, :]), r=["rden"], w=["rden"])
                        A("pe", lambda e: e.matmul(pb[4][0:64, :], lhsT=onesF[0:1, 0:64], rhs=rden[0:1, :],
                                                   start=True, stop=True), r=["rden", "onesF"], w=[("pb", 4)])
                        A("dve", lambda e: e.tensor_tensor(
                            out=t1, in0=pb[bO][0:64, :], in1=fzT[:, h, Q * 512:(Q + 1) * 512], op=ALU.mult),
                          r=[("pb", bO), ("fzT", Q)], w=["t1"])
                        if pbase == 0:
                            A("dve", lambda e: e.tensor_tensor(
                                out=yTb[0:64, kc, Q * 512:(Q + 1) * 512], in0=t1, in1=pb[4][0:64, :], op=ALU.mult),
                              r=["t1", ("pb", 4)], w=[("yTb", kc)])
                        else:
                            A("dve", lambda e: e.tensor_tensor(out=ytmp, in0=t1, in1=pb[4][0:64, :], op=ALU.mult),
                              r=["t1", ("pb", 4)], w=["ytmp"])
                            A("act", lambda e: e.copy(out=yTb[64:128, kc, Q * 512:(Q + 1) * 512], in_=ytmp),
                              r=["ytmp"], w=[("yTb", kc)])
                if dbg and first_layer_dbg and half == 0:
                    A("sp", lambda e: e.dma_start(out=d_yb[:, :, :], in_=yTb[:]), r=[("yTb", 0), ("yTb", 1)], dma="dbg_yb")

                if STOP_AFTER == "B":
                    continue
                P.barrier()
                sg = [carve(i * 2048, 128, [512], F32) for i in range(2)]
                zcT = carve(4096, 128, [2, NHALF], BF16)
                dg31 = carve(8192, 128, [31, 128], BF16)
                yf = carve(16384, 128, [512], F32)
                ybf = carve(18432, 128, [512], BF16)
                ysq = carve(19456, 128, [512], BF16)
                msq = carve(20480, 128, [512], F32)
                var = carve(22528, 128, [512], F32)
                tt_ = carve(24576, 128, [512], F32)
                ss_ = carve(26624, 128, [512], F32)
                if half == 0:
                    A("dve", lambda e: e.memset(ubuf[:, :, 0:30], 0.0), w=["ubuf"], r=["ubuf"])
                else:
                    A("act", lambda e: e.copy(out=ubuf[:, :, 0:30], in_=ubuf[:, :, NHALF:NHALF + 30]), w=["ubuf"], r=["ubuf"])
                slot, offs = jobs.get()
                for c in range(2):
                    for tt in range(2):
                        bank = next_acc()
                        for k in range(8):
                            A("pe", lambda e, k=k, tt=tt, bank=bank, c=c: e.matmul(
                                pb[bank][:], lhsT=wbuf[slot][:, k, 256 + c * 128:256 + (c + 1) * 128],
                                rhs=hT[:, k, tt * 512:(tt + 1) * 512], start=(k == 0), stop=(k == 7)),
                              r=[("wb", slot), ("hT", tt)], w=[("pb", bank)])
                        A("act", lambda e, bank=bank, c=c, tt=tt: e.activation(
                            out=sg[tt % 2], in_=pb[bank][:], func=AF.Sigmoid, bias=vecs[:, V_BCG + c:V_BCG + c + 1], scale=1.0),
                          r=[("pb", bank), "vecs"], w=[("sg", tt % 2)])
                        bank = next_acc()
                        for k in range(8):
                            A("pe", lambda e, k=k, tt=tt, bank=bank, c=c: e.matmul(
                                pb[bank][:], lhsT=wbuf[slot][:, k, c * 128:(c + 1) * 128],
                                rhs=hT[:, k, tt * 512:(tt + 1) * 512], start=(k == 0), stop=(k == 7)),
                              r=[("wb", slot), ("hT", tt)], w=[("pb", bank)])
                        A("dve", lambda e, bank=bank, c=c, tt=tt: e.scalar_tensor_tensor(
                            out=ubuf[:, c, 30 + tt * 512:30 + (tt + 1) * 512], in0=pb[bank][:],
                            scalar=vecs[:, V_BCA + c:V_BCA + c + 1], in1=sg[tt % 2], op0=ALU.add, op1=ALU.mult),
                          r=[("pb", bank), "vecs", ("sg", tt % 2)], w=["ubuf"])
                slot, offs = jobs.get()
                for c in range(2):
                    def evcz(bank, tt, c=c):
                        A("act", lambda e: e.activation(out=zcT[:, c, tt * 512:(tt + 1) * 512], in_=pb[bank][:], func=AF.Silu,
                                                        bias=vecs[:, V_BCZ + c:V_BCZ + c + 1], scale=1.0),
                          r=[("pb", bank), "vecs"], w=[("zcT", c)])
                    proj_fm(slot, c * 128, 128, evcz)
                for c in range(2):
                    for j in range(31):
                        A("dve", lambda e, c=c, j=j: e.tensor_scalar(
                            out=dg31[:, j, :], in0=ident_b[:], scalar1=vecs[:, V_DW + j * 2 + c:V_DW + j * 2 + c + 1],
                            scalar2=None, op0=ALU.mult), r=["ident_b", "vecs"], w=["dg31"])
                    for tt in range(2):
                        bank = 2 + tt
                        for j in range(31):
                            A("pe", lambda e, c=c, j=j, tt=tt, bank=bank: e.matmul(
                                pb[bank][:], lhsT=dg31[:, j, :], rhs=ubuf[:, c, tt * 512 + j:tt * 512 + j + 512],
                                start=(j == 0), stop=(j == 30)), r=["dg31", "ubuf"], w=[("pb", bank)])
                        bcol = vecs[:, V_DWB + c:V_DWB + c + 1]
                        A("act", lambda e, bank=bank, bcol=bcol: e.activation(out=yf, in_=pb[bank][:], func=AF.Identity,
                                                                              bias=bcol, scale=1.0),
                          r=[("pb", bank), "vecs"], w=["yf"])
                        A("act", lambda e, bank=bank, bcol=bcol: e.activation(out=ysq, in_=pb[bank][:], func=AF.Square,
                                                                              bias=bcol, scale=1.0),
                          r=[("pb", bank), "vecs"], w=["ysq"])
                        A("dve", lambda e: e.tensor_copy(out=ybf, in_=yf), r=["yf"], w=["ybf"])
                        A("pe", lambda e: e.matmul(pb[4][:], lhsT=blk64[:], rhs=ybf, start=True, stop=True),
                          r=["blk64", "ybf"], w=[("pb", 4)])
                        A("pe", lambda e: e.matmul(pb[5][:], lhsT=blk64[:], rhs=ysq, start=True, stop=True),
                          r=["blk64", "ysq"], w=[("pb", 5)])
                        A("act", lambda e: e.activation(out=msq, in_=pb[4][:], func=AF.Square), r=[("pb", 4)], w=["msq"])
                        A("dve", lambda e: e.tensor_tensor(out=var, in0=pb[5][:], in1=msq, op=ALU.subtract),
                          r=[("pb", 5), "msq"], w=["var"])
                        A("act", lambda e: e.activation(out=var, in_=var, func=AF.Ln, bias=cst[:, CE5:CE5 + 1], scale=1.0),
                          r=["var", "cst"], w=["var"])
                        A("act", lambda e: e.activation(out=var, in_=var, func=AF.Exp, scale=-0.5), r=["var"], w=["var"])
                        A("dve", lambda e: e.tensor_tensor(out=tt_, in0=yf, in1=pb[4][:], op=ALU.subtract),
                          r=["yf", ("pb", 4)], w=["tt_"])
                        A("dve", lambda e: e.tensor_tensor(out=tt_, in0=tt_, in1=var, op=ALU.mult), r=["tt_", "var"], w=["tt_"])
                        A("act", lambda e, c=c: e.activation(out=ss_, in_=tt_, func=AF.Silu,
                                                             scale=vecs[:, V_LNG + c:V_LNG + c + 1],
                                                             bias=vecs[:, V_LNB + c:V_LNB + c + 1]),
                          r=["tt_", "vecs"], w=["ss_"])
                        A("dve", lambda e, c=c, tt=tt: e.tensor_tensor(out=yTc[:, c, tt * 512:(tt + 1) * 512], in0=ss_,
                                                                         in1=zcT[:, c, tt * 512:(tt + 1) * 512], op=ALU.mult),
                          r=["ss_", ("zcT", c)], w=[("yTc", c)])
                if dbg and first_layer_dbg and half == 0:
                    A("sp", lambda e: e.dma_start(out=d_yc[:, :, :], in_=yTc[:]), r=[("yTc", 0), ("yTc", 1)], dma="dbg_yc")

                if STOP_AFTER == "C":
                    continue
                P.barrier()
                ycat = [yTa[:, 0, :], yTa[:, 1, :], yTa[:, 2, :], yTa[:, 3, :], yTb[:, 0, :], yTb[:, 1, :],
                        yTc[:, 0, :], yTc[:, 1, :]]
                for jb in range(2):
                    slot, offs = jobs.get()
                    for d4 in range(4):
                        dc = jb * 4 + d4
                        for tt in range(2):
                            bank = next_acc()
                            for k in range(8):
                                A("pe", lambda e, k=k, tt=tt, bank=bank, d4=d4, slot=slot: e.matmul(
                                    pb[bank][:], lhsT=wbuf[slot][:, k, d4 * 128:(d4 + 1) * 128],
                                    rhs=ycat[k][:, tt * 512:(tt + 1) * 512], start=(k == 0), stop=(k == 7)),
                                  r=[("wb", slot), "ycat"], w=[("pb", bank)])
                            tok = slice(t0 + tt * 512, t0 + (tt + 1) * 512)
                            A("dve", lambda e, bank=bank, dc=dc, tok=tok: e.scalar_tensor_tensor(
                                out=xT[:, dc, tok], in0=pb[bank][:], scalar=modv[:, l, 16 + dc, s:s + 1], in1=xT[:, dc, tok],
                                op0=ALU.mult, op1=ALU.add),
                              r=[("pb", bank), "modv", ("xT", (t0 + tt * 512) // 512)], w=[("xT", (t0 + tt * 512) // 512)])

        for s in range(NS):
            load_x(s)
            for l in range(L):
                if s == 0:
                    load_vecs(l)
                    ada(l)
                layer(l, s, first_layer_dbg=(s == 0 and l == 0))
            store_x(s)
        P.finalize_and_emit()
    return nc


def _chunks(v):
    return np.ascontiguousarray(v.reshape(-1, 128).T)


def _host_vecs(inp, l):
    b_in = inp["b_in"][l]
    cols = [
        _chunks(inp["norm_g"][l]), _chunks(inp["b_ada"][l]),
        _chunks(b_in[C_MQ:C_MQ + 512]), _chunks(b_in[C_MK:C_MK + 512]), _chunks(b_in[C_MZ:C_MZ + 512]),
        _chunks(b_in[C_FQ:C_FQ + 256]), _chunks(b_in[C_FK:C_FK + 256]),
        _chunks(b_in[C_CA:C_CA + 256]), _chunks(b_in[C_CG:C_CG + 256]), _chunks(b_in[C_CZ:C_CZ + 256]),
        _chunks(inp["m_conv_b"][l]), _chunks(inp["m_hn_g"][l]),
        _chunks(inp["c_dw_b"][l]), _chunks(inp["c_ln_g"][l]), _chunks(inp["c_ln_b"][l]),
    ]
    cw = inp["m_conv_w"][l]
    cols.append(np.concatenate([_chunks(cw[j]) for j in range(4)], axis=1))
    dw = inp["c_dw_w"][l]
    cols.append(np.concatenate([_chunks(dw[j]) for j in range(31)], axis=1))
    v = np.concatenate(cols, axis=1).astype(np.float32)
    assert v.shape == (128, NV), v.shape
    vec64 = np.ascontiguousarray(np.concatenate([b_in[C_FZ:C_FZ + 256].reshape(4, 64).T, b_in[C_FQ:C_FQ + 256].reshape(4, 64).T,
                                                 b_in[C_FK:C_FK + 256].reshape(4, 64).T], axis=1)).astype(np.float32)
    gbv = np.stack([b_in[C_MI:C_MI + 4], b_in[C_MF:C_MF + 4], b_in[C_FF:C_FF + 4]], axis=1).astype(np.float32)
    return v, vec64, gbv


_NC_CACHE = {}


def _get_nc(L, NS, final_norm, dbg=False):
    key = (L, NS, final_norm, dbg)
    if key not in _NC_CACHE:
        _NC_CACHE[key] = build(L, NS, final_norm, dbg)
    return _NC_CACHE[key]


FUSED = False


def kernel(x, c, norm_g, w_ada, b_ada, w_in, b_in, m_conv_w, m_conv_b, m_hn_g,
           c_dw_w, c_dw_b, c_ln_g, c_ln_b, w_out, final_g):
    inp = dict(norm_g=np.asarray(norm_g), b_ada=np.asarray(b_ada), b_in=np.asarray(b_in),
               m_conv_w=np.asarray(m_conv_w), m_conv_b=np.asarray(m_conv_b), m_hn_g=np.asarray(m_hn_g),
               c_dw_w=np.asarray(c_dw_w), c_dw_b=np.asarray(c_dw_b), c_ln_g=np.asarray(c_ln_g),
               c_ln_b=np.asarray(c_ln_b))
    x = np.asarray(x, dtype=np.float32)
    c = np.asarray(c, dtype=np.float32)
    w_ada = np.asarray(w_ada, dtype=np.float32)
    w_in = np.asarray(w_in, dtype=np.float32)
    w_out = np.asarray(w_out, dtype=np.float32)
    b_in_a = np.asarray(b_in, dtype=np.float32)
    fg = np.asarray(final_g, dtype=np.float32).reshape(1, D)
    DEPTH = w_in.shape[0]
    hv = [_host_vecs(inp, l) for l in range(DEPTH)]
    n = 8
    if FUSED:
        nc = _get_nc(DEPTH, 2, True)
        in_maps = []
        for i in range(n):
            cs = c[2 * i:2 * i + 2]
            cT = np.ascontiguousarray(cs.reshape(2, 8, 128).transpose(2, 1, 0))
            in_maps.append({
                "x": np.ascontiguousarray(x[2 * i:2 * i + 2]), "cT": cT,
                "vecs": np.stack([h[0] for h in hv]), "vec64": np.stack([h[1] for h in hv]),
                "gb": np.stack([h[2] for h in hv]),
                "w_ada": w_ada, "w_in": w_in, "b_in": np.ascontiguousarray(b_in_a.reshape(DEPTH, 1, DIN)),
                "w_out": w_out, "final_g": fg})
        res = run_bass_kernel_spmd(nc, in_maps, core_ids=list(range(n)))
        return np.concatenate([r["out"] for r in res.results], axis=0)
    cur = x.copy()
    for l in range(DEPTH):
        nc = _get_nc(1, 1, l == DEPTH - 1)
        for sidx in range(2):
            in_maps = []
            for i in range(n):
                b = 2 * i + sidx
                cT = np.ascontiguousarray(c[b:b + 1].reshape(1, 8, 128).transpose(2, 1, 0))
                in_maps.append({
                    "x": np.ascontiguousarray(cur[b:b + 1]), "cT": cT,
                    "vecs": hv[l][0][None], "vec64": hv[l][1][None], "gb": hv[l][2][None],
                    "w_ada": w_ada[l:l + 1], "w_in": w_in[l:l + 1],
                    "b_in": np.ascontiguousarray(b_in_a[l].reshape(1, 1, DIN)),
                    "w_out": w_out[l:l + 1], "final_g": fg})
            res = run_bass_kernel_spmd(nc, in_maps, core_ids=list(range(n)))
            for i in range(n):
                cur[2 * i + sidx] = res.results[i]["out"][0]
    return cur
```

```python
import math
import numpy as np
import concourse.bass as bass
import concourse.mybir as mybir
from concourse.bass_utils import run_bass_kernel_spmd
from contextlib import ExitStack

AF = mybir.ActivationFunctionType
ALU = mybir.AluOpType
F32 = mybir.dt.float32
BF16 = mybir.dt.bfloat16

ENGS = ["pe", "act", "dve", "pool", "sp"]
SEM_EPOCH = 30000
SAME_ENG_SYNC = True
STOP_AFTER = None

S = 2048
D = 1024
NHALF = 1024
DIN = 4364
C_MQ, C_MK, C_MV, C_MO, C_MZ, C_MI, C_MF, C_FQ, C_FK, C_FV, C_FZ, C_FF, C_CA, C_CG, C_CZ = (
    0, 512, 1024, 1536, 2048, 2560, 2564, 2568, 2824, 3080, 3336, 3592, 3596, 3852, 4108)
V_NG, V_BA, V_BQ, V_BK, V_BZ, V_BFQ, V_BFK, V_BCA, V_BCG, V_BCZ, V_CB, V_HG, V_DWB, V_LNG, V_LNB, V_CW, V_DW, NV = (
    0, 8, 32, 36, 40, 44, 46, 48, 50, 52, 54, 62, 66, 68, 70, 72, 104, 166)


class Op:
    __slots__ = ("eng", "idx", "fn", "dma", "deps", "signal", "sig", "dcount", "waits")

    def __init__(self, eng, idx, fn, dma):
        self.eng = eng
        self.idx = idx
        self.fn = fn
        self.dma = dma
        self.deps = None
        self.signal = False
        self.sig = None
        self.dcount = None
        self.waits = []


class _Call:
    __slots__ = ("name", "a", "kw")

    def __init__(self, name, a, kw):
        self.name = name
        self.a = a
        self.kw = kw

    def __call__(self, e):
        return getattr(e, self.name)(*self.a, **self.kw)


class _Rec:
    def __getattr__(self, name):
        return lambda *a, **kw: _Call(name, a, kw)


_REC = _Rec()


class Prog:
    def __init__(self, nc, stack):
        self.nc = nc
        self.stack = stack
        self.ops = {e: [] for e in ENGS}
        self.reg = {}
        self.dma_cnt = {}
        self.dma_sem = {}
        self.pend = {e: None for e in ENGS}

    def barrier(self):
        snap_c = {e: len(self.ops[e]) - 1 for e in ENGS if e != "sp" and len(self.ops[e]) > 0}
        snap_c = {e: i for e, i in snap_c.items()}
        snap_d = {k: c for k, c in self.dma_cnt.items() if not (isinstance(k, tuple) and k[0] == "wb")}
        for e in ENGS:
            self.pend[e] = (dict(snap_c), dict(snap_d))

    def op(self, eng, fn, r=(), w=(), dma=None):
        call = fn(_REC)
        assert isinstance(call, _Call), "op lambda must return e.<instr>(...)"
        fn = call
        o = Op(eng, len(self.ops[eng]), fn, dma)
        deps = []
        for k in r:
            st = self.reg.get(k)
            if st is not None and st[0] is not None:
                deps.append(st[0])
        for k in w:
            st = self.reg.get(k)
            if st is not None:
                if st[0] is not None:
                    deps.append(st[0])
                deps.extend(st[1])
        cdeps = {}
        ddeps = {}
        for d in deps:
            if d.dma is not None:
                ddeps[d.dma] = self.dma_cnt[d.dma]
            else:
                if d.eng == eng and (eng == "pe" or not SAME_ENG_SYNC):
                    continue
                if cdeps.get(d.eng, -1) < d.idx:
                    cdeps[d.eng] = d.idx
        pb = self.pend[eng]
        if pb is not None:
            self.pend[eng] = None
            for e2, i2 in pb[0].items():
                if e2 == eng:
                    continue
                tgt = self.ops[e2][i2]
                j = i2
                while j >= 0 and self.ops[e2][j].dma is not None:
                    j -= 1
                if j >= 0 and cdeps.get(e2, -1) < j:
                    cdeps[e2] = j
            for k, c in pb[1].items():
                if ddeps.get(k, 0) < c:
                    ddeps[k] = c
        o.deps = (cdeps, ddeps)
        for k in r:
            st = self.reg.get(k)
            if st is None:
                st = [None, []]
                self.reg[k] = st
            st[1].append(o)
        for k in w:
            self.reg[k] = [o, []]
        if dma is not None:
            self.dma_cnt[dma] = self.dma_cnt.get(dma, 0) + 16
            o.dcount = self.dma_cnt[dma]
        self.ops[eng].append(o)
        return o

    def finalize_and_emit(self):
        nc = self.nc
        for e in ENGS:
            for o in self.ops[e]:
                for de, di in o.deps[0].items():
                    self.ops[de][di].signal = True
        nsig = {}
        for e in ENGS:
            c = 0
            for o in self.ops[e]:
                if o.signal and o.dma is None:
                    o.sig = c
                    c += 1
            nsig[e] = c
        sems = {}
        for e in ENGS:
            n = (nsig[e] + SEM_EPOCH - 1) // SEM_EPOCH
            sems[e] = [self.stack.enter_context(nc.semaphore(f"s_{e}_{i}")) for i in range(n)]
        for k in self.dma_cnt:
            self.dma_sem[k] = self.stack.enter_context(nc.semaphore(f"d_{len(self.dma_sem)}"))
        for e in ENGS:
            seen = {}
            dseen = {}
            for o in self.ops[e]:
                for de, di in o.deps[0].items():
                    s = self.ops[de][di].sig
                    if seen.get(de, -1) >= s:
                        continue
                    seen[de] = s
                    o.waits.append((sems[de][s // SEM_EPOCH], s % SEM_EPOCH + 1))
                for k, cnt in o.deps[1].items():
                    if dseen.get(k, 0) >= cnt:
                        continue
                    dseen[k] = cnt
                    o.waits.append((self.dma_sem[k], cnt))
        blk = self.stack.enter_context(nc.Block())
        prog = self

        def emit(e, name):
            for o in prog.ops[name]:
                for (s, v) in o.waits:
                    e.wait_ge(s, v)
                ins = o.fn(e)
                if o.dma is not None:
                    ins.then_inc(prog.dma_sem[o.dma], 16)
                elif o.signal:
                    ins.then_inc(sems[name][o.sig // SEM_EPOCH], 1)
            for k, cnt in prog.dma_cnt.items():
                if any(o.dma == k for o in prog.ops[name]):
                    e.wait_ge(prog.dma_sem[k], cnt)

        @blk.tensor
        def _(e):
            emit(e, "pe")

        @blk.scalar
        def _(e):
            emit(e, "act")

        @blk.vector
        def _(e):
            emit(e, "dve")

        @blk.gpsimd
        def _(e):
            emit(e, "pool")

        @blk.sync
        def _(e):
            emit(e, "sp")


def build(L, NS, final_norm, dbg=False):
    nc = bass.Bass("TRN2", target_bir_lowering=False)

    def din(n, s, d=F32):
        return nc.dram_tensor(n, s, d, kind="ExternalInput").ap()

    x_d = din("x", [NS, S, D])
    cT_d = din("cT", [128, 8, NS])
    vecs_d = din("vecs", [L, 128, NV])
    vec64_d = din("vec64", [L, 64, 12])
    gb_d = din("gb", [L, 4, 3])
    wada_d = din("w_ada", [L, D, 3 * D])
    win_d = din("w_in", [L, D, DIN])
    bin_d = din("b_in", [L, 1, DIN])
    wout_d = din("w_out", [L, D, D])
    fg_d = din("final_g", [1, D])
    out_d = nc.dram_tensor("out", [NS, S, D], F32, kind="ExternalOutput").ap()
    cq_scr = nc.dram_tensor("cq_scr", [4, NHALF], BF16).ap()
    if dbg:
        d_h = nc.dram_tensor("d_h", [128, 8, NHALF], BF16, kind="ExternalOutput").ap()
        d_ya = nc.dram_tensor("d_ya", [128, 4, NHALF], BF16, kind="ExternalOutput").ap()
        d_yb = nc.dram_tensor("d_yb", [128, 2, NHALF], BF16, kind="ExternalOutput").ap()
        d_yc = nc.dram_tensor("d_yc", [128, 2, NHALF], BF16, kind="ExternalOutput").ap()
        d_q = nc.dram_tensor("d_q", [128, 2, NHALF], BF16, kind="ExternalOutput").ap()
        d_gt = nc.dram_tensor("d_gt", [128, 64], F32, kind="ExternalOutput").ap()
        d_O = nc.dram_tensor("d_O", [128, 512], F32, kind="ExternalOutput").ap()
        d_pt = nc.dram_tensor("d_pt", [128, 512], BF16, kind="ExternalOutput").ap()
        d_fq = nc.dram_tensor("d_fq", [128, 4, NHALF], BF16, kind="ExternalOutput").ap()
        d_fk = nc.dram_tensor("d_fk", [128, 4, S], BF16, kind="ExternalOutput").ap()
        d_fz = nc.dram_tensor("d_fz", [64, 4, NHALF], BF16, kind="ExternalOutput").ap()
        d_V = nc.dram_tensor("d_V", [128, 16 * 4 * 65], BF16, kind="ExternalOutput").ap()
        d_cs = nc.dram_tensor("d_cs", [128, 64], F32, kind="ExternalOutput").ap()

    with ExitStack() as st:
        P = Prog(nc, st)
        A = P.op

        def sb(n, s, d):
            return st.enter_context(nc.sbuf_tensor(n, s, d))

        xT = sb("xT", [128, 8, S], F32)
        hT = sb("hT", [128, 8, NHALF], BF16)
        yTa = sb("yTa", [128, 4, NHALF], BF16)
        yTb = sb("yTb", [128, 2, NHALF], BF16)
        yTc = sb("yTc", [128, 2, NHALF], BF16)
        wbuf = [sb(f"wbuf{i}", [128, 8, 512], BF16) for i in range(2)]
        fkT = sb("fkT", [128, 2, S], BF16)
        Vext = sb("Vext", [128, 16, 4, 65], BF16)
        ubuf = sb("ubuf", [128, 2, 30 + NHALF], BF16)
        Cst = sb("Cst", [128, 4, 130], F32)
        cumspT = sb("cumspT", [128, 16, 4], F32)
        cumq_bf = sb("cumq_bf", [4, NHALF], BF16)
        carry = sb("carry", [4, 2], F32)
        wtm = sb("wtm", [128, 8, 2, 4], F32)
        dbc = sb("dbc", [128, 4, 8], F32)
        halo = sb("halo", [128, 8, 4], BF16)
        ident_f = sb("ident_f", [128, 128], F32)
        ident_b = sb("ident_b", [128, 128], BF16)
        ones_b = sb("ones_b", [128, 128], BF16)
        maskT = sb("maskT", [128, 128], BF16)
        blk64 = sb("blk64", [128, 128], BF16)
        negmask = sb("negmask", [128, 128], BF16)
        selF = sb("selF", [4, 4, 128], F32)
        selB = sb("selB", [4, 4, 128], BF16)
        onesF = sb("onesF", [128, 64], F32)
        cst = sb("cst", [128, 8], F32)
        vecs = sb("vecs_sb", [128, NV], F32)
        vec64 = sb("vec64_sb", [64, 12], F32)
        gb = sb("gb_sb", [4, 4], F32)
        cact = sb("cact", [128, 8, NS], BF16)
        cin = sb("cin", [128, 8, NS], F32)
        modv = sb("modv", [128, L, 24, NS], F32)
        Gv = sb("Gv", [128, 8], F32)
        SCRB = 36 * 1024
        scr = sb("scr", [128, SCRB // 2], BF16)
        pb = [st.enter_context(nc.psum_tensor(f"pb{i}", [128, 512], F32)) for i in range(8)]

        def carve(off, npart, shape, dt):
            n = 1
            for v in shape:
                n *= v
            nb = n * (4 if dt == F32 else 2)
            assert off % 4 == 0 and off + nb <= SCRB, (off, nb)
            ap = scr[0:npart, off // 2:(off + nb) // 2]
            if dt == F32:
                ap = ap.bitcast(F32)
            if len(shape) == 2:
                ap = ap.rearrange("p (a b) -> p a b", b=shape[1])
            elif len(shape) == 3:
                ap = ap.rearrange("p (a b c) -> p a b c", b=shape[1], c=shape[2])
            return ap

        def pbb(i):
            return pb[i][:].bitcast(BF16)

        CE6, CE5, C1, CLN, C0 = 0, 1, 2, 3, 4
        for i, v in enumerate([1e-6, 1e-5, 1.0, -0.5 * math.log(128.0), 0.0]):
            A("dve", lambda e, i=i, v=v: e.memset(cst[:, i:i + 1], v), w=["cst"], r=["cst"])
        A("pool", lambda e: e.memset(ident_f[:], 0.0), w=["ident_f"])
        A("pool", lambda e: e.affine_select(out=ident_f[:], in_=ident_f[:], pattern=[[-1, 128]],
                                            compare_op=ALU.not_equal, fill=1.0, base=0, channel_multiplier=1),
          r=["ident_f"], w=["ident_f"])
        A("dve", lambda e: e.tensor_copy(out=ident_b[:], in_=ident_f[:]), r=["ident_f"], w=["ident_b"])
        A("dve", lambda e: e.memset(ones_b[:], 1.0), w=["ones_b"])
        A("pool", lambda e: e.memset(maskT[:], 1.0), w=["maskT"])
        A("pool", lambda e: e.affine_select(out=maskT[:], in_=maskT[:], pattern=[[1, 128]],
                                            compare_op=ALU.is_ge, fill=0.0, base=0, channel_multiplier=-1),
          r=["maskT"], w=["maskT"])
        A("pool", lambda e: e.memset(negmask[:], 0.0), w=["negmask"])
        A("pool", lambda e: e.affine_select(out=negmask[:], in_=negmask[:], pattern=[[1, 128]],
                                            compare_op=ALU.is_ge, fill=-30000.0, base=0, channel_multiplier=-1),
          r=["negmask"], w=["negmask"])
        A("dve", lambda e: e.memset(blk64[:], 0.0), w=["blk64"])
        A("dve", lambda e: e.memset(blk64[0:64, 0:64], 1.0 / 64), r=["blk64"], w=["blk64"])
        A("dve", lambda e: e.memset(blk64[64:128, 64:128], 1.0 / 64), r=["blk64"], w=["blk64"])
        A("pool", lambda e: e.memset(selF[:], 0.0), w=["selF"])
        A("pool", lambda e: e.affine_select(out=selF[:], in_=selF[:], pattern=[[1, 4], [0, 128]],
                                            compare_op=ALU.not_equal, fill=1.0, base=0, channel_multiplier=-1),
          r=["selF"], w=["selF"])
        A("dve", lambda e: e.tensor_copy(out=selB[:], in_=selF[:]), r=["selF"], w=["selB"])
        A("dve", lambda e: e.memset(onesF[:], 1.0), w=["onesF"])
        A("dve", lambda e: e.memset(halo[:], 0.0), w=["halo"])
        A("sp", lambda e: e.dma_start(out=cin[:], in_=cT_d[:, :, :]), w=["cin"], dma="cin")
        A("act", lambda e: e.activation(out=cact[:], in_=cin[:], func=AF.Silu), r=["cin"], w=["cact"])

        wstate = {"n": 0}

        def wload(src, segs):
            slot = wstate["n"] % 2
            wstate["n"] += 1
            offs = []
            o = 0
            for (c0, ncol) in segs:
                A("pool", lambda e, o=o, c0=c0, ncol=ncol, slot=slot: e.dma_start(
                    out=wbuf[slot][:, :, o:o + ncol],
                    in_=src[:, c0:c0 + ncol].rearrange("(k p) n -> p k n", p=128)),
                  w=[("wb", slot)], r=[("wb", slot)], dma=("wb", slot))
                offs.append(o)
                o += ncol
            return slot, offs

        class Jobs:
            def __init__(self, lst):
                self.lst = lst
                self.i = 0
                self.loaded = None

            def get(self):
                if self.loaded is None:
                    self.loaded = wload(*self.lst[self.i])
                cur = self.loaded
                self.i += 1
                self.loaded = wload(*self.lst[self.i]) if self.i < len(self.lst) else None
                return cur

        def load_x(s):
            P.barrier()
            xin = [carve(i * 4096, 128, [D], F32) for i in range(2)]
            for t in range(16):
                A("sp", lambda e, t=t: e.dma_start(out=xin[t % 2], in_=x_d[s, t * 128:(t + 1) * 128, :]),
                  w=[("xin", t % 2)], dma=("xin", t % 2))
                for hb in range(2):
                    bank = 2 * (t % 2) + hb
                    for c4 in range(4):
                        c = hb * 4 + c4
                        A("pe", lambda e, t=t, c=c, c4=c4, bank=bank: e.transpose(
                            out=pb[bank][:, c4 * 128:(c4 + 1) * 128], in_=xin[t % 2][:, c * 128:(c + 1) * 128],
                            identity=ident_f[:]), r=[("xin", t % 2), "ident_f"], w=[("pb", bank)])
                    eng = "act" if hb == 0 else "dve"
                    if eng == "act":
                        A("act", lambda e, t=t, hb=hb, bank=bank: e.copy(
                            out=xT[:, hb * 4:hb * 4 + 4, t * 128:(t + 1) * 128],
                            in_=pb[bank][:].rearrange("p (a b) -> p a b", b=128)),
                          r=[("pb", bank)], w=[("xT", t // 4)])
                    else:
                        A("dve", lambda e, t=t, hb=hb, bank=bank: e.tensor_copy(
                            out=xT[:, hb * 4:hb * 4 + 4, t * 128:(t + 1) * 128],
                            in_=pb[bank][:].rearrange("p (a b) -> p a b", b=128)),
                          r=[("pb", bank)], w=[("xT", t // 4)])

        def store_x(s):
            P.barrier()
            xo = [carve(i * 4096, 128, [D], F32) for i in range(2)]
            fgb = carve(8192, 128, [D], F32)
            junk = carve(12288, 128, [D], F32)
            stat = carve(16384, 128, [8], F32)
            if final_norm:
                A("sp", lambda e: e.dma_start(out=fgb, in_=fg_d.partition_broadcast(128)), w=["fgb"], dma="fgb")
            for t in range(16):
                for hb in range(2):
                    bank = 2 * (t % 2) + hb
                    for c4 in range(4):
                        c = hb * 4 + c4
                        A("pe", lambda e, t=t, c=c, c4=c4, bank=bank: e.transpose(
                            out=pb[bank][:, c4 * 128:(c4 + 1) * 128], in_=xT[:, c, t * 128:(t + 1) * 128],
                            identity=ident_f[:]), r=[("xT", t // 4), "ident_f"], w=[("pb", bank)])
                    if hb == 0:
                        A("act", lambda e, t=t, bank=bank: e.copy(out=xo[t % 2][:, 0:512], in_=pb[bank][:]),
                          r=[("pb", bank)], w=[("xo", t % 2)])
                    else:
                        A("dve", lambda e, t=t, bank=bank: e.tensor_copy(out=xo[t % 2][:, 512:1024], in_=pb[bank][:]),
                          r=[("pb", bank)], w=[("xo", t % 2)])
                if final_norm:
                    A("act", lambda e, t=t: e.activation(out=junk, in_=xo[t % 2], func=AF.Square,
                                                          accum_out=stat[:, 0:1]),
                      r=[("xo", t % 2)], w=["junk", "stat"])
                    A("act", lambda e: e.activation(out=stat[:, 1:2], in_=stat[:, 0:1], func=AF.Ln,
                                                    scale=1.0 / D, bias=cst[:, CE6:CE6 + 1]),
                      r=["stat", "cst"], w=["stat"])
                    A("act", lambda e: e.activation(out=stat[:, 2:3], in_=stat[:, 1:2], func=AF.Exp, scale=-0.5),
                      r=["stat"], w=["stat"])
                    A("dve", lambda e, t=t: e.scalar_tensor_tensor(out=xo[t % 2], in0=xo[t % 2], scalar=stat[:, 2:3],
                                                                   in1=fgb, op0=ALU.mult, op1=ALU.mult),
                      r=[("xo", t % 2), "stat", "fgb"], w=[("xo", t % 2)])
                A("sp", lambda e, t=t: e.dma_start(out=out_d[s, t * 128:(t + 1) * 128, :], in_=xo[t % 2]),
                  r=[("xo", t % 2)], dma=("xo", t % 2))

        def ada(l):
            P.barrier()
            jobs = Jobs([(wada_d[l], [(g * 512, 512)]) for g in range(6)])
            for g in range(6):
                slot, offs = jobs.get()
                for fc4 in range(4):
                    fc = g * 4 + fc4
                    for k in range(8):
                        A("pe", lambda e, slot=slot, fc4=fc4, fc=fc, k=k: e.matmul(
                            pb[4][:, fc * NS:(fc + 1) * NS], lhsT=wbuf[slot][:, k, fc4 * 128:(fc4 + 1) * 128],
                            rhs=cact[:, k, :], start=(k == 0), stop=(k == 7)),
                          r=[("wb", slot), "cact"], w=[("pb", 4)])
            A("dve", lambda e: e.tensor_tensor(
                out=modv[:, l, :, :], in0=pb[4][:, 0:24 * NS].rearrange("p (a b) -> p a b", b=NS),
                in1=vecs[:, V_BA:V_BA + 24].unsqueeze(2).to_broadcast([128, 24, NS]), op=ALU.add),
              r=[("pb", 4), "vecs"], w=["modv"])

        def load_vecs(l):
            P.barrier()
            A("sp", lambda e: e.dma_start(out=vecs[:], in_=vecs_d[l, :, :]), w=["vecs"], dma="vecs")
            A("sp", lambda e: e.dma_start(out=vec64[:], in_=vec64_d[l, :, :]), w=["vec64"], dma="vec64")
            A("sp", lambda e: e.dma_start(out=gb[:, 0:3], in_=gb_d[l, :, :]), w=["gb"], dma="gb")

        acc_state = {"n": 0}

        def next_acc():
            b = acc_state["n"] % 2
            acc_state["n"] += 1
            return b

        def proj_fm(slot, col0, M, evac, tts=(0, 1)):
            for tt in tts:
                bank = next_acc()
                for k in range(8):
                    A("pe", lambda e, k=k, tt=tt, bank=bank: e.matmul(
                        pb[bank][0:M, :], lhsT=wbuf[slot][:, k, col0:col0 + M], rhs=hT[:, k, tt * 512:(tt + 1) * 512],
                        start=(k == 0), stop=(k == 7)),
                      r=[("wb", slot), ("hT", tt)], w=[("pb", bank)])
                evac(bank, tt)

        def proj_tm(slot, col0, N, evac):
            for t8 in range(8):
                bank = next_acc()
                for k in range(8):
                    A("pe", lambda e, k=k, t8=t8, bank=bank: e.matmul(
                        pb[bank][:, 0:N], lhsT=hT[:, k, t8 * 128:(t8 + 1) * 128], rhs=wbuf[slot][:, k, col0:col0 + N],
                        start=(k == 0), stop=(k == 7)),
                      r=[("wb", slot), ("hT", t8 // 4)], w=[("pb", bank)])
                evac(bank, t8)

        def layer(l, s, first_layer_dbg):
            load_vecs(l)
            A("dve", lambda e: e.scalar_tensor_tensor(out=Gv[:], in0=modv[:, l, 8:16, s], scalar=1.0,
                                                      in1=vecs[:, V_NG:V_NG + 8], op0=ALU.add, op1=ALU.mult),
              r=["modv", "vecs"], w=["Gv"])
            A("dve", lambda e: e.memset(Cst[:], 0.0), w=["Cst"], r=["Cst"])
            A("dve", lambda e: e.memset(carry[:], 0.0), w=["carry"], r=["carry"])
            A("dve", lambda e: e.memset(halo[:], 0.0), w=["halo"], r=["halo"])
            A("dve", lambda e: e.memset(Vext[:, :, :, 64:65], 1.0), w=["Vext"], r=["Vext"])
            win = win_d[l]
            wout = wout_d[l]
            for half in range(2):
                t0 = half * NHALF
                joblist = [(win, [(C_MI, 8), (C_FF, 4)])]
                for hp in range(2):
                    joblist.append((win, [(C_MQ + hp * 256, 256), (C_MK + hp * 256, 256)]))
                    joblist.append((win, [(C_MZ + hp * 256, 256)]))
                    joblist.append((win, [(C_MV + hp * 256, 256), (C_MO + hp * 256, 256)]))
                joblist.append((win, [(C_FQ, 256), (C_FK, 256)]))
                joblist.append((win, [(C_FV, 256), (C_FZ, 256)]))
                joblist.append((win, [(C_CA, 256), (C_CG, 256)]))
                joblist.append((win, [(C_CZ, 256)]))
                joblist.append((wout, [(0, 512)]))
                joblist.append((wout, [(512, 512)]))
                jobs = Jobs(joblist)

                P.barrier()
                sq = carve(0, 128, [8, 512], BF16)
                tmpf = [carve(8192 + i * 2048, 128, [512], F32) for i in range(2)]
                lnv = carve(12288, 128, [512], F32)
                for tt in range(2):
                    tok = slice(t0 + tt * 512, t0 + (tt + 1) * 512)
                    for c in range(8):
                        A("act", lambda e, c=c, tok=tok: e.activation(out=sq[:, c, :], in_=xT[:, c, tok], func=AF.Square),
                          r=[("xT", (t0 + tt * 512) // 512)], w=[("sq", c)])
                        A("pe", lambda e, c=c: e.matmul(pb[4][:], lhsT=ones_b[:], rhs=sq[:, c, :],
                                                        start=(c == 0), stop=(c == 7)),
                          r=[("sq", c), "ones_b"], w=[("pb", 4)])
                    A("act", lambda e: e.activation(out=lnv, in_=pb[4][:], func=AF.Ln, scale=1.0 / D,
                                                    bias=cst[:, CE6:CE6 + 1]), r=[("pb", 4), "cst"], w=["lnv"])
                    A("act", lambda e: e.activation(out=pb[5][:], in_=lnv, func=AF.Exp, scale=-0.5),
                      r=["lnv"], w=[("pb", 5)])
                    for c in range(8):
                        A("dve", lambda e, c=c, tok=tok: e.scalar_tensor_tensor(
                            out=tmpf[c % 2], in0=xT[:, c, tok], scalar=Gv[:, c:c + 1], in1=pb[5][:],
                            op0=ALU.mult, op1=ALU.mult),
                          r=[("xT", (t0 + tt * 512) // 512), "Gv", ("pb", 5)], w=[("tmpf", c % 2)])
                        A("act", lambda e, c=c, tt=tt: e.activation(
                            out=hT[:, c, tt * 512:(tt + 1) * 512], in_=tmpf[c % 2], func=AF.Identity,
                            bias=modv[:, l, c, s:s + 1], scale=1.0),
                          r=[("tmpf", c % 2), "modv"], w=[("hT", tt)])
                if dbg and first_layer_dbg and half == 0:
                    A("sp", lambda e: e.dma_start(out=d_h[:, :, :], in_=hT[:]), r=[("hT", 0), ("hT", 1)], dma="dbg_h")

                if STOP_AFTER == "H":
                    continue
                P.barrier()
                g1 = carve(0, 4, [NHALF], F32)
                g2 = carve(4096, 4, [NHALF], F32)
                g3 = carve(8192, 4, [NHALF], F32)
                g4 = carve(12288, 4, [NHALF], F32)
                g5 = carve(16384, 4, [NHALF], F32)
                msk = carve(20480, 4, [NHALF], F32)
                dsm = carve(24576, 4, [8], F32)
                slot, offs = jobs.get()

                gname = {id(g1): "g1", id(g2): "g2", id(g3): "g3", id(g4): "g4", id(g5): "g5"}

                def gate_proj(gi, dst):
                    def ev(bank, tt):
                        A("act", lambda e, bank=bank, tt=tt: e.activation(
                            out=dst[:, tt * 512:(tt + 1) * 512], in_=pb[bank][0:4, :], func=AF.Identity,
                            bias=gb[:, gi:gi + 1], scale=1.0), r=[("pb", bank), "gb"], w=[("g", gname[id(dst)], tt)])
                    proj_fm(slot, 4 * gi, 4, ev)

                gate_proj(0, g1)
                gate_proj(1, g2)
                gate_proj(2, g4)
                gk = lambda t: [("g", gname[id(t)], 0), ("g", gname[id(t)], 1)]
                for gt in (g2, g4):
                    A("act", lambda e, gt=gt: e.activation(out=gt, in_=gt, func=AF.Exp, scale=-1.0), r=gk(gt), w=gk(gt))
                    A("act", lambda e, gt=gt: e.activation(out=gt, in_=gt, func=AF.Ln, bias=cst[0:4, C1:C1 + 1], scale=1.0),
                      r=gk(gt) + ["cst"], w=gk(gt))
                A("dve", lambda e: e.memset(msk, 1.0), w=["msk"])
                A("dve", lambda e: e.memset(msk.rearrange("p (a b) -> p a b", b=128)[:, :, 0:1], 0.0), r=["msk"], w=["msk"])
                A("dve", lambda e: e.tensor_tensor_scan(out=g3, data0=msk, data1=g2, initial=0.0,
                                                        op0=ALU.mult, op1=ALU.add), r=["msk"] + gk(g2), w=gk(g3))
                g3v = g3.rearrange("p (a b) -> p a b", b=128)
                A("act", lambda e: e.activation(out=dsm, in_=g3v[:, :, 127], func=AF.Exp, scale=-1.0), r=gk(g3), w=["dsm"])
                A("dve", lambda e: e.tensor_tensor(out=g2.rearrange("p (a b) -> p a b", b=128), in0=g3v,
                                                   in1=g3v[:, :, 127:128].to_broadcast([4, 8, 128]), op=ALU.subtract),
                  r=gk(g3), w=gk(g2))
                A("dve", lambda e: e.tensor_tensor(out=g1, in0=g1, in1=g2, op=ALU.add), r=gk(g1) + gk(g2), w=gk(g1))
                A("act", lambda e: e.activation(out=g1, in_=g1, func=AF.Exp, bias=cst[0:4, CLN:CLN + 1], scale=1.0),
                  r=gk(g1) + ["cst"], w=gk(g1))
                A("act", lambda e: e.activation(out=g2, in_=g2, func=AF.Exp), r=gk(g2), w=gk(g2))
                A("dve", lambda e: e.memset(msk, 1.0), r=["msk"] + gk(g3), w=["msk"])
                A("dve", lambda e: e.tensor_tensor_scan(out=g5, data0=msk, data1=g4, initial=carry[:, 0:1],
                                                        op0=ALU.mult, op1=ALU.add),
                  r=["msk", "carry"] + gk(g4), w=gk(g5))
                A("dve", lambda e: e.tensor_copy(out=carry[:, 0:1], in_=g5[:, NHALF - 1:NHALF]), r=gk(g5), w=["carry"])
                A("act", lambda e: e.activation(out=cumq_bf[:], in_=g5, func=AF.Identity, scale=-1.0),
                  r=gk(g5), w=["cumq_bf"])
                for t8 in range(8):
                    for qi, gt in enumerate((g1, g2)):
                        A("pe", lambda e, t8=t8, qi=qi, gt=gt: e.transpose(
                            out=pb[6][:, (t8 * 2 + qi) * 4:(t8 * 2 + qi) * 4 + 4], in_=gt[:, t8 * 128:(t8 + 1) * 128],
                            identity=ident_f[0:4, 0:4]), r=gk(gt) + ["ident_f"], w=[("pb", 6)])
                    A("pe", lambda e, t8=t8: e.transpose(
                        out=pb[7][:, t8 * 4:t8 * 4 + 4], in_=g5[:, t8 * 128:(t8 + 1) * 128],
                        identity=ident_f[0:4, 0:4]), r=gk(g5) + ["ident_f"], w=[("pb", 7)])
                A("dve", lambda e: e.tensor_copy(out=wtm[:].rearrange("p a b c -> p (a b c)"), in_=pb[6][:, 0:64]),
                  r=[("pb", 6)], w=["wtm"])
                A("dve", lambda e: e.tensor_copy(out=cumspT[:, half * 8:half * 8 + 8, :].rearrange("p a b -> p (a b)"),
                                                 in_=pb[7][:, 0:32]), r=[("pb", 7)], w=["cumspT"])
                for h in range(4):
                    A("pe", lambda e, h=h: e.matmul(pb[5][:, h * 8:(h + 1) * 8], lhsT=selF[:, h, :], rhs=dsm,
                                                    start=True, stop=True), r=["selF", "dsm"], w=[("pb", 5)])
                A("dve", lambda e: e.tensor_copy(out=dbc[:].rearrange("p a b -> p (a b)"), in_=pb[5][:, 0:32]),
                  r=[("pb", 5)], w=["dbc"])
                if dbg and first_layer_dbg and half == 0:
                    A("sp", lambda e: e.dma_start(out=d_gt[:, :], in_=wtm[:].rearrange("p a b c -> p (a b c)")),
                      r=["wtm"], dma="dbg_gt")

                if STOP_AFTER == "G":
                    continue
                for hp in range(2):
                    P.barrier()
                    qT = carve(0, 128, [2, NHALF], BF16)
                    kT = carve(4096, 128, [2, NHALF], BF16)
                    zT = carve(8192, 128, [2, NHALF], BF16)
                    vext = carve(12288, 128, [8, 2, 130], BF16)
                    osb = carve(16448, 128, [8, 256], BF16)
                    pre = [carve(20544 + i * 2064, 128, [3 + NHALF + 5], BF16) for i in range(2)]
                    dg4 = carve(24672, 128, [16, 128], BF16)
                    bbc = carve(28768, 128, [512], F32)
                    tbase = 30816
                    kp = [carve(tbase + i * 256, 128, [128], BF16) for i in range(2)]
                    spb = [carve(tbase + 512 + i * 256, 128, [128], BF16) for i in range(2)]
                    cbf = [carve(tbase + 1024 + i * 264, 128, [130], BF16) for i in range(2)]
                    hhf = [carve(tbase + 1552 + i * 512, 128, [128], F32) for i in range(2)]
                    hnb = [carve(tbase + 2576 + i * 256, 128, [128], BF16) for i in range(2)]
                    osig = carve(tbase + 3088, 128, [256], F32)
                    stt = [carve(tbase + 4112 + i * 64, 128, [16], F32) for i in range(2)]
                    for ci in range(4):
                        chunk = (0 if ci < 2 else 4) + hp * 2 + (ci % 2)
                        for j in range(4):
                            A("dve", lambda e, ci=ci, j=j, chunk=chunk: e.tensor_scalar(
                                out=dg4[:, ci * 4 + j, :], in0=ident_b[:],
                                scalar1=vecs[:, V_CW + j * 8 + chunk:V_CW + j * 8 + chunk + 1], scalar2=None,
                                op0=ALU.mult), r=["ident_b", "vecs"], w=[("dg4", ci)])
                    A("sp", lambda e, hp=hp: e.dma_start(out=bbc[:, 0:256],
                                                         in_=bin_d[l, :, C_MV + hp * 256:C_MV + hp * 256 + 256].partition_broadcast(128)),
                      w=["bbc"], r=["bbc"], dma="bbc")
                    A("sp", lambda e, hp=hp: e.dma_start(out=bbc[:, 256:512],
                                                         in_=bin_d[l, :, C_MO + hp * 256:C_MO + hp * 256 + 256].partition_broadcast(128)),
                      w=["bbc"], r=["bbc"], dma="bbc")
                    A("dve", lambda e: e.memset(vext, 1.0), w=["vext"], r=["vext"])
                    slot, offs = jobs.get()
                    for ci in range(4):
                        chunk = (0 if ci < 2 else 4) + hp * 2 + (ci % 2)
                        pr = pre[ci % 2]
                        dstT = qT if ci < 2 else kT
                        bcol = (V_BQ if ci < 2 else V_BK) + hp * 2 + (ci % 2)
                        A("act", lambda e, pr=pr, chunk=chunk: e.copy(out=pr[:, 0:3], in_=halo[:, chunk, 0:3]),
                          r=["halo"], w=[("pre", ci % 2, 0)])

                        def ev(bank, tt, pr=pr, bcol=bcol, ci=ci):
                            A("act", lambda e: e.activation(out=pr[:, 3 + tt * 512:3 + (tt + 1) * 512], in_=pb[bank][:],
                                                            func=AF.Identity, bias=vecs[:, bcol:bcol + 1], scale=1.0),
                              r=[("pb", bank), "vecs"], w=[("pre", ci % 2, tt)])
                        proj_fm(slot, ci * 128, 128, ev)
                        A("act", lambda e, pr=pr, chunk=chunk: e.copy(out=halo[:, chunk, 0:3], in_=pr[:, NHALF:NHALF + 3]),
                          r=[("pre", ci % 2, 1)], w=["halo"])
                        for tt in range(2):
                            bank = 2 + (tt % 2)
                            for j in range(4):
                                A("pe", lambda e, j=j, tt=tt, bank=bank, pr=pr, ci=ci: e.matmul(
                                    pb[bank][:], lhsT=dg4[:, ci * 4 + j, :], rhs=pr[:, tt * 512 + j:tt * 512 + j + 512],
                                    start=(j == 0), stop=(j == 3)),
                                  r=[("dg4", ci), ("pre", ci % 2, 0), ("pre", ci % 2, 1)], w=[("pb", bank)])
                            A("act", lambda e, tt=tt, bank=bank, dstT=dstT, ci=ci, chunk=chunk: e.activation(
                                out=dstT[:, ci % 2, tt * 512:(tt + 1) * 512], in_=pb[bank][:], func=AF.Silu,
                                bias=vecs[:, V_CB + chunk:V_CB + chunk + 1], scale=1.0),
                              r=[("pb", bank), "vecs"], w=[("qk", ci, tt)])
                    if dbg and first_layer_dbg and half == 0 and hp == 0:
                        A("sp", lambda e: e.dma_start(out=d_q[:, :, :], in_=qT), r=[("qk", 0, 0), ("qk", 0, 1), ("qk", 1, 0), ("qk", 1, 1)], dma="dbg_q")
                    slot, offs = jobs.get()
                    for i in range(2):
                        def ev(bank, tt, i=i):
                            A("act", lambda e: e.activation(out=zT[:, i, tt * 512:(tt + 1) * 512], in_=pb[bank][:],
                                                            func=AF.Silu, bias=vecs[:, V_BZ + hp * 2 + i:V_BZ + hp * 2 + i + 1],
                                                            scale=1.0), r=[("pb", bank), "vecs"], w=[("zT", i, tt)])
                        proj_fm(slot, i * 128, 128, ev)
                    slot, offs = jobs.get()

                    def ev_vo(bank, t8):
                        A("dve", lambda e: e.tensor_tensor(
                            out=vext[:, t8, :, 0:128], in0=pb[bank][:, 0:256].rearrange("p (a b) -> p a b", b=128),
                            in1=bbc[:, 0:256].rearrange("p (a b) -> p a b", b=128), op=ALU.add),
                          r=[("pb", bank), "bbc", "vext"], w=[("vext", t8)])
                        A("dve", lambda e: e.tensor_tensor(out=osig, in0=pb[bank][:, 256:512], in1=bbc[:, 256:512], op=ALU.add),
                          r=[("pb", bank), "bbc"], w=["osig"])
                        A("act", lambda e: e.activation(out=osb[:, t8, :], in_=osig, func=AF.Sigmoid),
                          r=["osig"], w=[("osb", t8)])
                    proj_tm(slot, 0, 512, ev_vo)
                    P.barrier()
                    it = 0
                    for hh in range(2):
                        h = hp * 2 + hh
                        for c8 in range(8):
                            sset = it % 2
                            it += 1
                            bT, bSt, bN, bC, bT2 = sset, 2 + sset, 4 + sset, 6, 7
                            tok = slice(c8 * 128, (c8 + 1) * 128)
                            wcol = wtm[:, c8, 0, h:h + 1]
                            fcol = wtm[:, c8, 1, h:h + 1]
                            dcol = dbc[:, h, c8:c8 + 1]
                            qk_r = [("qk", hh, c8 // 4), ("qk", 2 + hh, c8 // 4)]
                            tpv = pbb(bT)[:, 0:128]
                            stv = pb[bSt][:, 0:128]
                            tp2v = pbb(bT2)[:, 0:128]
                            A("pe", lambda e, hh=hh, tok=tok, tpv=tpv: e.transpose(out=tpv, in_=kT[:, hh, tok], identity=ident_b[:]),
                              r=qk_r + ["ident_b"], w=[("pb", bT)])
                            A("act", lambda e, tpv=tpv, wcol=wcol, sset=sset: e.activation(
                                out=kp[sset], in_=tpv, func=AF.Identity, scale=wcol),
                              r=[("pb", bT), "wtm"], w=[("kp", sset)])
                            A("pe", lambda e, hh=hh, tok=tok, stv=stv: e.matmul(stv, lhsT=kT[:, hh, tok], rhs=qT[:, hh, tok],
                                                                               start=True, stop=True),
                              r=qk_r, w=[("pb", bSt)])
                            A("dve", lambda e, stv=stv, wcol=wcol, sset=sset: e.scalar_tensor_tensor(
                                out=spb[sset], in0=stv, scalar=wcol, in1=maskT[:], op0=ALU.mult, op1=ALU.mult),
                              r=[("pb", bSt), "wtm", "maskT"], w=[("spb", sset)])
                            A("dve", lambda e, h=h, dcol=dcol, sset=sset: e.tensor_scalar(
                                out=cbf[sset], in0=Cst[:, h, :], scalar1=dcol, scalar2=None, op0=ALU.mult),
                              r=["Cst", "dbc"], w=[("cbf", sset)])
                            A("pe", lambda e, sset=sset, c8=c8, hh=hh, bN=bN: e.matmul(
                                pb[bN][:, 0:130], lhsT=spb[sset], rhs=vext[:, c8, hh, :], start=True, stop=False),
                              r=[("spb", sset), ("vext", c8), "vext"], w=[("pb", bN)])
                            A("pe", lambda e, sset=sset, hh=hh, tok=tok, bN=bN: e.matmul(
                                pb[bN][:, 0:130], lhsT=qT[:, hh, tok], rhs=cbf[sset], start=False, stop=True),
                              r=qk_r + [("cbf", sset)], w=[("pb", bN)])
                            A("pe", lambda e, sset=sset, c8=c8, hh=hh, bC=bC: e.matmul(
                                pb[bC][:, 0:130], lhsT=kp[sset], rhs=vext[:, c8, hh, :], start=True, stop=True),
                              r=[("kp", sset), ("vext", c8), "vext"], w=[("pb", bC)])
                            A("dve", lambda e, h=h, dcol=dcol, bC=bC: e.scalar_tensor_tensor(
                                out=Cst[:, h, :], in0=Cst[:, h, :], scalar=dcol, in1=pb[bC][:, 0:130],
                                op0=ALU.mult, op1=ALU.add), r=["Cst", "dbc", ("pb", bC)], w=["Cst"])
                            sv = stt[sset]
                            A("dve", lambda e, bN=bN, fcol=fcol, sv=sv: e.tensor_scalar(
                                out=sv[:, 12:13], in0=pb[bN][:, 128:129], scalar1=-1.0, scalar2=fcol,
                                op0=ALU.mult, op1=ALU.max), r=[("pb", bN), "wtm"], w=[("stt", sset)])
                            A("dve", lambda e, bN=bN, sv=sv: e.tensor_tensor(
                                out=sv[:, 0:1], in0=sv[:, 12:13], in1=pb[bN][:, 128:129], op=ALU.max),
                              r=[("pb", bN), ("stt", sset)], w=[("stt", sset)])
                            A("dve", lambda e, sv=sv: e.reciprocal(out=sv[:, 1:2], in_=sv[:, 0:1]),
                              r=[("stt", sset)], w=[("stt", sset)])
                            A("dve", lambda e, bN=bN, sv=sv, sset=sset, c8=c8, hh=hh: e.scalar_tensor_tensor(
                                out=hhf[sset], in0=pb[bN][:, 0:128], scalar=sv[:, 1:2], in1=osb[:, c8, hh * 128:(hh + 1) * 128],
                                op0=ALU.mult, op1=ALU.mult), r=[("pb", bN), ("stt", sset), ("osb", c8)], w=[("hhf", sset)])
                            A("dve", lambda e, sv=sv, sset=sset: e.bn_stats(out=sv[:, 2:8], in_=hhf[sset]),
                              r=[("hhf", sset)], w=[("stt", sset)])
                            A("dve", lambda e, sv=sv: e.bn_aggr(out=sv[:, 8:10], in_=sv[:, 2:8]),
                              r=[("stt", sset)], w=[("stt", sset)])
                            A("act", lambda e, sv=sv: e.activation(out=sv[:, 10:11], in_=sv[:, 9:10], func=AF.Ln,
                                                                   bias=cst[:, CE5:CE5 + 1], scale=1.0),
                              r=[("stt", sset), "cst"], w=[("stt", sset)])
                            A("act", lambda e, sv=sv: e.activation(out=sv[:, 11:12], in_=sv[:, 10:11], func=AF.Exp, scale=-0.5),
                              r=[("stt", sset)], w=[("stt", sset)])
                            A("dve", lambda e, sv=sv, sset=sset: e.tensor_scalar(
                                out=hnb[sset], in0=hhf[sset], scalar1=sv[:, 8:9], scalar2=sv[:, 11:12],
                                op0=ALU.subtract, op1=ALU.mult), r=[("hhf", sset), ("stt", sset)], w=[("hnb", sset)])
                            A("pe", lambda e, sset=sset, tp2v=tp2v: e.transpose(out=tp2v, in_=hnb[sset], identity=ident_b[:]),
                              r=[("hnb", sset), "ident_b"], w=[("pb", bT2)])
                            A("dve", lambda e, tp2v=tp2v, h=h, hh=hh, tok=tok: e.scalar_tensor_tensor(
                                out=yTa[:, h, tok], in0=tp2v, scalar=vecs[:, V_HG + h:V_HG + h + 1], in1=zT[:, hh, tok],
                                op0=ALU.mult, op1=ALU.mult),
                              r=[("pb", bT2), "vecs", ("zT", hh, c8 // 4)], w=[("yTa", h)])
                if dbg and first_layer_dbg and half == 0:
                    A("sp", lambda e: e.dma_start(out=d_ya[:, :, :], in_=yTa[:]), r=[("yTa", i) for i in range(4)], dma="dbg_ya")

                if STOP_AFTER == "A":
                    continue
                P.barrier()
                fqT = carve(0, 128, [2, NHALF], BF16)
                fzT = carve(8192, 64, [4, NHALF], BF16)
                ptb = [carve(16384 + i * 1024, 128, [512], BF16) for i in range(2)]
                rden = carve(18432, 128, [512], F32)
                t1 = carve(20480, 64, [512], F32)
                ytmp = carve(22528, 64, [512], BF16)
                bbf = carve(23552, 128, [256], F32)
                A("sp", lambda e: e.dma_start(out=bbf, in_=bin_d[l, :, C_FV:C_FV + 256].partition_broadcast(128)),
                  w=["bbf"], dma="bbf")
                slot, offs = jobs.get()
                for i in range(2):
                    def evq(bank, tt, i=i):
                        A("dve", lambda e: e.tensor_scalar(out=fqT[:, i, tt * 512:(tt + 1) * 512], in0=pb[bank][:],
                                                           scalar1=vecs[:, V_BFQ + i:V_BFQ + i + 1], scalar2=0.125,
                                                           op0=ALU.add, op1=ALU.mult),
                          r=[("pb", bank), "vecs"], w=[("fqT", tt)])
                    proj_fm(slot, i * 128, 128, evq)

                    def evk(bank, tt, i=i):
                        A("act", lambda e: e.activation(out=fkT[:, i, t0 + tt * 512:t0 + (tt + 1) * 512], in_=pb[bank][:],
                                                        func=AF.Identity, bias=vecs[:, V_BFK + i:V_BFK + i + 1], scale=1.0),
                          r=[("pb", bank), "vecs"], w=["fkT"])
                    proj_fm(slot, 256 + i * 128, 128, evk)
                slot, offs = jobs.get()

                def ev_fv(bank, t8):
                    A("dve", lambda e: e.tensor_tensor(
                        out=Vext[:, half * 8 + t8, :, 0:64], in0=pb[bank][:, 0:256].rearrange("p (a b) -> p a b", b=64),
                        in1=bbf.rearrange("p (a b) -> p a b", b=64), op=ALU.add),
                      r=[("pb", bank), "bbf"], w=["Vext"])
                proj_tm(slot, 0, 256, ev_fv)
                for h in range(4):
                    def evz(bank, tt, h=h):
                        A("act", lambda e: e.activation(out=fzT[:, h, tt * 512:(tt + 1) * 512], in_=pb[bank][0:64, :],
                                                        func=AF.Silu, bias=vec64[:, h:h + 1], scale=1.0),
                          r=[("pb", bank), "vec64"], w=[("fzT", tt)])
                    proj_fm(slot, 256 + h * 64, 64, evz)
                P.barrier()
                nS = 0
                nO = 0
                for h in range(4):
                    kc, pbase = h // 2, (h % 2) * 64
                    for Q in range(2):
                        q0t = (t0 + Q * 512) // 128
                        nkb = q0t + 4
                        bO = 2 + (nO % 2)
                        nO += 1
                        for kb in range(nkb):
                            j = kb - q0t
                            nc0 = max(0, j) * 128
                            N = 512 - nc0
                            bS = nS % 2
                            pt = ptb[nS % 2]
                            nS += 1
                            qs = slice(Q * 512 + nc0, Q * 512 + 512)
                            regs = [(0, N, False)] if j < 0 else ([(0, 128, True)] + ([(128, N, False)] if N > 128 else []))
                            for (ra, rb, dg) in regs:
                                qsr = slice(Q * 512 + nc0 + ra, Q * 512 + nc0 + rb)
                                A("pe", lambda e, ra=ra, rb=rb, qsr=qsr: e.matmul(
                                    pb[bS][:, ra:rb], lhsT=fkT[pbase:pbase + 64, kc, kb * 128:(kb + 1) * 128],
                                    rhs=fqT[pbase:pbase + 64, kc, qsr], start=True, stop=False),
                                  r=["fkT", ("fqT", Q)], w=[("pb", bS)])
                                A("pe", lambda e, ra=ra, rb=rb, qsr=qsr, dg=dg: e.matmul(
                                    pb[bS][:, ra:rb], lhsT=selB[:, h, :], rhs=cumq_bf[:, qsr], start=False, stop=(not dg)),
                                  r=["selB", "cumq_bf"], w=[("pb", bS)])
                                if dg:
                                    A("pe", lambda e, ra=ra, rb=rb: e.matmul(
                                        pb[bS][:, ra:rb], lhsT=ident_b[:], rhs=negmask[:], start=False, stop=True),
                                      r=["ident_b", "negmask"], w=[("pb", bS)])
                            A("act", lambda e: e.activation(
                                out=pt[:, 0:N], in_=pb[bS][:, 0:N], func=AF.Exp, bias=cumspT[:, kb, h:h + 1], scale=1.0),
                              r=[("pb", bS), "cumspT"], w=[("pt", bS)])
                            A("pe", lambda e: e.matmul(
                                pb[bO][0:65, nc0:512], lhsT=Vext[:, kb, h, :], rhs=pt[:, 0:N],
                                start=(kb == 0), stop=(kb == nkb - 1)),
                              r=["Vext", ("pt", bS)], w=[("pb", bO)])
                        A("act", lambda e: e.copy(out=rden[0:1, :], in_=pb[bO][64:65, :]),
                          r=[("pb", bO)], w=["rden"])
                        A("dve", lambda e: e.reciprocal(out=rden[0:1, :], in_=rden[0:1, :]), r=["rden"], w=["rden"])
                        A("pe", lambda e: e.matmul(pb[4][0:64, :], lhsT=onesF[0:1, 0:64], rhs=rden[0:1, :],
                                                   start=True, stop=True), r=["rden", "onesF"], w=[("pb", 4)])
                        A("dve", lambda e: e.tensor_tensor(
                            out=t1, in0=pb[bO][0:64, :], in1=fzT[:, h, Q * 512:(Q + 1) * 512], op=ALU.mult),
                          r=[("pb", bO), ("fzT", Q)], w=["t1"])
                        if pbase == 0:
                            A("dve", lambda e: e.tensor_tensor(
                                out=yTb[0:64, kc, Q * 512:(Q + 1) * 512], in0=t1, in1=pb[4][0:64, :], op=ALU.mult),
                              r=["t1", ("pb", 4)], w=[("yTb", kc)])
                        else:
                            A("dve", lambda e: e.tensor_tensor(out=ytmp, in0=t1, in1=pb[4][0:64, :], op=ALU.mult),
                              r=["t1", ("pb", 4)], w=["ytmp"])
                            A("act", lambda e: e.copy(out=yTb[64:128, kc, Q * 512:(Q + 1) * 512], in_=ytmp),
                              r=["ytmp"], w=[("yTb", kc)])
                if dbg and first_layer_dbg and half == 0:
                    A("sp", lambda e: e.dma_start(out=d_yb[:, :, :], in_=yTb[:]), r=[("yTb", 0), ("yTb", 1)], dma="dbg_yb")

                if STOP_AFTER == "B":
                    continue
                P.barrier()
                sg = [carve(i * 2048, 128, [512], F32) for i in range(2)]
                zcT = carve(4096, 128, [2, NHALF], BF16)
                dg31 = carve(8192, 128, [31, 128], BF16)
                yf = carve(16384, 128, [512], F32)
                ybf = carve(18432, 128, [512], BF16)
                ysq = carve(19456, 128, [512], BF16)
                msq = carve(20480, 128, [512], F32)
                var = carve(22528, 128, [512], F32)
                tt_ = carve(24576, 128, [512], F32)
                ss_ = carve(26624, 128, [512], F32)
                if half == 0:
                    A("dve", lambda e: e.memset(ubuf[:, :, 0:30], 0.0), w=["ubuf"], r=["ubuf"])
                else:
                    A("act", lambda e: e.copy(out=ubuf[:, :, 0:30], in_=ubuf[:, :, NHALF:NHALF + 30]), w=["ubuf"], r=["ubuf"])
                slot, offs = jobs.get()
                for c in range(2):
                    for tt in range(2):
                        bank = next_acc()
                        for k in range(8):
                            A("pe", lambda e, k=k, tt=tt, bank=bank, c=c: e.matmul(
                                pb[bank][:], lhsT=wbuf[slot][:, k, 256 + c * 128:256 + (c + 1) * 128],
                                rhs=hT[:, k, tt * 512:(tt + 1) * 512], start=(k == 0), stop=(k == 7)),
                              r=[("wb", slot), ("hT", tt)], w=[("pb", bank)])
                        A("act", lambda e, bank=bank, c=c, tt=tt: e.activation(
                            out=sg[tt % 2], in_=pb[bank][:], func=AF.Sigmoid, bias=vecs[:, V_BCG + c:V_BCG + c + 1], scale=1.0),
                          r=[("pb", bank), "vecs"], w=[("sg", tt % 2)])
                        bank = next_acc()
                        for k in range(8):
                            A("pe", lambda e, k=k, tt=tt, bank=bank, c=c: e.matmul(
                                pb[bank][:], lhsT=wbuf[slot][:, k, c * 128:(c + 1) * 128],
                                rhs=hT[:, k, tt * 512:(tt + 1) * 512], start=(k == 0), stop=(k == 7)),
                              r=[("wb", slot), ("hT", tt)], w=[("pb", bank)])
                        A("dve", lambda e, bank=bank, c=c, tt=tt: e.scalar_tensor_tensor(
                            out=ubuf[:, c, 30 + tt * 512:30 + (tt + 1) * 512], in0=pb[bank][:],
                            scalar=vecs[:, V_BCA + c:V_BCA + c + 1], in1=sg[tt % 2], op0=ALU.add, op1=ALU.mult),
                          r=[("pb", bank), "vecs", ("sg", tt % 2)], w=["ubuf"])
                slot, offs = jobs.get()
                for c in range(2):
                    def evcz(bank, tt, c=c):
                        A("act", lambda e: e.activation(out=zcT[:, c, tt * 512:(tt + 1) * 512], in_=pb[bank][:], func=AF.Silu,
                                                        bias=vecs[:, V_BCZ + c:V_BCZ + c + 1], scale=1.0),
                          r=[("pb", bank), "vecs"], w=[("zcT", c)])
                    proj_fm(slot, c * 128, 128, evcz)
                for c in range(2):
                    for j in range(31):
                        A("dve", lambda e, c=c, j=j: e.tensor_scalar(
                            out=dg31[:, j, :], in0=ident_b[:], scalar1=vecs[:, V_DW + j * 2 + c:V_DW + j * 2 + c + 1],
                            scalar2=None, op0=ALU.mult), r=["ident_b", "vecs"], w=["dg31"])
                    for tt in range(2):
                        bank = 2 + tt
                        for j in range(31):
                            A("pe", lambda e, c=c, j=j, tt=tt, bank=bank: e.matmul(
                                pb[bank][:], lhsT=dg31[:, j, :], rhs=ubuf[:, c, tt * 512 + j:tt * 512 + j + 512],
                                start=(j == 0), stop=(j == 30)), r=["dg31", "ubuf"], w=[("pb", bank)])
                        bcol = vecs[:, V_DWB + c:V_DWB + c + 1]
                        A("act", lambda e, bank=bank, bcol=bcol: e.activation(out=yf, in_=pb[bank][:], func=AF.Identity,
                                                                              bias=bcol, scale=1.0),
                          r=[("pb", bank), "vecs"], w=["yf"])
                        A("act", lambda e, bank=bank, bcol=bcol: e.activation(out=ysq, in_=pb[bank][:], func=AF.Square,
                                                                              bias=bcol, scale=1.0),
                          r=[("pb", bank), "vecs"], w=["ysq"])
                        A("dve", lambda e: e.tensor_copy(out=ybf, in_=yf), r=["yf"], w=["ybf"])
                        A("pe", lambda e: e.matmul(pb[4][:], lhsT=blk64[:], rhs=ybf, start=True, stop=True),
                          r=["blk64", "ybf"], w=[("pb", 4)])
                        A("pe", lambda e: e.matmul(pb[5][:], lhsT=blk64[:], rhs=ysq, start=True, stop=True),
                          r=["blk64", "ysq"], w=[("pb", 5)])
                        A("act", lambda e: e.activation(out=msq, in_=pb[4][:], func=AF.Square), r=[("pb", 4)], w=["msq"])
                        A("dve", lambda e: e.tensor_tensor(out=var, in0=pb[5][:], in1=msq, op=ALU.subtract),
                          r=[("pb", 5), "msq"], w=["var"])
                        A("act", lambda e: e.activation(out=var, in_=var, func=AF.Ln, bias=cst[:, CE5:CE5 + 1], scale=1.0),
                          r=["var", "cst"], w=["var"])
                        A("act", lambda e: e.activation(out=var, in_=var, func=AF.Exp, scale=-0.5), r=["var"], w=["var"])
                        A("dve", lambda e: e.tensor_tensor(out=tt_, in0=yf, in1=pb[4][:], op=ALU.subtract),
                          r=["yf", ("pb", 4)], w=["tt_"])
                        A("dve", lambda e: e.tensor_tensor(out=tt_, in0=tt_, in1=var, op=ALU.mult), r=["tt_", "var"], w=["tt_"])
                        A("act", lambda e, c=c: e.activation(out=ss_, in_=tt_, func=AF.Silu,
                                                             scale=vecs[:, V_LNG + c:V_LNG + c + 1],
                                                             bias=vecs[:, V_LNB + c:V_LNB + c + 1]),
                          r=["tt_", "vecs"], w=["ss_"])
                        A("dve", lambda e, c=c, tt=tt: e.tensor_tensor(out=yTc[:, c, tt * 512:(tt + 1) * 512], in0=ss_,
                                                                         in1=zcT[:, c, tt * 512:(tt + 1) * 512], op=ALU.mult),
                          r=["ss_", ("zcT", c)], w=[("yTc", c)])
                if dbg and first_layer_dbg and half == 0:
                    A("sp", lambda e: e.dma_start(out=d_yc[:, :, :], in_=yTc[:]), r=[("yTc", 0), ("yTc", 1)], dma="dbg_yc")

                if STOP_AFTER == "C":
                    continue
                P.barrier()
                ycat = [yTa[:, 0, :], yTa[:, 1, :], yTa[:, 2, :], yTa[:, 3, :], yTb[:, 0, :], yTb[:, 1, :],
                        yTc[:, 0, :], yTc[:, 1, :]]
                for jb in range(2):
                    slot, offs = jobs.get()
                    for d4 in range(4):
                        dc = jb * 4 + d4
                        for tt in range(2):
                            bank = next_acc()
                            for k in range(8):
                                A("pe", lambda e, k=k, tt=tt, bank=bank, d4=d4, slot=slot: e.matmul(
                                    pb[bank][:], lhsT=wbuf[slot][:, k, d4 * 128:(d4 + 1) * 128],
                                    rhs=ycat[k][:, tt * 512:(tt + 1) * 512], start=(k == 0), stop=(k == 7)),
                                  r=[("wb", slot), "ycat"], w=[("pb", bank)])
                            tok = slice(t0 + tt * 512, t0 + (tt + 1) * 512)
                            A("dve", lambda e, bank=bank, dc=dc, tok=tok: e.scalar_tensor_tensor(
                                out=xT[:, dc, tok], in0=pb[bank][:], scalar=modv[:, l, 16 + dc, s:s + 1], in1=xT[:, dc, tok],
                                op0=ALU.mult, op1=ALU.add),
                              r=[("pb", bank), "modv", ("xT", (t0 + tt * 512) // 512)], w=[("xT", (t0 + tt * 512) // 512)])

        for s in range(NS):
            load_x(s)
            for l in range(L):
                if s == 0:
                    load_vecs(l)
                    ada(l)
                layer(l, s, first_layer_dbg=(s == 0 and l == 0))
            store_x(s)
        P.finalize_and_emit()
    return nc


def _chunks(v):
    return np.ascontiguousarray(v.reshape(-1, 128).T)


def _host_vecs(inp, l):
    b_in = inp["b_in"][l]
    cols = [
        _chunks(inp["norm_g"][l]), _chunks(inp["b_ada"][l]),
        _chunks(b_in[C_MQ:C_MQ + 512]), _chunks(b_in[C_MK:C_MK + 512]), _chunks(b_in[C_MZ:C_MZ + 512]),
        _chunks(b_in[C_FQ:C_FQ + 256]), _chunks(b_in[C_FK:C_FK + 256]),
        _chunks(b_in[C_CA:C_CA + 256]), _chunks(b_in[C_CG:C_CG + 256]), _chunks(b_in[C_CZ:C_CZ + 256]),
        _chunks(inp["m_conv_b"][l]), _chunks(inp["m_hn_g"][l]),
        _chunks(inp["c_dw_b"][l]), _chunks(inp["c_ln_g"][l]), _chunks(inp["c_ln_b"][l]),
    ]
    cw = inp["m_conv_w"][l]
    cols.append(np.concatenate([_chunks(cw[j]) for j in range(4)], axis=1))
    dw = inp["c_dw_w"][l]
    cols.append(np.concatenate([_chunks(dw[j]) for j in range(31)], axis=1))
    v = np.concatenate(cols, axis=1).astype(np.float32)
    assert v.shape == (128, NV), v.shape
    vec64 = np.ascontiguousarray(np.concatenate([b_in[C_FZ:C_FZ + 256].reshape(4, 64).T, b_in[C_FQ:C_FQ + 256].reshape(4, 64).T,
                                                 b_in[C_FK:C_FK + 256].reshape(4, 64).T], axis=1)).astype(np.float32)
    gbv = np.stack([b_in[C_MI:C_MI + 4], b_in[C_MF:C_MF + 4], b_in[C_FF:C_FF + 4]], axis=1).astype(np.float32)
    return v, vec64, gbv


_NC_CACHE = {}


def _get_nc(L, NS, final_norm, dbg=False):
    key = (L, NS, final_norm, dbg)
    if key not in _NC_CACHE:
        _NC_CACHE[key] = build(L, NS, final_norm, dbg)
    return _NC_CACHE[key]


FUSED = True


def kernel(x, c, norm_g, w_ada, b_ada, w_in, b_in, m_conv_w, m_conv_b, m_hn_g,
           c_dw_w, c_dw_b, c_ln_g, c_ln_b, w_out, final_g):
    inp = dict(norm_g=np.asarray(norm_g), b_ada=np.asarray(b_ada), b_in=np.asarray(b_in),
               m_conv_w=np.asarray(m_conv_w), m_conv_b=np.asarray(m_conv_b), m_hn_g=np.asarray(m_hn_g),
               c_dw_w=np.asarray(c_dw_w), c_dw_b=np.asarray(c_dw_b), c_ln_g=np.asarray(c_ln_g),
               c_ln_b=np.asarray(c_ln_b))
    x = np.asarray(x, dtype=np.float32)
    c = np.asarray(c, dtype=np.float32)
    w_ada = np.asarray(w_ada, dtype=np.float32)
    w_in = np.asarray(w_in, dtype=np.float32)
    w_out = np.asarray(w_out, dtype=np.float32)
    b_in_a = np.asarray(b_in, dtype=np.float32)
    fg = np.asarray(final_g, dtype=np.float32).reshape(1, D)
    DEPTH = w_in.shape[0]
    hv = [_host_vecs(inp, l) for l in range(DEPTH)]
    n = 8
    if FUSED:
        nc = _get_nc(DEPTH, 2, True)
        in_maps = []
        for i in range(n):
            cs = c[2 * i:2 * i + 2]
            cT = np.ascontiguousarray(cs.reshape(2, 8, 128).transpose(2, 1, 0))
            in_maps.append({
                "x": np.ascontiguousarray(x[2 * i:2 * i + 2]), "cT": cT,
                "vecs": np.stack([h[0] for h in hv]), "vec64": np.stack([h[1] for h in hv]),
                "gb": np.stack([h[2] for h in hv]),
                "w_ada": w_ada, "w_in": w_in, "b_in": np.ascontiguousarray(b_in_a.reshape(DEPTH, 1, DIN)),
                "w_out": w_out, "final_g": fg})
        res = run_bass_kernel_spmd(nc, in_maps, core_ids=list(range(n)))
        return np.concatenate([r["out"] for r in res.results], axis=0)
    cur = x.copy()
    for l in range(DEPTH):
        nc = _get_nc(1, 1, l == DEPTH - 1)
        for sidx in range(2):
            in_maps = []
            for i in range(n):
                b = 2 * i + sidx
                cT = np.ascontiguousarray(c[b:b + 1].reshape(1, 8, 128).transpose(2, 1, 0))
                in_maps.append({
                    "x": np.ascontiguousarray(cur[b:b + 1]), "cT": cT,
                    "vecs": hv[l][0][None], "vec64": hv[l][1][None], "gb": hv[l][2][None],
                    "w_ada": w_ada[l:l + 1], "w_in": w_in[l:l + 1],
                    "b_in": np.ascontiguousarray(b_in_a[l].reshape(1, 1, DIN)),
                    "w_out": w_out[l:l + 1], "final_g": fg})
            res = run_bass_kernel_spmd(nc, in_maps, core_ids=list(range(n)))
            for i in range(n):
                cur[2 * i + sidx] = res.results[i]["out"][0]
    return cur
```

```python
import math
import numpy as np
import concourse.bass as bass
import concourse.mybir as mybir
from concourse.bass_utils import run_bass_kernel_spmd
from contextlib import ExitStack

AF = mybir.ActivationFunctionType
ALU = mybir.AluOpType
F32 = mybir.dt.float32
BF16 = mybir.dt.bfloat16

ENGS = ["pe", "act", "dve", "pool", "sp"]
SEM_EPOCH = 30000
SAME_ENG_SYNC = True
STOP_AFTER = None

S = 2048
D = 1024
NHALF = 1024
DIN = 4364
C_MQ, C_MK, C_MV, C_MO, C_MZ, C_MI, C_MF, C_FQ, C_FK, C_FV, C_FZ, C_FF, C_CA, C_CG, C_CZ = (
    0, 512, 1024, 1536, 2048, 2560, 2564, 2568, 2824, 3080, 3336, 3592, 3596, 3852, 4108)
V_NG, V_BA, V_BQ, V_BK, V_BZ, V_BFQ, V_BFK, V_BCA, V_BCG, V_BCZ, V_CB, V_HG, V_DWB, V_LNG, V_LNB, V_CW, V_DW, NV = (
    0, 8, 32, 36, 40, 44, 46, 48, 50, 52, 54, 62, 66, 68, 70, 72, 104, 166)


class Op:
    __slots__ = ("eng", "idx", "fn", "dma", "deps", "signal", "sig", "dcount", "waits")

    def __init__(self, eng, idx, fn, dma):
        self.eng = eng
        self.idx = idx
        self.fn = fn
        self.dma = dma
        self.deps = None
        self.signal = False
        self.sig = None
        self.dcount = None
        self.waits = []


class _Call:
    __slots__ = ("name", "a", "kw")

    def __init__(self, name, a, kw):
        self.name = name
        self.a = a
        self.kw = kw

    def __call__(self, e):
        return getattr(e, self.name)(*self.a, **self.kw)


class _Rec:
    def __getattr__(self, name):
        return lambda *a, **kw: _Call(name, a, kw)


_REC = _Rec()


class Prog:
    def __init__(self, nc, stack):
        self.nc = nc
        self.stack = stack
        self.ops = {e: [] for e in ENGS}
        self.reg = {}
        self.dma_cnt = {}
        self.dma_sem = {}
        self.pend = {e: None for e in ENGS}

    def barrier(self):
        snap_c = {e: len(self.ops[e]) - 1 for e in ENGS if e != "sp" and len(self.ops[e]) > 0}
        snap_c = {e: i for e, i in snap_c.items()}
        snap_d = {k: c for k, c in self.dma_cnt.items() if not (isinstance(k, tuple) and k[0] == "wb")}
        for e in ENGS:
            self.pend[e] = (dict(snap_c), dict(snap_d))

    def op(self, eng, fn, r=(), w=(), dma=None):
        call = fn(_REC)
        assert isinstance(call, _Call), "op lambda must return e.<instr>(...)"
        fn = call
        o = Op(eng, len(self.ops[eng]), fn, dma)
        deps = []
        for k in r:
            st = self.reg.get(k)
            if st is not None and st[0] is not None:
                deps.append(st[0])
        for k in w:
            st = self.reg.get(k)
            if st is not None:
                if st[0] is not None:
                    deps.append(st[0])
                deps.extend(st[1])
        cdeps = {}
        ddeps = {}
        for d in deps:
            if d.dma is not None:
                ddeps[d.dma] = self.dma_cnt[d.dma]
            else:
                if d.eng == eng and (eng == "pe" or not SAME_ENG_SYNC):
                    continue
                if cdeps.get(d.eng, -1) < d.idx:
                    cdeps[d.eng] = d.idx
        pb = self.pend[eng]
        if pb is not None:
            self.pend[eng] = None
            for e2, i2 in pb[0].items():
                if e2 == eng:
                    continue
                tgt = self.ops[e2][i2]
                j = i2
                while j >= 0 and self.ops[e2][j].dma is not None:
                    j -= 1
                if j >= 0 and cdeps.get(e2, -1) < j:
                    cdeps[e2] = j
            for k, c in pb[1].items():
                if ddeps.get(k, 0) < c:
                    ddeps[k] = c
        o.deps = (cdeps, ddeps)
        for k in r:
            st = self.reg.get(k)
            if st is None:
                st = [None, []]
                self.reg[k] = st
            st[1].append(o)
        for k in w:
            self.reg[k] = [o, []]
        if dma is not None:
            self.dma_cnt[dma] = self.dma_cnt.get(dma, 0) + 16
            o.dcount = self.dma_cnt[dma]
        self.ops[eng].append(o)
        return o

    def finalize_and_emit(self):
        nc = self.nc
        for e in ENGS:
            for o in self.ops[e]:
                for de, di in o.deps[0].items():
                    self.ops[de][di].signal = True
        nsig = {}
        for e in ENGS:
            c = 0
            for o in self.ops[e]:
                if o.signal and o.dma is None:
                    o.sig = c
                    c += 1
            nsig[e] = c
        sems = {}
        for e in ENGS:
            n = (nsig[e] + SEM_EPOCH - 1) // SEM_EPOCH
            sems[e] = [self.stack.enter_context(nc.semaphore(f"s_{e}_{i}")) for i in range(n)]
        for k in self.dma_cnt:
            self.dma_sem[k] = self.stack.enter_context(nc.semaphore(f"d_{len(self.dma_sem)}"))
        for e in ENGS:
            seen = {}
            dseen = {}
            for o in self.ops[e]:
                for de, di in o.deps[0].items():
                    s = self.ops[de][di].sig
                    if seen.get(de, -1) >= s:
                        continue
                    seen[de] = s
                    o.waits.append((sems[de][s // SEM_EPOCH], s % SEM_EPOCH + 1))
                for k, cnt in o.deps[1].items():
                    if dseen.get(k, 0) >= cnt:
                        continue
                    dseen[k] = cnt
                    o.waits.append((self.dma_sem[k], cnt))
        blk = self.stack.enter_context(nc.Block())
        prog = self

        def emit(e, name):
            for o in prog.ops[name]:
                for (s, v) in o.waits:
                    e.wait_ge(s, v)
                ins = o.fn(e)
                if o.dma is not None:
                    ins.then_inc(prog.dma_sem[o.dma], 16)
                elif o.signal:
                    ins.then_inc(sems[name][o.sig // SEM_EPOCH], 1)
            for k, cnt in prog.dma_cnt.items():
                if any(o.dma == k for o in prog.ops[name]):
                    e.wait_ge(prog.dma_sem[k], cnt)

        @blk.tensor
        def _(e):
            emit(e, "pe")

        @blk.scalar
        def _(e):
            emit(e, "act")

        @blk.vector
        def _(e):
            emit(e, "dve")

        @blk.gpsimd
        def _(e):
            emit(e, "pool")

        @blk.sync
        def _(e):
            emit(e, "sp")


def build(L, NS, final_norm, dbg=False):
    nc = bass.Bass("TRN2", target_bir_lowering=False)

    def din(n, s, d=F32):
        return nc.dram_tensor(n, s, d, kind="ExternalInput").ap()

    x_d = din("x", [NS, S, D])
    cT_d = din("cT", [128, 8, NS])
    vecs_d = din("vecs", [L, 128, NV])
    vec64_d = din("vec64", [L, 64, 12])
    gb_d = din("gb", [L, 4, 3])
    wada_d = din("w_ada", [L, D, 3 * D])
    win_d = din("w_in", [L, D, DIN])
    bin_d = din("b_in", [L, 1, DIN])
    wout_d = din("w_out", [L, D, D])
    fg_d = din("final_g", [1, D])
    out_d = nc.dram_tensor("out", [NS, S, D], F32, kind="ExternalOutput").ap()
    cq_scr = nc.dram_tensor("cq_scr", [4, NHALF], BF16).ap()
    if dbg:
        d_h = nc.dram_tensor("d_h", [128, 8, NHALF], BF16, kind="ExternalOutput").ap()
        d_ya = nc.dram_tensor("d_ya", [128, 4, NHALF], BF16, kind="ExternalOutput").ap()
        d_yb = nc.dram_tensor("d_yb", [128, 2, NHALF], BF16, kind="ExternalOutput").ap()
        d_yc = nc.dram_tensor("d_yc", [128, 2, NHALF], BF16, kind="ExternalOutput").ap()
        d_q = nc.dram_tensor("d_q", [128, 2, NHALF], BF16, kind="ExternalOutput").ap()
        d_gt = nc.dram_tensor("d_gt", [128, 64], F32, kind="ExternalOutput").ap()
        d_O = nc.dram_tensor("d_O", [128, 512], F32, kind="ExternalOutput").ap()
        d_pt = nc.dram_tensor("d_pt", [128, 512], BF16, kind="ExternalOutput").ap()
        d_fq = nc.dram_tensor("d_fq", [128, 4, NHALF], BF16, kind="ExternalOutput").ap()
        d_fk = nc.dram_tensor("d_fk", [128, 4, S], BF16, kind="ExternalOutput").ap()
        d_fz = nc.dram_tensor("d_fz", [64, 4, NHALF], BF16, kind="ExternalOutput").ap()
        d_V = nc.dram_tensor("d_V", [128, 16 * 4 * 65], BF16, kind="ExternalOutput").ap()
        d_cs = nc.dram_tensor("d_cs", [128, 64], F32, kind="ExternalOutput").ap()

    with ExitStack() as st:
        P = Prog(nc, st)
        A = P.op

        def sb(n, s, d):
            return st.enter_context(nc.sbuf_tensor(n, s, d))

        xT = sb("xT", [128, 8, S], F32)
        hT = sb("hT", [128, 8, NHALF], BF16)
        yTa = sb("yTa", [128, 4, NHALF], BF16)
        yTb = sb("yTb", [128, 2, NHALF], BF16)
        yTc = sb("yTc", [128, 2, NHALF], BF16)
        wbuf = [sb(f"wbuf{i}", [128, 8, 512], BF16) for i in range(2)]
        fkT = sb("fkT", [128, 2, S], BF16)
        Vext = sb("Vext", [128, 16, 4, 65], BF16)
        ubuf = sb("ubuf", [128, 2, 30 + NHALF], BF16)
        Cst = sb("Cst", [128, 4, 130], F32)
        cumspT = sb("cumspT", [128, 16, 4], F32)
        cumq_bf = sb("cumq_bf", [4, NHALF], BF16)
        carry = sb("carry", [4, 2], F32)
        wtm = sb("wtm", [128, 8, 2, 4], F32)
        dbc = sb("dbc", [128, 4, 8], F32)
        halo = sb("halo", [128, 8, 4], BF16)
        ident_f = sb("ident_f", [128, 128], F32)
        ident_b = sb("ident_b", [128, 128], BF16)
        ones_b = sb("ones_b", [128, 128], BF16)
        maskT = sb("maskT", [128, 128], BF16)
        blk64 = sb("blk64", [128, 128], BF16)
        negmask = sb("negmask", [128, 128], BF16)
        selF = sb("selF", [4, 4, 128], F32)
        selB = sb("selB", [4, 4, 128], BF16)
        onesF = sb("onesF", [128, 64], F32)
        cst = sb("cst", [128, 8], F32)
        vecs = sb("vecs_sb", [128, NV], F32)
        vec64 = sb("vec64_sb", [64, 12], F32)
        gb = sb("gb_sb", [4, 4], F32)
        cact = sb("cact", [128, 8, NS], BF16)
        cin = sb("cin", [128, 8, NS], F32)
        modv = sb("modv", [128, L, 24, NS], F32)
        Gv = sb("Gv", [128, 8], F32)
        SCRB = 36 * 1024
        scr = sb("scr", [128, SCRB // 2], BF16)
        pb = [st.enter_context(nc.psum_tensor(f"pb{i}", [128, 512], F32)) for i in range(8)]

        def carve(off, npart, shape, dt):
            n = 1
            for v in shape:
                n *= v
            nb = n * (4 if dt == F32 else 2)
            assert off % 4 == 0 and off + nb <= SCRB, (off, nb)
            ap = scr[0:npart, off // 2:(off + nb) // 2]
            if dt == F32:
                ap = ap.bitcast(F32)
            if len(shape) == 2:
                ap = ap.rearrange("p (a b) -> p a b", b=shape[1])
            elif len(shape) == 3:
                ap = ap.rearrange("p (a b c) -> p a b c", b=shape[1], c=shape[2])
            return ap

        def pbb(i):
            return pb[i][:].bitcast(BF16)

        CE6, CE5, C1, CLN, C0 = 0, 1, 2, 3, 4
        for i, v in enumerate([1e-6, 1e-5, 1.0, -0.5 * math.log(128.0), 0.0]):
            A("dve", lambda e, i=i, v=v: e.memset(cst[:, i:i + 1], v), w=["cst"], r=["cst"])
        A("pool", lambda e: e.memset(ident_f[:], 0.0), w=["ident_f"])
        A("pool", lambda e: e.affine_select(out=ident_f[:], in_=ident_f[:], pattern=[[-1, 128]],
                                            compare_op=ALU.not_equal, fill=1.0, base=0, channel_multiplier=1),
          r=["ident_f"], w=["ident_f"])
        A("dve", lambda e: e.tensor_copy(out=ident_b[:], in_=ident_f[:]), r=["ident_f"], w=["ident_b"])
        A("dve", lambda e: e.memset(ones_b[:], 1.0), w=["ones_b"])
        A("pool", lambda e: e.memset(maskT[:], 1.0), w=["maskT"])
        A("pool", lambda e: e.affine_select(out=maskT[:], in_=maskT[:], pattern=[[1, 128]],
                                            compare_op=ALU.is_ge, fill=0.0, base=0, channel_multiplier=-1),
          r=["maskT"], w=["maskT"])
        A("pool", lambda e: e.memset(negmask[:], 0.0), w=["negmask"])
        A("pool", lambda e: e.affine_select(out=negmask[:], in_=negmask[:], pattern=[[1, 128]],
                                            compare_op=ALU.is_ge, fill=-30000.0, base=0, channel_multiplier=-1),
          r=["negmask"], w=["negmask"])
        A("dve", lambda e: e.memset(blk64[:], 0.0), w=["blk64"])
        A("dve", lambda e: e.memset(blk64[0:64, 0:64], 1.0 / 64), r=["blk64"], w=["blk64"])
        A("dve", lambda e: e.memset(blk64[64:128, 64:128], 1.0 / 64), r=["blk64"], w=["blk64"])
        A("pool", lambda e: e.memset(selF[:], 0.0), w=["selF"])
        A("pool", lambda e: e.affine_select(out=selF[:], in_=selF[:], pattern=[[1, 4], [0, 128]],
                                            compare_op=ALU.not_equal, fill=1.0, base=0, channel_multiplier=-1),
          r=["selF"], w=["selF"])
        A("dve", lambda e: e.tensor_copy(out=selB[:], in_=selF[:]), r=["selF"], w=["selB"])
        A("dve", lambda e: e.memset(onesF[:], 1.0), w=["onesF"])
        A("dve", lambda e: e.memset(halo[:], 0.0), w=["halo"])
        A("sp", lambda e: e.dma_start(out=cin[:], in_=cT_d[:, :, :]), w=["cin"], dma="cin")
        A("act", lambda e: e.activation(out=cact[:], in_=cin[:], func=AF.Silu), r=["cin"], w=["cact"])

        wstate = {"n": 0}

        def wload(src, segs):
            slot = wstate["n"] % 2
            wstate["n"] += 1
            offs = []
            o = 0
            for (c0, ncol) in segs:
                A("pool", lambda e, o=o, c0=c0, ncol=ncol, slot=slot: e.dma_start(
                    out=wbuf[slot][:, :, o:o + ncol],
                    in_=src[:, c0:c0 + ncol].rearrange("(k p) n -> p k n", p=128)),
                  w=[("wb", slot)], r=[("wb", slot)], dma=("wb", slot))
                offs.append(o)
                o += ncol
            return slot, offs

        class Jobs:
            def __init__(self, lst):
                self.lst = lst
                self.i = 0
                self.loaded = None

            def get(self):
                if self.loaded is None:
                    self.loaded = wload(*self.lst[self.i])
                cur = self.loaded
                self.i += 1
                self.loaded = wload(*self.lst[self.i]) if self.i < len(self.lst) else None
                return cur

        def load_x(s):
            P.barrier()
            xin = [carve(i * 4096, 128, [D], F32) for i in range(2)]
            for t in range(16):
                A("sp", lambda e, t=t: e.dma_start(out=xin[t % 2], in_=x_d[s, t * 128:(t + 1) * 128, :]),
                  w=[("xin", t % 2)], dma=("xin", t % 2))
                for hb in range(2):
                    bank = 2 * (t % 2) + hb
                    for c4 in range(4):
                        c = hb * 4 + c4
                        A("pe", lambda e, t=t, c=c, c4=c4, bank=bank: e.transpose(
                            out=pb[bank][:, c4 * 128:(c4 + 1) * 128], in_=xin[t % 2][:, c * 128:(c + 1) * 128],
                            identity=ident_f[:]), r=[("xin", t % 2), "ident_f"], w=[("pb", bank)])
                    eng = "act" if hb == 0 else "dve"
                    if eng == "act":
                        A("act", lambda e, t=t, hb=hb, bank=bank: e.copy(
                            out=xT[:, hb * 4:hb * 4 + 4, t * 128:(t + 1) * 128],
                            in_=pb[bank][:].rearrange("p (a b) -> p a b", b=128)),
                          r=[("pb", bank)], w=[("xT", t // 4)])
                    else:
                        A("dve", lambda e, t=t, hb=hb, bank=bank: e.tensor_copy(
                            out=xT[:, hb * 4:hb * 4 + 4, t * 128:(t + 1) * 128],
                            in_=pb[bank][:].rearrange("p (a b) -> p a b", b=128)),
                          r=[("pb", bank)], w=[("xT", t // 4)])

        def store_x(s):
            P.barrier()
            xo = [carve(i * 4096, 128, [D], F32) for i in range(2)]
            fgb = carve(8192, 128, [D], F32)
            junk = carve(12288, 128, [D], F32)
            stat = carve(16384, 128, [8], F32)
            if final_norm:
                A("sp", lambda e: e.dma_start(out=fgb, in_=fg_d.partition_broadcast(128)), w=["fgb"], dma="fgb")
            for t in range(16):
                for hb in range(2):
                    bank = 2 * (t % 2) + hb
                    for c4 in range(4):
                        c = hb * 4 + c4
                        A("pe", lambda e, t=t, c=c, c4=c4, bank=bank: e.transpose(
                            out=pb[bank][:, c4 * 128:(c4 + 1) * 128], in_=xT[:, c, t * 128:(t + 1) * 128],
                            identity=ident_f[:]), r=[("xT", t // 4), "ident_f"], w=[("pb", bank)])
                    if hb == 0:
                        A("act", lambda e, t=t, bank=bank: e.copy(out=xo[t % 2][:, 0:512], in_=pb[bank][:]),
                          r=[("pb", bank)], w=[("xo", t % 2)])
                    else:
                        A("dve", lambda e, t=t, bank=bank: e.tensor_copy(out=xo[t % 2][:, 512:1024], in_=pb[bank][:]),
                          r=[("pb", bank)], w=[("xo", t % 2)])
                if final_norm:
                    A("act", lambda e, t=t: e.activation(out=junk, in_=xo[t % 2], func=AF.Square,
                                                          accum_out=stat[:, 0:1]),
                      r=[("xo", t % 2)], w=["junk", "stat"])
                    A("act", lambda e: e.activation(out=stat[:, 1:2], in_=stat[:, 0:1], func=AF.Ln,
                                                    scale=1.0 / D, bias=cst[:, CE6:CE6 + 1]),
                      r=["stat", "cst"], w=["stat"])
                    A("act", lambda e: e.activation(out=stat[:, 2:3], in_=stat[:, 1:2], func=AF.Exp, scale=-0.5),
                      r=["stat"], w=["stat"])
                    A("dve", lambda e, t=t: e.scalar_tensor_tensor(out=xo[t % 2], in0=xo[t % 2], scalar=stat[:, 2:3],
                                                                   in1=fgb, op0=ALU.mult, op1=ALU.mult),
                      r=[("xo", t % 2), "stat", "fgb"], w=[("xo", t % 2)])
                A("sp", lambda e, t=t: e.dma_start(out=out_d[s, t * 128:(t + 1) * 128, :], in_=xo[t % 2]),
                  r=[("xo", t % 2)], dma=("xo", t % 2))

        def ada(l):
            P.barrier()
            jobs = Jobs([(wada_d[l], [(g * 512, 512)]) for g in range(6)])
            for g in range(6):
                slot, offs = jobs.get()
                for fc4 in range(4):
                    fc = g * 4 + fc4
                    for k in range(8):
                        A("pe", lambda e, slot=slot, fc4=fc4, fc=fc, k=k: e.matmul(
                            pb[4][:, fc * NS:(fc + 1) * NS], lhsT=wbuf[slot][:, k, fc4 * 128:(fc4 + 1) * 128],
                            rhs=cact[:, k, :], start=(k == 0), stop=(k == 7)),
                          r=[("wb", slot), "cact"], w=[("pb", 4)])
            A("dve", lambda e: e.tensor_tensor(
                out=modv[:, l, :, :], in0=pb[4][:, 0:24 * NS].rearrange("p (a b) -> p a b", b=NS),
                in1=vecs[:, V_BA:V_BA + 24].unsqueeze(2).to_broadcast([128, 24, NS]), op=ALU.add),
              r=[("pb", 4), "vecs"], w=["modv"])

        def load_vecs(l):
            P.barrier()
            A("sp", lambda e: e.dma_start(out=vecs[:], in_=vecs_d[l, :, :]), w=["vecs"], dma="vecs")
            A("sp", lambda e: e.dma_start(out=vec64[:], in_=vec64_d[l, :, :]), w=["vec64"], dma="vec64")
            A("sp", lambda e: e.dma_start(out=gb[:, 0:3], in_=gb_d[l, :, :]), w=["gb"], dma="gb")

        acc_state = {"n": 0}

        def next_acc():
            b = acc_state["n"] % 2
            acc_state["n"] += 1
            return b

        def proj_fm(slot, col0, M, evac, tts=(0, 1)):
            for tt in tts:
                bank = next_acc()
                for k in range(8):
                    A("pe", lambda e, k=k, tt=tt, bank=bank: e.matmul(
                        pb[bank][0:M, :], lhsT=wbuf[slot][:, k, col0:col0 + M], rhs=hT[:, k, tt * 512:(tt + 1) * 512],
                        start=(k == 0), stop=(k == 7)),
                      r=[("wb", slot), ("hT", tt)], w=[("pb", bank)])
                evac(bank, tt)

        def proj_tm(slot, col0, N, evac):
            for t8 in range(8):
                bank = next_acc()
                for k in range(8):
                    A("pe", lambda e, k=k, t8=t8, bank=bank: e.matmul(
                        pb[bank][:, 0:N], lhsT=hT[:, k, t8 * 128:(t8 + 1) * 128], rhs=wbuf[slot][:, k, col0:col0 + N],
                        start=(k == 0), stop=(k == 7)),
                      r=[("wb", slot), ("hT", t8 // 4)], w=[("pb", bank)])
                evac(bank, t8)

        def layer(l, s, first_layer_dbg):
            load_vecs(l)
            A("dve", lambda e: e.scalar_tensor_tensor(out=Gv[:], in0=modv[:, l, 8:16, s], scalar=1.0,
                                                      in1=vecs[:, V_NG:V_NG + 8], op0=ALU.add, op1=ALU.mult),
              r=["modv", "vecs"], w=["Gv"])
            A("dve", lambda e: e.memset(Cst[:], 0.0), w=["Cst"], r=["Cst"])
            A("dve", lambda e: e.memset(carry[:], 0.0), w=["carry"], r=["carry"])
            A("dve", lambda e: e.memset(halo[:], 0.0), w=["halo"], r=["halo"])
            A("dve", lambda e: e.memset(Vext[:, :, :, 64:65], 1.0), w=["Vext"], r=["Vext"])
            win = win_d[l]
            wout = wout_d[l]
            for half in range(2):
                t0 = half * NHALF
                joblist = [(win, [(C_MI, 8), (C_FF, 4)])]
                for hp in range(2):
                    joblist.append((win, [(C_MQ + hp * 256, 256), (C_MK + hp * 256, 256)]))
                    joblist.append((win, [(C_MZ + hp * 256, 256)]))
                    joblist.append((win, [(C_MV + hp * 256, 256), (C_MO + hp * 256, 256)]))
                joblist.append((win, [(C_FQ, 256), (C_FK, 256)]))
                joblist.append((win, [(C_FV, 256), (C_FZ, 256)]))
                joblist.append((win, [(C_CA, 256), (C_CG, 256)]))
                joblist.append((win, [(C_CZ, 256)]))
                joblist.append((wout, [(0, 512)]))
                joblist.append((wout, [(512, 512)]))
                jobs = Jobs(joblist)

                P.barrier()
                sq = carve(0, 128, [8, 512], BF16)
                tmpf = [carve(8192 + i * 2048, 128, [512], F32) for i in range(2)]
                lnv = carve(12288, 128, [512], F32)
                for tt in range(2):
                    tok = slice(t0 + tt * 512, t0 + (tt + 1) * 512)
                    for c in range(8):
                        A("act", lambda e, c=c, tok=tok: e.activation(out=sq[:, c, :], in_=xT[:, c, tok], func=AF.Square),
                          r=[("xT", (t0 + tt * 512) // 512)], w=[("sq", c)])
                        A("pe", lambda e, c=c: e.matmul(pb[4][:], lhsT=ones_b[:], rhs=sq[:, c, :],
                                                        start=(c == 0), stop=(c == 7)),
                          r=[("sq", c), "ones_b"], w=[("pb", 4)])
                    A("act", lambda e: e.activation(out=lnv, in_=pb[4][:], func=AF.Ln, scale=1.0 / D,
                                                    bias=cst[:, CE6:CE6 + 1]), r=[("pb", 4), "cst"], w=["lnv"])
                    A("act", lambda e: e.activation(out=pb[5][:], in_=lnv, func=AF.Exp, scale=-0.5),
                      r=["lnv"], w=[("pb", 5)])
                    for c in range(8):
                        A("dve", lambda e, c=c, tok=tok: e.scalar_tensor_tensor(
                            out=tmpf[c % 2], in0=xT[:, c, tok], scalar=Gv[:, c:c + 1], in1=pb[5][:],
                            op0=ALU.mult, op1=ALU.mult),
                          r=[("xT", (t0 + tt * 512) // 512), "Gv", ("pb", 5)], w=[("tmpf", c % 2)])
                        A("act", lambda e, c=c, tt=tt: e.activation(
                            out=hT[:, c, tt * 512:(tt + 1) * 512], in_=tmpf[c % 2], func=AF.Identity,
                            bias=modv[:, l, c, s:s + 1], scale=1.0),
                          r=[("tmpf", c % 2), "modv"], w=[("hT", tt)])
                if dbg and first_layer_dbg and half == 0:
                    A("sp", lambda e: e.dma_start(out=d_h[:, :, :], in_=hT[:]), r=[("hT", 0), ("hT", 1)], dma="dbg_h")

                if STOP_AFTER == "H":
                    continue
                P.barrier()
                g1 = carve(0, 4, [NHALF], F32)
                g2 = carve(4096, 4, [NHALF], F32)
                g3 = carve(8192, 4, [NHALF], F32)
                g4 = carve(12288, 4, [NHALF], F32)
                g5 = carve(16384, 4, [NHALF], F32)
                msk = carve(20480, 4, [NHALF], F32)
                dsm = carve(24576, 4, [8], F32)
                slot, offs = jobs.get()

                gname = {id(g1): "g1", id(g2): "g2", id(g3): "g3", id(g4): "g4", id(g5): "g5"}

                def gate_proj(gi, dst):
                    def ev(bank, tt):
                        A("act", lambda e, bank=bank, tt=tt: e.activation(
                            out=dst[:, tt * 512:(tt + 1) * 512], in_=pb[bank][0:4, :], func=AF.Identity,
                            bias=gb[:, gi:gi + 1], scale=1.0), r=[("pb", bank), "gb"], w=[("g", gname[id(dst)], tt)])
                    proj_fm(slot, 4 * gi, 4, ev)

                gate_proj(0, g1)
                gate_proj(1, g2)
                gate_proj(2, g4)
                gk = lambda t: [("g", gname[id(t)], 0), ("g", gname[id(t)], 1)]
                for gt in (g2, g4):
                    A("act", lambda e, gt=gt: e.activation(out=gt, in_=gt, func=AF.Exp, scale=-1.0), r=gk(gt), w=gk(gt))
                    A("act", lambda e, gt=gt: e.activation(out=gt, in_=gt, func=AF.Ln, bias=cst[0:4, C1:C1 + 1], scale=1.0),
                      r=gk(gt) + ["cst"], w=gk(gt))
                A("dve", lambda e: e.memset(msk, 1.0), w=["msk"])
                A("dve", lambda e: e.memset(msk.rearrange("p (a b) -> p a b", b=128)[:, :, 0:1], 0.0), r=["msk"], w=["msk"])
                A("dve", lambda e: e.tensor_tensor_scan(out=g3, data0=msk, data1=g2, initial=0.0,
                                                        op0=ALU.mult, op1=ALU.add), r=["msk"] + gk(g2), w=gk(g3))
                g3v = g3.rearrange("p (a b) -> p a b", b=128)
                A("act", lambda e: e.activation(out=dsm, in_=g3v[:, :, 127], func=AF.Exp, scale=-1.0), r=gk(g3), w=["dsm"])
                A("dve", lambda e: e.tensor_tensor(out=g2.rearrange("p (a b) -> p a b", b=128), in0=g3v,
                                                   in1=g3v[:, :, 127:128].to_broadcast([4, 8, 128]), op=ALU.subtract),
                  r=gk(g3), w=gk(g2))
                A("dve", lambda e: e.tensor_tensor(out=g1, in0=g1, in1=g2, op=ALU.add), r=gk(g1) + gk(g2), w=gk(g1))
                A("act", lambda e: e.activation(out=g1, in_=g1, func=AF.Exp, bias=cst[0:4, CLN:CLN + 1], scale=1.0),
                  r=gk(g1) + ["cst"], w=gk(g1))
                A("act", lambda e: e.activation(out=g2, in_=g2, func=AF.Exp), r=gk(g2), w=gk(g2))
                A("dve", lambda e: e.memset(msk, 1.0), r=["msk"] + gk(g3), w=["msk"])
                A("dve", lambda e: e.tensor_tensor_scan(out=g5, data0=msk, data1=g4, initial=carry[:, 0:1],
                                                        op0=ALU.mult, op1=ALU.add),
                  r=["msk", "carry"] + gk(g4), w=gk(g5))
                A("dve", lambda e: e.tensor_copy(out=carry[:, 0:1], in_=g5[:, NHALF - 1:NHALF]), r=gk(g5), w=["carry"])
                A("act", lambda e: e.activation(out=cumq_bf[:], in_=g5, func=AF.Identity, scale=-1.0),
                  r=gk(g5), w=["cumq_bf"])
                for t8 in range(8):
                    for qi, gt in enumerate((g1, g2)):
                        A("pe", lambda e, t8=t8, qi=qi, gt=gt: e.transpose(
                            out=pb[6][:, (t8 * 2 + qi) * 4:(t8 * 2 + qi) * 4 + 4], in_=gt[:, t8 * 128:(t8 + 1) * 128],
                            identity=ident_f[0:4, 0:4]), r=gk(gt) + ["ident_f"], w=[("pb", 6)])
                    A("pe", lambda e, t8=t8: e.transpose(
                        out=pb[7][:, t8 * 4:t8 * 4 + 4], in_=g5[:, t8 * 128:(t8 + 1) * 128],
                        identity=ident_f[0:4, 0:4]), r=gk(g5) + ["ident_f"], w=[("pb", 7)])
                A("dve", lambda e: e.tensor_copy(out=wtm[:].rearrange("p a b c -> p (a b c)"), in_=pb[6][:, 0:64]),
                  r=[("pb", 6)], w=["wtm"])
                A("dve", lambda e: e.tensor_copy(out=cumspT[:, half * 8:half * 8 + 8, :].rearrange("p a b -> p (a b)"),
                                                 in_=pb[7][:, 0:32]), r=[("pb", 7)], w=["cumspT"])
                for h in range(4):
                    A("pe", lambda e, h=h: e.matmul(pb[5][:, h * 8:(h + 1) * 8], lhsT=selF[:, h, :], rhs=dsm,
                                                    start=True, stop=True), r=["selF", "dsm"], w=[("pb", 5)])
                A("dve", lambda e: e.tensor_copy(out=dbc[:].rearrange("p a b -> p (a b)"), in_=pb[5][:, 0:32]),
                  r=[("pb", 5)], w=["dbc"])
                if dbg and first_layer_dbg and half == 0:
                    A("sp", lambda e: e.dma_start(out=d_gt[:, :], in_=wtm[:].rearrange("p a b c -> p (a b c)")),
                      r=["wtm"], dma="dbg_gt")

                if STOP_AFTER == "G":
                    continue
                for hp in range(2):
                    P.barrier()
                    qT = carve(0, 128, [2, NHALF], BF16)
                    kT = carve(4096, 128, [2, NHALF], BF16)
                    zT = carve(8192, 128, [2, NHALF], BF16)
                    vext = carve(12288, 128, [8, 2, 130], BF16)
                    osb = carve(16448, 128, [8, 256], BF16)
                    pre = [carve(20544 + i * 2064, 128, [3 + NHALF + 5], BF16) for i in range(2)]
                    dg4 = carve(24672, 128, [16, 128], BF16)
                    bbc = carve(28768, 128, [512], F32)
                    tbase = 30816
                    kp = [carve(tbase + i * 256, 128, [128], BF16) for i in range(2)]
                    spb = [carve(tbase + 512 + i * 256, 128, [128], BF16) for i in range(2)]
                    cbf = [carve(tbase + 1024 + i * 264, 128, [130], BF16) for i in range(2)]
                    hhf = [carve(tbase + 1552 + i * 512, 128, [128], F32) for i in range(2)]
                    hnb = [carve(tbase + 2576 + i * 256, 128, [128], BF16) for i in range(2)]
                    osig = carve(tbase + 3088, 128, [256], F32)
                    stt = [carve(tbase + 4112 + i * 64, 128, [16], F32) for i in range(2)]
                    for ci in range(4):
                        chunk = (0 if ci < 2 else 4) + hp * 2 + (ci % 2)
                        for j in range(4):
                            A("dve", lambda e, ci=ci, j=j, chunk=chunk: e.tensor_scalar(
                                out=dg4[:, ci * 4 + j, :], in0=ident_b[:],
                                scalar1=vecs[:, V_CW + j * 8 + chunk:V_CW + j * 8 + chunk + 1], scalar2=None,
                                op0=ALU.mult), r=["ident_b", "vecs"], w=[("dg4", ci)])
                    A("sp", lambda e, hp=hp: e.dma_start(out=bbc[:, 0:256],
                                                         in_=bin_d[l, :, C_MV + hp * 256:C_MV + hp * 256 + 256].partition_broadcast(128)),
                      w=["bbc"], r=["bbc"], dma="bbc")
                    A("sp", lambda e, hp=hp: e.dma_start(out=bbc[:, 256:512],
                                                         in_=bin_d[l, :, C_MO + hp * 256:C_MO + hp * 256 + 256].partition_broadcast(128)),
                      w=["bbc"], r=["bbc"], dma="bbc")
                    A("dve", lambda e: e.memset(vext, 1.0), w=["vext"], r=["vext"])
                    slot, offs = jobs.get()
                    for ci in range(4):
                        chunk = (0 if ci < 2 else 4) + hp * 2 + (ci % 2)
                        pr = pre[ci % 2]
                        dstT = qT if ci < 2 else kT
                        bcol = (V_BQ if ci < 2 else V_BK) + hp * 2 + (ci % 2)
                        A("act", lambda e, pr=pr, chunk=chunk: e.copy(out=pr[:, 0:3], in_=halo[:, chunk, 0:3]),
                          r=["halo"], w=[("pre", ci % 2, 0)])

                        def ev(bank, tt, pr=pr, bcol=bcol, ci=ci):
                            A("act", lambda e: e.activation(out=pr[:, 3 + tt * 512:3 + (tt + 1) * 512], in_=pb[bank][:],
                                                            func=AF.Identity, bias=vecs[:, bcol:bcol + 1], scale=1.0),
                              r=[("pb", bank), "vecs"], w=[("pre", ci % 2, tt)])
                        proj_fm(slot, ci * 128, 128, ev)
                        A("act", lambda e, pr=pr, chunk=chunk: e.copy(out=halo[:, chunk, 0:3], in_=pr[:, NHALF:NHALF + 3]),
                          r=[("pre", ci % 2, 1)], w=["halo"])
                        for tt in range(2):
                            bank = 2 + (tt % 2)
                            for j in range(4):
                                A("pe", lambda e, j=j, tt=tt, bank=bank, pr=pr, ci=ci: e.matmul(
                                    pb[bank][:], lhsT=dg4[:, ci * 4 + j, :], rhs=pr[:, tt * 512 + j:tt * 512 + j + 512],
                                    start=(j == 0), stop=(j == 3)),
                                  r=[("dg4", ci), ("pre", ci % 2, 0), ("pre", ci % 2, 1)], w=[("pb", bank)])
                            A("act", lambda e, tt=tt, bank=bank, dstT=dstT, ci=ci, chunk=chunk: e.activation(
                                out=dstT[:, ci % 2, tt * 512:(tt + 1) * 512], in_=pb[bank][:], func=AF.Silu,
                                bias=vecs[:, V_CB + chunk:V_CB + chunk + 1], scale=1.0),
                              r=[("pb", bank), "vecs"], w=[("qk", ci, tt)])
                    if dbg and first_layer_dbg and half == 0 and hp == 0:
                        A("sp", lambda e: e.dma_start(out=d_q[:, :, :], in_=qT), r=[("qk", 0, 0), ("qk", 0, 1), ("qk", 1, 0), ("qk", 1, 1)], dma="dbg_q")
                    slot, offs = jobs.get()
                    for i in range(2):
                        def ev(bank, tt, i=i):
                            A("act", lambda e: e.activation(out=zT[:, i, tt * 512:(tt + 1) * 512], in_=pb[bank][:],
                                                            func=AF.Silu, bias=vecs[:, V_BZ + hp * 2 + i:V_BZ + hp * 2 + i + 1],
                                                            scale=1.0), r=[("pb", bank), "vecs"], w=[("zT", i, tt)])
                        proj_fm(slot, i * 128, 128, ev)
                    slot, offs = jobs.get()

                    def ev_vo(bank, t8):
                        A("dve", lambda e: e.tensor_tensor(
                            out=vext[:, t8, :, 0:128], in0=pb[bank][:, 0:256].rearrange("p (a b) -> p a b", b=128),
                            in1=bbc[:, 0:256].rearrange("p (a b) -> p a b", b=128), op=ALU.add),
                          r=[("pb", bank), "bbc", "vext"], w=[("vext", t8)])
                        A("dve", lambda e: e.tensor_tensor(out=osig, in0=pb[bank][:, 256:512], in1=bbc[:, 256:512], op=ALU.add),
                          r=[("pb", bank), "bbc"], w=["osig"])
                        A("act", lambda e: e.activation(out=osb[:, t8, :], in_=osig, func=AF.Sigmoid),
                          r=["osig"], w=[("osb", t8)])
                    proj_tm(slot, 0, 512, ev_vo)
                    P.barrier()
                    def stA(it, hh, c8):
                        h = hp * 2 + hh
                        sset = it % 2
                        bT, bSt, bN, bC = sset, 2 + sset, 4 + sset, 6
                        tok = slice(c8 * 128, (c8 + 1) * 128)
                        wcol = wtm[:, c8, 0, h:h + 1]
                        dcol = dbc[:, h, c8:c8 + 1]
                        qk_r = [("qk", hh, c8 // 4), ("qk", 2 + hh, c8 // 4)]
                        tpv = pbb(bT)[:, 0:128]
                        stv = pb[bSt][:, 0:128]
                        A("pe", lambda e: e.transpose(out=tpv, in_=kT[:, hh, tok], identity=ident_b[:]),
                          r=qk_r + ["ident_b"], w=[("pb", bT)])
                        A("act", lambda e: e.activation(out=kp[sset], in_=tpv, func=AF.Identity, scale=wcol),
                          r=[("pb", bT), "wtm"], w=[("kp", sset)])
                        A("pe", lambda e: e.matmul(stv, lhsT=kT[:, hh, tok], rhs=qT[:, hh, tok], start=True, stop=True),
                          r=qk_r, w=[("pb", bSt)])
                        A("dve", lambda e: e.scalar_tensor_tensor(
                            out=spb[sset], in0=stv, scalar=wcol, in1=maskT[:], op0=ALU.mult, op1=ALU.mult),
                          r=[("pb", bSt), "wtm", "maskT"], w=[("spb", sset)])
                        A("dve", lambda e: e.tensor_scalar(
                            out=cbf[sset], in0=Cst[:, h, :], scalar1=dcol, scalar2=None, op0=ALU.mult),
                          r=["Cst", "dbc"], w=[("cbf", sset)])
                        A("pe", lambda e: e.matmul(
                            pb[bN][:, 0:130], lhsT=spb[sset], rhs=vext[:, c8, hh, :], start=True, stop=False),
                          r=[("spb", sset), ("vext", c8), "vext"], w=[("pb", bN)])
                        A("pe", lambda e: e.matmul(
                            pb[bN][:, 0:130], lhsT=qT[:, hh, tok], rhs=cbf[sset], start=False, stop=True),
                          r=qk_r + [("cbf", sset)], w=[("pb", bN)])
                        A("pe", lambda e: e.matmul(
                            pb[bC][:, 0:130], lhsT=kp[sset], rhs=vext[:, c8, hh, :], start=True, stop=True),
                          r=[("kp", sset), ("vext", c8), "vext"], w=[("pb", bC)])
                        A("dve", lambda e: e.scalar_tensor_tensor(
                            out=Cst[:, h, :], in0=Cst[:, h, :], scalar=dcol, in1=pb[bC][:, 0:130],
                            op0=ALU.mult, op1=ALU.add), r=["Cst", "dbc", ("pb", bC)], w=["Cst"])

                    def stB(it, hh, c8):
                        h = hp * 2 + hh
                        sset = it % 2
                        bN = 4 + sset
                        fcol = wtm[:, c8, 1, h:h + 1]
                        sv = stt[sset]
                        A("dve", lambda e: e.tensor_scalar(
                            out=sv[:, 12:13], in0=pb[bN][:, 128:129], scalar1=-1.0, scalar2=fcol,
                            op0=ALU.mult, op1=ALU.max), r=[("pb", bN), "wtm"], w=[("stt", sset)])
                        A("dve", lambda e: e.tensor_tensor(
                            out=sv[:, 0:1], in0=sv[:, 12:13], in1=pb[bN][:, 128:129], op=ALU.max),
                          r=[("pb", bN), ("stt", sset)], w=[("stt", sset)])
                        A("dve", lambda e: e.reciprocal(out=sv[:, 1:2], in_=sv[:, 0:1]),
                          r=[("stt", sset)], w=[("stt", sset)])
                        A("dve", lambda e: e.scalar_tensor_tensor(
                            out=hhf[sset], in0=pb[bN][:, 0:128], scalar=sv[:, 1:2], in1=osb[:, c8, hh * 128:(hh + 1) * 128],
                            op0=ALU.mult, op1=ALU.mult), r=[("pb", bN), ("stt", sset), ("osb", c8)], w=[("hhf", sset)])
                        A("dve", lambda e: e.bn_stats(out=sv[:, 2:8], in_=hhf[sset]),
                          r=[("hhf", sset)], w=[("stt", sset)])
                        A("dve", lambda e: e.bn_aggr(out=sv[:, 8:10], in_=sv[:, 2:8]),
                          r=[("stt", sset)], w=[("stt", sset)])
                        A("act", lambda e: e.activation(out=sv[:, 10:11], in_=sv[:, 9:10], func=AF.Ln,
                                                        bias=cst[:, CE5:CE5 + 1], scale=1.0),
                          r=[("stt", sset), "cst"], w=[("stt", sset)])
                        A("act", lambda e: e.activation(out=sv[:, 11:12], in_=sv[:, 10:11], func=AF.Exp, scale=-0.5),
                          r=[("stt", sset)], w=[("stt", sset)])

                    def stC(it, hh, c8):
                        sset = it % 2
                        sv = stt[sset]
                        tp2v = pbb(7)[:, 0:128]
                        A("dve", lambda e: e.tensor_scalar(
                            out=hnb[sset], in0=hhf[sset], scalar1=sv[:, 8:9], scalar2=sv[:, 11:12],
                            op0=ALU.subtract, op1=ALU.mult), r=[("hhf", sset), ("stt", sset)], w=[("hnb", sset)])
                        A("pe", lambda e: e.transpose(out=tp2v, in_=hnb[sset], identity=ident_b[:]),
                          r=[("hnb", sset), "ident_b"], w=[("pb", 7)])

                    def stD(it, hh, c8):
                        h = hp * 2 + hh
                        sset = it % 2
                        tok = slice(c8 * 128, (c8 + 1) * 128)
                        tp2v = pbb(7)[:, 0:128]
                        A("dve", lambda e: e.scalar_tensor_tensor(
                            out=yTa[:, h, tok], in0=tp2v, scalar=vecs[:, V_HG + h:V_HG + h + 1], in1=zT[:, hh, tok],
                            op0=ALU.mult, op1=ALU.mult),
                          r=[("pb", 7), "vecs", ("zT", hh, c8 // 4)], w=[("yTa", h)])

                    its = [(i, i // 8, i % 8) for i in range(16)]
                    for step in range(16 + 3):
                        if step < 16:
                            stA(*its[step])
                        if 0 <= step - 1 < 16:
                            stB(*its[step - 1])
                        if 0 <= step - 3 < 16:
                            stD(*its[step - 3])
                        if 0 <= step - 2 < 16:
                            stC(*its[step - 2])
                if dbg and first_layer_dbg and half == 0:
                    A("sp", lambda e: e.dma_start(out=d_ya[:, :, :], in_=yTa[:]), r=[("yTa", i) for i in range(4)], dma="dbg_ya")

                if STOP_AFTER == "A":
                    continue
                P.barrier()
                fqT = carve(0, 128, [2, NHALF], BF16)
                fzT = carve(8192, 64, [4, NHALF], BF16)
                ptb = [carve(16384 + i * 1024, 128, [512], BF16) for i in range(2)]
                rden = carve(18432, 128, [512], F32)
                t1 = carve(20480, 64, [512], F32)
                ytmp = carve(22528, 64, [512], BF16)
                bbf = carve(23552, 128, [256], F32)
                A("sp", lambda e: e.dma_start(out=bbf, in_=bin_d[l, :, C_FV:C_FV + 256].partition_broadcast(128)),
                  w=["bbf"], dma="bbf")
                slot, offs = jobs.get()
                for i in range(2):
                    def evq(bank, tt, i=i):
                        A("dve", lambda e: e.tensor_scalar(out=fqT[:, i, tt * 512:(tt + 1) * 512], in0=pb[bank][:],
                                                           scalar1=vecs[:, V_BFQ + i:V_BFQ + i + 1], scalar2=0.125,
                                                           op0=ALU.add, op1=ALU.mult),
                          r=[("pb", bank), "vecs"], w=[("fqT", tt)])
                    proj_fm(slot, i * 128, 128, evq)

                    def evk(bank, tt, i=i):
                        A("act", lambda e: e.activation(out=fkT[:, i, t0 + tt * 512:t0 + (tt + 1) * 512], in_=pb[bank][:],
                                                        func=AF.Identity, bias=vecs[:, V_BFK + i:V_BFK + i + 1], scale=1.0),
                          r=[("pb", bank), "vecs"], w=["fkT"])
                    proj_fm(slot, 256 + i * 128, 128, evk)
                slot, offs = jobs.get()

                def ev_fv(bank, t8):
                    A("dve", lambda e: e.tensor_tensor(
                        out=Vext[:, half * 8 + t8, :, 0:64], in0=pb[bank][:, 0:256].rearrange("p (a b) -> p a b", b=64),
                        in1=bbf.rearrange("p (a b) -> p a b", b=64), op=ALU.add),
                      r=[("pb", bank), "bbf"], w=["Vext"])
                proj_tm(slot, 0, 256, ev_fv)
                for h in range(4):
                    def evz(bank, tt, h=h):
                        A("act", lambda e: e.activation(out=fzT[:, h, tt * 512:(tt + 1) * 512], in_=pb[bank][0:64, :],
                                                        func=AF.Silu, bias=vec64[:, h:h + 1], scale=1.0),
                          r=[("pb", bank), "vec64"], w=[("fzT", tt)])
                    proj_fm(slot, 256 + h * 64, 64, evz)
                P.barrier()
                fblocks = []
                nO = 0
                for h in range(4):
                    for Q in range(2):
                        q0t = (t0 + Q * 512) // 128
                        nkb = q0t + 4
                        bO = 2 + (nO % 2)
                        nO += 1
                        for kb in range(nkb):
                            fblocks.append((len(fblocks), h, Q, kb, nkb, q0t, bO))

                def foxS(nS, h, Q, kb, nkb, q0t, bO):
                    kc, pbase = h // 2, (h % 2) * 64
                    j = kb - q0t
                    nc0 = max(0, j) * 128
                    N = 512 - nc0
                    bS = nS % 2
                    pt = ptb[nS % 2]
                    regs = [(0, N, False)] if j < 0 else ([(0, 128, True)] + ([(128, N, False)] if N > 128 else []))
                    for (ra, rb, dg) in regs:
                        qsr = slice(Q * 512 + nc0 + ra, Q * 512 + nc0 + rb)
                        A("pe", lambda e: e.matmul(
                            pb[bS][:, ra:rb], lhsT=fkT[pbase:pbase + 64, kc, kb * 128:(kb + 1) * 128],
                            rhs=fqT[pbase:pbase + 64, kc, qsr], start=True, stop=False),
                          r=["fkT", ("fqT", Q)], w=[("pb", bS)])
                        A("pe", lambda e: e.matmul(
                            pb[bS][:, ra:rb], lhsT=selB[:, h, :], rhs=cumq_bf[:, qsr], start=False, stop=(not dg)),
                          r=["selB", "cumq_bf"], w=[("pb", bS)])
                        if dg:
                            A("pe", lambda e: e.matmul(
                                pb[bS][:, ra:rb], lhsT=ident_b[:], rhs=negmask[:], start=False, stop=True),
                              r=["ident_b", "negmask"], w=[("pb", bS)])
                    A("act", lambda e: e.activation(
                        out=pt[:, 0:N], in_=pb[bS][:, 0:N], func=AF.Exp, bias=cumspT[:, kb, h:h + 1], scale=1.0),
                      r=[("pb", bS), "cumspT"], w=[("pt", bS)])

                def foxPV(nS, h, Q, kb, nkb, q0t, bO):
                    kc, pbase = h // 2, (h % 2) * 64
                    j = kb - q0t
                    nc0 = max(0, j) * 128
                    N = 512 - nc0
                    bS = nS % 2
                    pt = ptb[nS % 2]
                    A("pe", lambda e: e.matmul(
                        pb[bO][0:65, nc0:512], lhsT=Vext[:, kb, h, :], rhs=pt[:, 0:N],
                        start=(kb == 0), stop=(kb == nkb - 1)),
                      r=["Vext", ("pt", bS)], w=[("pb", bO)])
                    if kb != nkb - 1:
                        return
                    A("act", lambda e: e.copy(out=rden[0:1, :], in_=pb[bO][64:65, :]),
                      r=[("pb", bO)], w=["rden"])
                    A("dve", lambda e: e.reciprocal(out=rden[0:1, :], in_=rden[0:1, :]), r=["rden"], w=["rden"])
                    A("pe", lambda e: e.matmul(pb[4][0:64, :], lhsT=onesF[0:1, 0:64], rhs=rden[0:1, :],
                                               start=True, stop=True), r=["rden", "onesF"], w=[("pb", 4)])
                    A("dve", lambda e: e.tensor_tensor(
                        out=t1, in0=pb[bO][0:64, :], in1=fzT[:, h, Q * 512:(Q + 1) * 512], op=ALU.mult),
                      r=[("pb", bO), ("fzT", Q)], w=["t1"])
                    if pbase == 0:
                        A("dve", lambda e: e.tensor_tensor(
                            out=yTb[0:64, kc, Q * 512:(Q + 1) * 512], in0=t1, in1=pb[4][0:64, :], op=ALU.mult),
                          r=["t1", ("pb", 4)], w=[("yTb", kc)])
                    else:
                        A("dve", lambda e: e.tensor_tensor(out=ytmp, in0=t1, in1=pb[4][0:64, :], op=ALU.mult),
                          r=["t1", ("pb", 4)], w=["ytmp"])
                        A("act", lambda e: e.copy(out=yTb[64:128, kc, Q * 512:(Q + 1) * 512], in_=ytmp),
                          r=["ytmp"], w=[("yTb", kc)])

                foxS(*fblocks[0])
                for bi in range(len(fblocks)):
                    if bi + 1 < len(fblocks):
                        foxS(*fblocks[bi + 1])
                    foxPV(*fblocks[bi])
                if dbg and first_layer_dbg and half == 0:
                    A("sp", lambda e: e.dma_start(out=d_yb[:, :, :], in_=yTb[:]), r=[("yTb", 0), ("yTb", 1)], dma="dbg_yb")

                if STOP_AFTER == "B":
                    continue
                P.barrier()
                sg = [carve(i * 2048, 128, [512], F32) for i in range(2)]
                zcT = carve(4096, 128, [2, NHALF], BF16)
                dg31 = carve(8192, 128, [31, 128], BF16)
                yf = carve(16384, 128, [512], F32)
                ybf = carve(18432, 128, [512], BF16)
                ysq = carve(19456, 128, [512], BF16)
                msq = carve(20480, 128, [512], F32)
                var = carve(22528, 128, [512], F32)
                tt_ = carve(24576, 128, [512], F32)
                ss_ = carve(26624, 128, [512], F32)
                if half == 0:
                    A("dve", lambda e: e.memset(ubuf[:, :, 0:30], 0.0), w=["ubuf"], r=["ubuf"])
                else:
                    A("act", lambda e: e.copy(out=ubuf[:, :, 0:30], in_=ubuf[:, :, NHALF:NHALF + 30]), w=["ubuf"], r=["ubuf"])
                slot, offs = jobs.get()
                for c in range(2):
                    for tt in range(2):
                        bank = next_acc()
                        for k in range(8):
                            A("pe", lambda e, k=k, tt=tt, bank=bank, c=c: e.matmul(
                                pb[bank][:], lhsT=wbuf[slot][:, k, 256 + c * 128:256 + (c + 1) * 128],
                                rhs=hT[:, k, tt * 512:(tt + 1) * 512], start=(k == 0), stop=(k == 7)),
                              r=[("wb", slot), ("hT", tt)], w=[("pb", bank)])
                        A("act", lambda e, bank=bank, c=c, tt=tt: e.activation(
                            out=sg[tt % 2], in_=pb[bank][:], func=AF.Sigmoid, bias=vecs[:, V_BCG + c:V_BCG + c + 1], scale=1.0),
                          r=[("pb", bank), "vecs"], w=[("sg", tt % 2)])
                        bank = next_acc()
                        for k in range(8):
                            A("pe", lambda e, k=k, tt=tt, bank=bank, c=c: e.matmul(
                                pb[bank][:], lhsT=wbuf[slot][:, k, c * 128:(c + 1) * 128],
                                rhs=hT[:, k, tt * 512:(tt + 1) * 512], start=(k == 0), stop=(k == 7)),
                              r=[("wb", slot), ("hT", tt)], w=[("pb", bank)])
                        A("dve", lambda e, bank=bank, c=c, tt=tt: e.scalar_tensor_tensor(
                            out=ubuf[:, c, 30 + tt * 512:30 + (tt + 1) * 512], in0=pb[bank][:],
                            scalar=vecs[:, V_BCA + c:V_BCA + c + 1], in1=sg[tt % 2], op0=ALU.add, op1=ALU.mult),
                          r=[("pb", bank), "vecs", ("sg", tt % 2)], w=["ubuf"])
                slot, offs = jobs.get()
                for c in range(2):
                    def evcz(bank, tt, c=c):
                        A("act", lambda e: e.activation(out=zcT[:, c, tt * 512:(tt + 1) * 512], in_=pb[bank][:], func=AF.Silu,
                                                        bias=vecs[:, V_BCZ + c:V_BCZ + c + 1], scale=1.0),
                          r=[("pb", bank), "vecs"], w=[("zcT", c)])
                    proj_fm(slot, c * 128, 128, evcz)
                for c in range(2):
                    for j in range(31):
                        A("dve", lambda e, c=c, j=j: e.tensor_scalar(
                            out=dg31[:, j, :], in0=ident_b[:], scalar1=vecs[:, V_DW + j * 2 + c:V_DW + j * 2 + c + 1],
                            scalar2=None, op0=ALU.mult), r=["ident_b", "vecs"], w=["dg31"])
                    for tt in range(2):
                        bank = 2 + tt
                        for j in range(31):
                            A("pe", lambda e, c=c, j=j, tt=tt, bank=bank: e.matmul(
                                pb[bank][:], lhsT=dg31[:, j, :], rhs=ubuf[:, c, tt * 512 + j:tt * 512 + j + 512],
                                start=(j == 0), stop=(j == 30)), r=["dg31", "ubuf"], w=[("pb", bank)])
                        bcol = vecs[:, V_DWB + c:V_DWB + c + 1]
                        A("act", lambda e, bank=bank, bcol=bcol: e.activation(out=yf, in_=pb[bank][:], func=AF.Identity,
                                                                              bias=bcol, scale=1.0),
                          r=[("pb", bank), "vecs"], w=["yf"])
                        A("act", lambda e, bank=bank, bcol=bcol: e.activation(out=ysq, in_=pb[bank][:], func=AF.Square,
                                                                              bias=bcol, scale=1.0),
                          r=[("pb", bank), "vecs"], w=["ysq"])
                        A("dve", lambda e: e.tensor_copy(out=ybf, in_=yf), r=["yf"], w=["ybf"])
                        A("pe", lambda e: e.matmul(pb[4][:], lhsT=blk64[:], rhs=ybf, start=True, stop=True),
                          r=["blk64", "ybf"], w=[("pb", 4)])
                        A("pe", lambda e: e.matmul(pb[5][:], lhsT=blk64[:], rhs=ysq, start=True, stop=True),
                          r=["blk64", "ysq"], w=[("pb", 5)])
                        A("act", lambda e: e.activation(out=msq, in_=pb[4][:], func=AF.Square), r=[("pb", 4)], w=["msq"])
                        A("dve", lambda e: e.tensor_tensor(out=var, in0=pb[5][:], in1=msq, op=ALU.subtract),
                          r=[("pb", 5), "msq"], w=["var"])
                        A("act", lambda e: e.activation(out=var, in_=var, func=AF.Ln, bias=cst[:, CE5:CE5 + 1], scale=1.0),
                          r=["var", "cst"], w=["var"])
                        A("act", lambda e: e.activation(out=var, in_=var, func=AF.Exp, scale=-0.5), r=["var"], w=["var"])
                        A("dve", lambda e: e.tensor_tensor(out=tt_, in0=yf, in1=pb[4][:], op=ALU.subtract),
                          r=["yf", ("pb", 4)], w=["tt_"])
                        A("dve", lambda e: e.tensor_tensor(out=tt_, in0=tt_, in1=var, op=ALU.mult), r=["tt_", "var"], w=["tt_"])
                        A("act", lambda e, c=c: e.activation(out=ss_, in_=tt_, func=AF.Silu,
                                                             scale=vecs[:, V_LNG + c:V_LNG + c + 1],
                                                             bias=vecs[:, V_LNB + c:V_LNB + c + 1]),
                          r=["tt_", "vecs"], w=["ss_"])
                        A("dve", lambda e, c=c, tt=tt: e.tensor_tensor(out=yTc[:, c, tt * 512:(tt + 1) * 512], in0=ss_,
                                                                         in1=zcT[:, c, tt * 512:(tt + 1) * 512], op=ALU.mult),
                          r=["ss_", ("zcT", c)], w=[("yTc", c)])
                if dbg and first_layer_dbg and half == 0:
                    A("sp", lambda e: e.dma_start(out=d_yc[:, :, :], in_=yTc[:]), r=[("yTc", 0), ("yTc", 1)], dma="dbg_yc")

                if STOP_AFTER == "C":
                    continue
                P.barrier()
                ycat = [yTa[:, 0, :], yTa[:, 1, :], yTa[:, 2, :], yTa[:, 3, :], yTb[:, 0, :], yTb[:, 1, :],
                        yTc[:, 0, :], yTc[:, 1, :]]
                for jb in range(2):
                    slot, offs = jobs.get()
                    for d4 in range(4):
                        dc = jb * 4 + d4
                        for tt in range(2):
                            bank = next_acc()
                            for k in range(8):
                                A("pe", lambda e, k=k, tt=tt, bank=bank, d4=d4, slot=slot: e.matmul(
                                    pb[bank][:], lhsT=wbuf[slot][:, k, d4 * 128:(d4 + 1) * 128],
                                    rhs=ycat[k][:, tt * 512:(tt + 1) * 512], start=(k == 0), stop=(k == 7)),
                                  r=[("wb", slot), "ycat"], w=[("pb", bank)])
                            tok = slice(t0 + tt * 512, t0 + (tt + 1) * 512)
                            A("dve", lambda e, bank=bank, dc=dc, tok=tok: e.scalar_tensor_tensor(
                                out=xT[:, dc, tok], in0=pb[bank][:], scalar=modv[:, l, 16 + dc, s:s + 1], in1=xT[:, dc, tok],
                                op0=ALU.mult, op1=ALU.add),
                              r=[("pb", bank), "modv", ("xT", (t0 + tt * 512) // 512)], w=[("xT", (t0 + tt * 512) // 512)])

        for s in range(NS):
            load_x(s)
            for l in range(L):
                if s == 0:
                    load_vecs(l)
                    ada(l)
                layer(l, s, first_layer_dbg=(s == 0 and l == 0))
            store_x(s)
        P.finalize_and_emit()
    return nc


def _chunks(v):
    return np.ascontiguousarray(v.reshape(-1, 128).T)


def _host_vecs(inp, l):
    b_in = inp["b_in"][l]
    cols = [
        _chunks(inp["norm_g"][l]), _chunks(inp["b_ada"][l]),
        _chunks(b_in[C_MQ:C_MQ + 512]), _chunks(b_in[C_MK:C_MK + 512]), _chunks(b_in[C_MZ:C_MZ + 512]),
        _chunks(b_in[C_FQ:C_FQ + 256]), _chunks(b_in[C_FK:C_FK + 256]),
        _chunks(b_in[C_CA:C_CA + 256]), _chunks(b_in[C_CG:C_CG + 256]), _chunks(b_in[C_CZ:C_CZ + 256]),
        _chunks(inp["m_conv_b"][l]), _chunks(inp["m_hn_g"][l]),
        _chunks(inp["c_dw_b"][l]), _chunks(inp["c_ln_g"][l]), _chunks(inp["c_ln_b"][l]),
    ]
    cw = inp["m_conv_w"][l]
    cols.append(np.concatenate([_chunks(cw[j]) for j in range(4)], axis=1))
    dw = inp["c_dw_w"][l]
    cols.append(np.concatenate([_chunks(dw[j]) for j in range(31)], axis=1))
    v = np.concatenate(cols, axis=1).astype(np.float32)
    assert v.shape == (128, NV), v.shape
    vec64 = np.ascontiguousarray(np.concatenate([b_in[C_FZ:C_FZ + 256].reshape(4, 64).T, b_in[C_FQ:C_FQ + 256].reshape(4, 64).T,
                                                 b_in[C_FK:C_FK + 256].reshape(4, 64).T], axis=1)).astype(np.float32)
    gbv = np.stack([b_in[C_MI:C_MI + 4], b_in[C_MF:C_MF + 4], b_in[C_FF:C_FF + 4]], axis=1).astype(np.float32)
    return v, vec64, gbv


_NC_CACHE = {}


def _get_nc(L, NS, final_norm, dbg=False):
    key = (L, NS, final_norm, dbg)
    if key not in _NC_CACHE:
        _NC_CACHE[key] = build(L, NS, final_norm, dbg)
    return _NC_CACHE[key]


FUSED = True


def kernel(x, c, norm_g, w_ada, b_ada, w_in, b_in, m_conv_w, m_conv_b, m_hn_g,
           c_dw_w, c_dw_b, c_ln_g, c_ln_b, w_out, final_g):
    inp = dict(norm_g=np.asarray(norm_g), b_ada=np.asarray(b_ada), b_in=np.asarray(b_in),
               m_conv_w=np.asarray(m_conv_w), m_conv_b=np.asarray(m_conv_b), m_hn_g=np.asarray(m_hn_g),
               c_dw_w=np.asarray(c_dw_w), c_dw_b=np.asarray(c_dw_b), c_ln_g=np.asarray(c_ln_g),
               c_ln_b=np.asarray(c_ln_b))
    x = np.asarray(x, dtype=np.float32)
    c = np.asarray(c, dtype=np.float32)
    w_ada = np.asarray(w_ada, dtype=np.float32)
    w_in = np.asarray(w_in, dtype=np.float32)
    w_out = np.asarray(w_out, dtype=np.float32)
    b_in_a = np.asarray(b_in, dtype=np.float32)
    fg = np.asarray(final_g, dtype=np.float32).reshape(1, D)
    DEPTH = w_in.shape[0]
    hv = [_host_vecs(inp, l) for l in range(DEPTH)]
    n = 8
    if FUSED:
        nc = _get_nc(DEPTH, 2, True)
        in_maps = []
        for i in range(n):
            cs = c[2 * i:2 * i + 2]
            cT = np.ascontiguousarray(cs.reshape(2, 8, 128).transpose(2, 1, 0))
            in_maps.append({
                "x": np.ascontiguousarray(x[2 * i:2 * i + 2]), "cT": cT,
                "vecs": np.stack([h[0] for h in hv]), "vec64": np.stack([h[1] for h in hv]),
                "gb": np.stack([h[2] for h in hv]),
                "w_ada": w_ada, "w_in": w_in, "b_in": np.ascontiguousarray(b_in_a.reshape(DEPTH, 1, DIN)),
                "w_out": w_out, "final_g": fg})
        res = run_bass_kernel_spmd(nc, in_maps, core_ids=list(range(n)))
        return np.concatenate([r["out"] for r in res.results], axis=0)
    cur = x.copy()
    for l in range(DEPTH):
        nc = _get_nc(1, 1, l == DEPTH - 1)
        for sidx in range(2):
            in_maps = []
            for i in range(n):
                b = 2 * i + sidx
                cT = np.ascontiguousarray(c[b:b + 1].reshape(1, 8, 128).transpose(2, 1, 0))
                in_maps.append({
                    "x": np.ascontiguousarray(cur[b:b + 1]), "cT": cT,
                    "vecs": hv[l][0][None], "vec64": hv[l][1][None], "gb": hv[l][2][None],
                    "w_ada": w_ada[l:l + 1], "w_in": w_in[l:l + 1],
                    "b_in": np.ascontiguousarray(b_in_a[l].reshape(1, 1, DIN)),
                    "w_out": w_out[l:l + 1], "final_g": fg})
            res = run_bass_kernel_spmd(nc, in_maps, core_ids=list(range(n)))
            for i in range(n):
                cur[2 * i + sidx] = res.results[i]["out"][0]
    return cur
```

```python
import math
import numpy as np
import concourse.bass as bass
import concourse.mybir as mybir
from concourse.bass_utils import run_bass_kernel_spmd
from contextlib import ExitStack

AF = mybir.ActivationFunctionType
ALU = mybir.AluOpType
F32 = mybir.dt.float32
BF16 = mybir.dt.bfloat16

ENGS = ["pe", "act", "dve", "pool", "sp"]
SEM_EPOCH = 30000
SAME_ENG_SYNC = True
STOP_AFTER = None

S = 2048
D = 1024
NHALF = 1024
DIN = 4364
C_MQ, C_MK, C_MV, C_MO, C_MZ, C_MI, C_MF, C_FQ, C_FK, C_FV, C_FZ, C_FF, C_CA, C_CG, C_CZ = (
    0, 512, 1024, 1536, 2048, 2560, 2564, 2568, 2824, 3080, 3336, 3592, 3596, 3852, 4108)
V_NG, V_BA, V_BQ, V_BK, V_BZ, V_BFQ, V_BFK, V_BCA, V_BCG, V_BCZ, V_CB, V_HG, V_DWB, V_LNG, V_LNB, V_CW, V_DW, NV = (
    0, 8, 32, 36, 40, 44, 46, 48, 50, 52, 54, 62, 66, 68, 70, 72, 104, 166)


class Op:
    __slots__ = ("eng", "idx", "fn", "dma", "deps", "signal", "sig", "dcount", "waits")

    def __init__(self, eng, idx, fn, dma):
        self.eng = eng
        self.idx = idx
        self.fn = fn
        self.dma = dma
        self.deps = None
        self.signal = False
        self.sig = None
        self.dcount = None
        self.waits = []


class _Call:
    __slots__ = ("name", "a", "kw")

    def __init__(self, name, a, kw):
        self.name = name
        self.a = a
        self.kw = kw

    def __call__(self, e):
        return getattr(e, self.name)(*self.a, **self.kw)


class _Rec:
    def __getattr__(self, name):
        return lambda *a, **kw: _Call(name, a, kw)


_REC = _Rec()


class Prog:
    def __init__(self, nc, stack):
        self.nc = nc
        self.stack = stack
        self.ops = {e: [] for e in ENGS}
        self.reg = {}
        self.dma_cnt = {}
        self.dma_sem = {}
        self.pend = {e: None for e in ENGS}

    def barrier(self):
        snap_c = {e: len(self.ops[e]) - 1 for e in ENGS if e != "sp" and len(self.ops[e]) > 0}
        snap_c = {e: i for e, i in snap_c.items()}
        snap_d = {k: c for k, c in self.dma_cnt.items() if not (isinstance(k, tuple) and k[0] == "wb")}
        for e in ENGS:
            self.pend[e] = (dict(snap_c), dict(snap_d))

    def op(self, eng, fn, r=(), w=(), dma=None):
        call = fn(_REC)
        assert isinstance(call, _Call), "op lambda must return e.<instr>(...)"
        fn = call
        o = Op(eng, len(self.ops[eng]), fn, dma)
        deps = []
        for k in r:
            st = self.reg.get(k)
            if st is not None and st[0] is not None:
                deps.append(st[0])
        for k in w:
            st = self.reg.get(k)
            if st is not None:
                if st[0] is not None:
                    deps.append(st[0])
                deps.extend(st[1])
        cdeps = {}
        ddeps = {}
        for d in deps:
            if d.dma is not None:
                ddeps[d.dma] = self.dma_cnt[d.dma]
            else:
                if d.eng == eng and (eng == "pe" or not SAME_ENG_SYNC):
                    continue
                if cdeps.get(d.eng, -1) < d.idx:
                    cdeps[d.eng] = d.idx
        pb = self.pend[eng]
        if pb is not None:
            self.pend[eng] = None
            for e2, i2 in pb[0].items():
                if e2 == eng:
                    continue
                tgt = self.ops[e2][i2]
                j = i2
                while j >= 0 and self.ops[e2][j].dma is not None:
                    j -= 1
                if j >= 0 and cdeps.get(e2, -1) < j:
                    cdeps[e2] = j
            for k, c in pb[1].items():
                if ddeps.get(k, 0) < c:
                    ddeps[k] = c
        o.deps = (cdeps, ddeps)
        for k in r:
            st = self.reg.get(k)
            if st is None:
                st = [None, []]
                self.reg[k] = st
            st[1].append(o)
        for k in w:
            self.reg[k] = [o, []]
        if dma is not None:
            self.dma_cnt[dma] = self.dma_cnt.get(dma, 0) + 16
            o.dcount = self.dma_cnt[dma]
        self.ops[eng].append(o)
        return o

    def finalize_and_emit(self):
        nc = self.nc
        for e in ENGS:
            for o in self.ops[e]:
                for de, di in o.deps[0].items():
                    self.ops[de][di].signal = True
        nsig = {}
        for e in ENGS:
            c = 0
            for o in self.ops[e]:
                if o.signal and o.dma is None:
                    o.sig = c
                    c += 1
            nsig[e] = c
        sems = {}
        for e in ENGS:
            n = (nsig[e] + SEM_EPOCH - 1) // SEM_EPOCH
            sems[e] = [self.stack.enter_context(nc.semaphore(f"s_{e}_{i}")) for i in range(n)]
        for k in self.dma_cnt:
            self.dma_sem[k] = self.stack.enter_context(nc.semaphore(f"d_{len(self.dma_sem)}"))
        for e in ENGS:
            seen = {}
            dseen = {}
            for o in self.ops[e]:
                for de, di in o.deps[0].items():
                    s = self.ops[de][di].sig
                    if seen.get(de, -1) >= s:
                        continue
                    seen[de] = s
                    o.waits.append((sems[de][s // SEM_EPOCH], s % SEM_EPOCH + 1))
                for k, cnt in o.deps[1].items():
                    if dseen.get(k, 0) >= cnt:
                        continue
                    dseen[k] = cnt
                    o.waits.append((self.dma_sem[k], cnt))
        blk = self.stack.enter_context(nc.Block())
        prog = self

        def emit(e, name):
            for o in prog.ops[name]:
                for (s, v) in o.waits:
                    e.wait_ge(s, v)
                ins = o.fn(e)
                if o.dma is not None:
                    ins.then_inc(prog.dma_sem[o.dma], 16)
                elif o.signal:
                    ins.then_inc(sems[name][o.sig // SEM_EPOCH], 1)
            for k, cnt in prog.dma_cnt.items():
                if any(o.dma == k for o in prog.ops[name]):
                    e.wait_ge(prog.dma_sem[k], cnt)

        @blk.tensor
        def _(e):
            emit(e, "pe")

        @blk.scalar
        def _(e):
            emit(e, "act")

        @blk.vector
        def _(e):
            emit(e, "dve")

        @blk.gpsimd
        def _(e):
            emit(e, "pool")

        @blk.sync
        def _(e):
            emit(e, "sp")


def build(L, NS, final_norm, dbg=False):
    nc = bass.Bass("TRN2", target_bir_lowering=False)

    def din(n, s, d=F32):
        return nc.dram_tensor(n, s, d, kind="ExternalInput").ap()

    x_d = din("x", [NS, S, D])
    cT_d = din("cT", [128, 8, NS])
    vecs_d = din("vecs", [L, 128, NV])
    vec64_d = din("vec64", [L, 64, 12])
    gb_d = din("gb", [L, 4, 3])
    wada_d = din("w_ada", [L, D, 3 * D])
    win_d = din("w_in", [L, D, DIN])
    bin_d = din("b_in", [L, 1, DIN])
    wout_d = din("w_out", [L, D, D])
    fg_d = din("final_g", [1, D])
    out_d = nc.dram_tensor("out", [NS, S, D], F32, kind="ExternalOutput").ap()
    cq_scr = nc.dram_tensor("cq_scr", [4, NHALF], BF16).ap()
    if dbg:
        d_h = nc.dram_tensor("d_h", [128, 8, NHALF], BF16, kind="ExternalOutput").ap()
        d_ya = nc.dram_tensor("d_ya", [128, 4, NHALF], BF16, kind="ExternalOutput").ap()
        d_yb = nc.dram_tensor("d_yb", [128, 2, NHALF], BF16, kind="ExternalOutput").ap()
        d_yc = nc.dram_tensor("d_yc", [128, 2, NHALF], BF16, kind="ExternalOutput").ap()
        d_q = nc.dram_tensor("d_q", [128, 2, NHALF], BF16, kind="ExternalOutput").ap()
        d_gt = nc.dram_tensor("d_gt", [128, 64], F32, kind="ExternalOutput").ap()
        d_O = nc.dram_tensor("d_O", [128, 512], F32, kind="ExternalOutput").ap()
        d_pt = nc.dram_tensor("d_pt", [128, 512], BF16, kind="ExternalOutput").ap()
        d_fq = nc.dram_tensor("d_fq", [128, 4, NHALF], BF16, kind="ExternalOutput").ap()
        d_fk = nc.dram_tensor("d_fk", [128, 4, S], BF16, kind="ExternalOutput").ap()
        d_fz = nc.dram_tensor("d_fz", [64, 4, NHALF], BF16, kind="ExternalOutput").ap()
        d_V = nc.dram_tensor("d_V", [128, 16 * 4 * 65], BF16, kind="ExternalOutput").ap()
        d_cs = nc.dram_tensor("d_cs", [128, 64], F32, kind="ExternalOutput").ap()

    with ExitStack() as st:
        P = Prog(nc, st)
        A = P.op

        def sb(n, s, d):
            return st.enter_context(nc.sbuf_tensor(n, s, d))

        xT = sb("xT", [128, 8, S], F32)
        hT = sb("hT", [128, 8, NHALF], BF16)
        yTa = sb("yTa", [128, 4, NHALF], BF16)
        yTb = sb("yTb", [128, 2, NHALF], BF16)
        yTc = sb("yTc", [128, 2, NHALF], BF16)
        NWB = 3
        wbuf = [sb(f"wbuf{i}", [128, 8, 512], BF16) for i in range(NWB)]
        fkT = sb("fkT", [128, 2, S], BF16)
        Vext = sb("Vext", [128, 16, 4, 65], BF16)
        ubuf = sb("ubuf", [128, 2, 30 + NHALF], BF16)
        Cst = sb("Cst", [128, 4, 130], F32)
        cumspT = sb("cumspT", [128, 16, 4], F32)
        cumq_bf = sb("cumq_bf", [4, NHALF], BF16)
        carry = sb("carry", [4, 2], F32)
        wtm = sb("wtm", [128, 8, 2, 4], F32)
        dbc = sb("dbc", [128, 4, 8], F32)
        halo = sb("halo", [128, 8, 4], BF16)
        ident_f = sb("ident_f", [128, 128], F32)
        ident_b = sb("ident_b", [128, 128], BF16)
        ones_b = sb("ones_b", [128, 128], BF16)
        maskT = sb("maskT", [128, 128], BF16)
        blk64 = sb("blk64", [128, 128], BF16)
        negmask = sb("negmask", [128, 128], BF16)
        selF = sb("selF", [4, 4, 128], F32)
        selB = sb("selB", [4, 4, 128], BF16)
        onesF = sb("onesF", [128, 64], F32)
        cst = sb("cst", [128, 8], F32)
        vecs = sb("vecs_sb", [128, NV], F32)
        vec64 = sb("vec64_sb", [64, 12], F32)
        gb = sb("gb_sb", [4, 4], F32)
        cact = sb("cact", [128, 8, NS], BF16)
        cin = sb("cin", [128, 8, NS], F32)
        modv = sb("modv", [128, L, 24, NS], F32)
        Gv = sb("Gv", [128, 8], F32)
        SCRB = 36 * 1024
        scr = sb("scr", [128, SCRB // 2], BF16)
        pb = [st.enter_context(nc.psum_tensor(f"pb{i}", [128, 512], F32)) for i in range(8)]

        def carve(off, npart, shape, dt):
            n = 1
            for v in shape:
                n *= v
            nb = n * (4 if dt == F32 else 2)
            assert off % 4 == 0 and off + nb <= SCRB, (off, nb)
            ap = scr[0:npart, off // 2:(off + nb) // 2]
            if dt == F32:
                ap = ap.bitcast(F32)
            if len(shape) == 2:
                ap = ap.rearrange("p (a b) -> p a b", b=shape[1])
            elif len(shape) == 3:
                ap = ap.rearrange("p (a b c) -> p a b c", b=shape[1], c=shape[2])
            return ap

        def pbb(i):
            return pb[i][:].bitcast(BF16)

        CE6, CE5, C1, CLN, C0 = 0, 1, 2, 3, 4
        for i, v in enumerate([1e-6, 1e-5, 1.0, -0.5 * math.log(128.0), 0.0]):
            A("dve", lambda e, i=i, v=v: e.memset(cst[:, i:i + 1], v), w=["cst"], r=["cst"])
        A("pool", lambda e: e.memset(ident_f[:], 0.0), w=["ident_f"])
        A("pool", lambda e: e.affine_select(out=ident_f[:], in_=ident_f[:], pattern=[[-1, 128]],
                                            compare_op=ALU.not_equal, fill=1.0, base=0, channel_multiplier=1),
          r=["ident_f"], w=["ident_f"])
        A("dve", lambda e: e.tensor_copy(out=ident_b[:], in_=ident_f[:]), r=["ident_f"], w=["ident_b"])
        A("dve", lambda e: e.memset(ones_b[:], 1.0), w=["ones_b"])
        A("pool", lambda e: e.memset(maskT[:], 1.0), w=["maskT"])
        A("pool", lambda e: e.affine_select(out=maskT[:], in_=maskT[:], pattern=[[1, 128]],
                                            compare_op=ALU.is_ge, fill=0.0, base=0, channel_multiplier=-1),
          r=["maskT"], w=["maskT"])
        A("pool", lambda e: e.memset(negmask[:], 0.0), w=["negmask"])
        A("pool", lambda e: e.affine_select(out=negmask[:], in_=negmask[:], pattern=[[1, 128]],
                                            compare_op=ALU.is_ge, fill=-30000.0, base=0, channel_multiplier=-1),
          r=["negmask"], w=["negmask"])
        A("dve", lambda e: e.memset(blk64[:], 0.0), w=["blk64"])
        A("dve", lambda e: e.memset(blk64[0:64, 0:64], 1.0 / 64), r=["blk64"], w=["blk64"])
        A("dve", lambda e: e.memset(blk64[64:128, 64:128], 1.0 / 64), r=["blk64"], w=["blk64"])
        A("pool", lambda e: e.memset(selF[:], 0.0), w=["selF"])
        A("pool", lambda e: e.affine_select(out=selF[:], in_=selF[:], pattern=[[1, 4], [0, 128]],
                                            compare_op=ALU.not_equal, fill=1.0, base=0, channel_multiplier=-1),
          r=["selF"], w=["selF"])
        A("dve", lambda e: e.tensor_copy(out=selB[:], in_=selF[:]), r=["selF"], w=["selB"])
        A("dve", lambda e: e.memset(onesF[:], 1.0), w=["onesF"])
        A("dve", lambda e: e.memset(halo[:], 0.0), w=["halo"])
        A("sp", lambda e: e.dma_start(out=cin[:], in_=cT_d[:, :, :]), w=["cin"], dma="cin")
        A("act", lambda e: e.activation(out=cact[:], in_=cin[:], func=AF.Silu), r=["cin"], w=["cact"])

        wstate = {"n": 0}

        def wload(src, segs):
            slot = wstate["n"] % NWB
            wstate["n"] += 1
            offs = []
            o = 0
            for (c0, ncol) in segs:
                A("pool", lambda e, o=o, c0=c0, ncol=ncol, slot=slot: e.dma_start(
                    out=wbuf[slot][:, :, o:o + ncol],
                    in_=src[:, c0:c0 + ncol].rearrange("(k p) n -> p k n", p=128)),
                  w=[("wb", slot)], r=[("wb", slot)], dma=("wb", slot))
                offs.append(o)
                o += ncol
            return slot, offs

        class Jobs:
            def __init__(self, lst):
                self.lst = lst
                self.i = 0
                self.q = []

            def get(self):
                while len(self.q) < len(self.lst) and len(self.q) < self.i + NWB:
                    self.q.append(wload(*self.lst[len(self.q)]))
                cur = self.q[self.i]
                self.i += 1
                return cur

        def load_x(s):
            P.barrier()
            xin = [carve(i * 4096, 128, [D], F32) for i in range(2)]
            for t in range(16):
                A("sp", lambda e, t=t: e.dma_start(out=xin[t % 2], in_=x_d[s, t * 128:(t + 1) * 128, :]),
                  w=[("xin", t % 2)], dma=("xin", t % 2))
                for hb in range(2):
                    bank = 2 * (t % 2) + hb
                    for c4 in range(4):
                        c = hb * 4 + c4
                        A("pe", lambda e, t=t, c=c, c4=c4, bank=bank: e.transpose(
                            out=pb[bank][:, c4 * 128:(c4 + 1) * 128], in_=xin[t % 2][:, c * 128:(c + 1) * 128],
                            identity=ident_f[:]), r=[("xin", t % 2), "ident_f"], w=[("pb", bank)])
                    eng = "act" if hb == 0 else "dve"
                    if eng == "act":
                        A("act", lambda e, t=t, hb=hb, bank=bank: e.copy(
                            out=xT[:, hb * 4:hb * 4 + 4, t * 128:(t + 1) * 128],
                            in_=pb[bank][:].rearrange("p (a b) -> p a b", b=128)),
                          r=[("pb", bank)], w=[("xT", t // 4)])
                    else:
                        A("dve", lambda e, t=t, hb=hb, bank=bank: e.tensor_copy(
                            out=xT[:, hb * 4:hb * 4 + 4, t * 128:(t + 1) * 128],
                            in_=pb[bank][:].rearrange("p (a b) -> p a b", b=128)),
                          r=[("pb", bank)], w=[("xT", t // 4)])

        def store_x(s):
            P.barrier()
            xo = [carve(i * 4096, 128, [D], F32) for i in range(2)]
            fgb = carve(8192, 128, [D], F32)
            junk = carve(12288, 128, [D], F32)
            stat = carve(16384, 128, [8], F32)
            if final_norm:
                A("sp", lambda e: e.dma_start(out=fgb, in_=fg_d.partition_broadcast(128)), w=["fgb"], dma="fgb")
            for t in range(16):
                for hb in range(2):
                    bank = 2 * (t % 2) + hb
                    for c4 in range(4):
                        c = hb * 4 + c4
                        A("pe", lambda e, t=t, c=c, c4=c4, bank=bank: e.transpose(
                            out=pb[bank][:, c4 * 128:(c4 + 1) * 128], in_=xT[:, c, t * 128:(t + 1) * 128],
                            identity=ident_f[:]), r=[("xT", t // 4), "ident_f"], w=[("pb", bank)])
                    if hb == 0:
                        A("act", lambda e, t=t, bank=bank: e.copy(out=xo[t % 2][:, 0:512], in_=pb[bank][:]),
                          r=[("pb", bank)], w=[("xo", t % 2)])
                    else:
                        A("dve", lambda e, t=t, bank=bank: e.tensor_copy(out=xo[t % 2][:, 512:1024], in_=pb[bank][:]),
                          r=[("pb", bank)], w=[("xo", t % 2)])
                if final_norm:
                    A("act", lambda e, t=t: e.activation(out=junk, in_=xo[t % 2], func=AF.Square,
                                                          accum_out=stat[:, 0:1]),
                      r=[("xo", t % 2)], w=["junk", "stat"])
                    A("act", lambda e: e.activation(out=stat[:, 1:2], in_=stat[:, 0:1], func=AF.Ln,
                                                    scale=1.0 / D, bias=cst[:, CE6:CE6 + 1]),
                      r=["stat", "cst"], w=["stat"])
                    A("act", lambda e: e.activation(out=stat[:, 2:3], in_=stat[:, 1:2], func=AF.Exp, scale=-0.5),
                      r=["stat"], w=["stat"])
                    A("dve", lambda e, t=t: e.scalar_tensor_tensor(out=xo[t % 2], in0=xo[t % 2], scalar=stat[:, 2:3],
                                                                   in1=fgb, op0=ALU.mult, op1=ALU.mult),
                      r=[("xo", t % 2), "stat", "fgb"], w=[("xo", t % 2)])
                A("sp", lambda e, t=t: e.dma_start(out=out_d[s, t * 128:(t + 1) * 128, :], in_=xo[t % 2]),
                  r=[("xo", t % 2)], dma=("xo", t % 2))

        def ada(l):
            P.barrier()
            jobs = Jobs([(wada_d[l], [(g * 512, 512)]) for g in range(6)])
            for g in range(6):
                slot, offs = jobs.get()
                for fc4 in range(4):
                    fc = g * 4 + fc4
                    for k in range(8):
                        A("pe", lambda e, slot=slot, fc4=fc4, fc=fc, k=k: e.matmul(
                            pb[4][:, fc * NS:(fc + 1) * NS], lhsT=wbuf[slot][:, k, fc4 * 128:(fc4 + 1) * 128],
                            rhs=cact[:, k, :], start=(k == 0), stop=(k == 7)),
                          r=[("wb", slot), "cact"], w=[("pb", 4)])
            A("dve", lambda e: e.tensor_tensor(
                out=modv[:, l, :, :], in0=pb[4][:, 0:24 * NS].rearrange("p (a b) -> p a b", b=NS),
                in1=vecs[:, V_BA:V_BA + 24].unsqueeze(2).to_broadcast([128, 24, NS]), op=ALU.add),
              r=[("pb", 4), "vecs"], w=["modv"])

        def load_vecs(l):
            P.barrier()
            A("sp", lambda e: e.dma_start(out=vecs[:], in_=vecs_d[l, :, :]), w=["vecs"], dma="vecs")
            A("sp", lambda e: e.dma_start(out=vec64[:], in_=vec64_d[l, :, :]), w=["vec64"], dma="vec64")
            A("sp", lambda e: e.dma_start(out=gb[:, 0:3], in_=gb_d[l, :, :]), w=["gb"], dma="gb")

        acc_state = {"n": 0}

        def next_acc():
            b = acc_state["n"] % 2
            acc_state["n"] += 1
            return b

        def proj_fm(slot, col0, M, evac, tts=(0, 1)):
            for tt in tts:
                bank = next_acc()
                for k in range(8):
                    A("pe", lambda e, k=k, tt=tt, bank=bank: e.matmul(
                        pb[bank][0:M, :], lhsT=wbuf[slot][:, k, col0:col0 + M], rhs=hT[:, k, tt * 512:(tt + 1) * 512],
                        start=(k == 0), stop=(k == 7)),
                      r=[("wb", slot), ("hT", tt)], w=[("pb", bank)])
                evac(bank, tt)

        def proj_tm(slot, col0, N, evac):
            for t8 in range(8):
                bank = next_acc()
                for k in range(8):
                    A("pe", lambda e, k=k, t8=t8, bank=bank: e.matmul(
                        pb[bank][:, 0:N], lhsT=hT[:, k, t8 * 128:(t8 + 1) * 128], rhs=wbuf[slot][:, k, col0:col0 + N],
                        start=(k == 0), stop=(k == 7)),
                      r=[("wb", slot), ("hT", t8 // 4)], w=[("pb", bank)])
                evac(bank, t8)

        def layer(l, s, first_layer_dbg):
            load_vecs(l)
            A("dve", lambda e: e.scalar_tensor_tensor(out=Gv[:], in0=modv[:, l, 8:16, s], scalar=1.0,
                                                      in1=vecs[:, V_NG:V_NG + 8], op0=ALU.add, op1=ALU.mult),
              r=["modv", "vecs"], w=["Gv"])
            A("dve", lambda e: e.memset(Cst[:], 0.0), w=["Cst"], r=["Cst"])
            A("dve", lambda e: e.memset(carry[:], 0.0), w=["carry"], r=["carry"])
            A("dve", lambda e: e.memset(halo[:], 0.0), w=["halo"], r=["halo"])
            A("dve", lambda e: e.memset(Vext[:, :, :, 64:65], 1.0), w=["Vext"], r=["Vext"])
            win = win_d[l]
            wout = wout_d[l]
            for half in range(2):
                t0 = half * NHALF
                joblist = [(win, [(C_MI, 8), (C_FF, 4)])]
                for hp in range(2):
                    joblist.append((win, [(C_MQ + hp * 256, 256), (C_MK + hp * 256, 256)]))
                    joblist.append((win, [(C_MZ + hp * 256, 256)]))
                    joblist.append((win, [(C_MV + hp * 256, 256), (C_MO + hp * 256, 256)]))
                joblist.append((win, [(C_FQ, 256), (C_FK, 256)]))
                joblist.append((win, [(C_FV, 256), (C_FZ, 256)]))
                joblist.append((win, [(C_CA, 256), (C_CG, 256)]))
                joblist.append((win, [(C_CZ, 256)]))
                joblist.append((wout, [(0, 512)]))
                joblist.append((wout, [(512, 512)]))
                jobs = Jobs(joblist)

                P.barrier()
                sq = carve(0, 128, [8, 512], BF16)
                tmpf = [carve(8192 + i * 2048, 128, [512], F32) for i in range(2)]
                lnv = carve(12288, 128, [512], F32)
                for tt in range(2):
                    tok = slice(t0 + tt * 512, t0 + (tt + 1) * 512)
                    for c in range(8):
                        A("act", lambda e, c=c, tok=tok: e.activation(out=sq[:, c, :], in_=xT[:, c, tok], func=AF.Square),
                          r=[("xT", (t0 + tt * 512) // 512)], w=[("sq", c)])
                        A("pe", lambda e, c=c: e.matmul(pb[4][:], lhsT=ones_b[:], rhs=sq[:, c, :],
                                                        start=(c == 0), stop=(c == 7)),
                          r=[("sq", c), "ones_b"], w=[("pb", 4)])
                    A("act", lambda e: e.activation(out=lnv, in_=pb[4][:], func=AF.Ln, scale=1.0 / D,
                                                    bias=cst[:, CE6:CE6 + 1]), r=[("pb", 4), "cst"], w=["lnv"])
                    A("act", lambda e: e.activation(out=pb[5][:], in_=lnv, func=AF.Exp, scale=-0.5),
                      r=["lnv"], w=[("pb", 5)])
                    for c in range(8):
                        A("dve", lambda e, c=c, tok=tok: e.scalar_tensor_tensor(
                            out=tmpf[c % 2], in0=xT[:, c, tok], scalar=Gv[:, c:c + 1], in1=pb[5][:],
                            op0=ALU.mult, op1=ALU.mult),
                          r=[("xT", (t0 + tt * 512) // 512), "Gv", ("pb", 5)], w=[("tmpf", c % 2)])
                        A("act", lambda e, c=c, tt=tt: e.activation(
                            out=hT[:, c, tt * 512:(tt + 1) * 512], in_=tmpf[c % 2], func=AF.Identity,
                            bias=modv[:, l, c, s:s + 1], scale=1.0),
                          r=[("tmpf", c % 2), "modv"], w=[("hT", tt)])
                if dbg and first_layer_dbg and half == 0:
                    A("sp", lambda e: e.dma_start(out=d_h[:, :, :], in_=hT[:]), r=[("hT", 0), ("hT", 1)], dma="dbg_h")

                if STOP_AFTER == "H":
                    continue
                P.barrier()
                g1 = carve(0, 4, [NHALF], F32)
                g2 = carve(4096, 4, [NHALF], F32)
                g3 = carve(8192, 4, [NHALF], F32)
                g4 = carve(12288, 4, [NHALF], F32)
                g5 = carve(16384, 4, [NHALF], F32)
                msk = carve(20480, 4, [NHALF], F32)
                dsm = carve(24576, 4, [8], F32)
                slot, offs = jobs.get()

                gname = {id(g1): "g1", id(g2): "g2", id(g3): "g3", id(g4): "g4", id(g5): "g5"}

                def gate_proj(gi, dst):
                    def ev(bank, tt):
                        A("act", lambda e, bank=bank, tt=tt: e.activation(
                            out=dst[:, tt * 512:(tt + 1) * 512], in_=pb[bank][0:4, :], func=AF.Identity,
                            bias=gb[:, gi:gi + 1], scale=1.0), r=[("pb", bank), "gb"], w=[("g", gname[id(dst)], tt)])
                    proj_fm(slot, 4 * gi, 4, ev)

                gate_proj(0, g1)
                gate_proj(1, g2)
                gate_proj(2, g4)
                gk = lambda t: [("g", gname[id(t)], 0), ("g", gname[id(t)], 1)]
                for gt in (g2, g4):
                    A("act", lambda e, gt=gt: e.activation(out=gt, in_=gt, func=AF.Exp, scale=-1.0), r=gk(gt), w=gk(gt))
                    A("act", lambda e, gt=gt: e.activation(out=gt, in_=gt, func=AF.Ln, bias=cst[0:4, C1:C1 + 1], scale=1.0),
                      r=gk(gt) + ["cst"], w=gk(gt))
                A("dve", lambda e: e.memset(msk, 1.0), w=["msk"])
                A("dve", lambda e: e.memset(msk.rearrange("p (a b) -> p a b", b=128)[:, :, 0:1], 0.0), r=["msk"], w=["msk"])
                A("dve", lambda e: e.tensor_tensor_scan(out=g3, data0=msk, data1=g2, initial=0.0,
                                                        op0=ALU.mult, op1=ALU.add), r=["msk"] + gk(g2), w=gk(g3))
                g3v = g3.rearrange("p (a b) -> p a b", b=128)
                A("act", lambda e: e.activation(out=dsm, in_=g3v[:, :, 127], func=AF.Exp, scale=-1.0), r=gk(g3), w=["dsm"])
                A("dve", lambda e: e.tensor_tensor(out=g2.rearrange("p (a b) -> p a b", b=128), in0=g3v,
                                                   in1=g3v[:, :, 127:128].to_broadcast([4, 8, 128]), op=ALU.subtract),
                  r=gk(g3), w=gk(g2))
                A("dve", lambda e: e.tensor_tensor(out=g1, in0=g1, in1=g2, op=ALU.add), r=gk(g1) + gk(g2), w=gk(g1))
                A("act", lambda e: e.activation(out=g1, in_=g1, func=AF.Exp, bias=cst[0:4, CLN:CLN + 1], scale=1.0),
                  r=gk(g1) + ["cst"], w=gk(g1))
                A("act", lambda e: e.activation(out=g2, in_=g2, func=AF.Exp), r=gk(g2), w=gk(g2))
                A("dve", lambda e: e.memset(msk, 1.0), r=["msk"] + gk(g3), w=["msk"])
                A("dve", lambda e: e.tensor_tensor_scan(out=g5, data0=msk, data1=g4, initial=carry[:, 0:1],
                                                        op0=ALU.mult, op1=ALU.add),
                  r=["msk", "carry"] + gk(g4), w=gk(g5))
                A("dve", lambda e: e.tensor_copy(out=carry[:, 0:1], in_=g5[:, NHALF - 1:NHALF]), r=gk(g5), w=["carry"])
                A("act", lambda e: e.activation(out=cumq_bf[:], in_=g5, func=AF.Identity, scale=-1.0),
                  r=gk(g5), w=["cumq_bf"])
                for t8 in range(8):
                    for qi, gt in enumerate((g1, g2)):
                        A("pe", lambda e, t8=t8, qi=qi, gt=gt: e.transpose(
                            out=pb[6][:, (t8 * 2 + qi) * 4:(t8 * 2 + qi) * 4 + 4], in_=gt[:, t8 * 128:(t8 + 1) * 128],
                            identity=ident_f[0:4, 0:4]), r=gk(gt) + ["ident_f"], w=[("pb", 6)])
                    A("pe", lambda e, t8=t8: e.transpose(
                        out=pb[7][:, t8 * 4:t8 * 4 + 4], in_=g5[:, t8 * 128:(t8 + 1) * 128],
                        identity=ident_f[0:4, 0:4]), r=gk(g5) + ["ident_f"], w=[("pb", 7)])
                A("dve", lambda e: e.tensor_copy(out=wtm[:].rearrange("p a b c -> p (a b c)"), in_=pb[6][:, 0:64]),
                  r=[("pb", 6)], w=["wtm"])
                A("dve", lambda e: e.tensor_copy(out=cumspT[:, half * 8:half * 8 + 8, :].rearrange("p a b -> p (a b)"),
                                                 in_=pb[7][:, 0:32]), r=[("pb", 7)], w=["cumspT"])
                for h in range(4):
                    A("pe", lambda e, h=h: e.matmul(pb[5][:, h * 8:(h + 1) * 8], lhsT=selF[:, h, :], rhs=dsm,
                                                    start=True, stop=True), r=["selF", "dsm"], w=[("pb", 5)])
                A("dve", lambda e: e.tensor_copy(out=dbc[:].rearrange("p a b -> p (a b)"), in_=pb[5][:, 0:32]),
                  r=[("pb", 5)], w=["dbc"])
                if dbg and first_layer_dbg and half == 0:
                    A("sp", lambda e: e.dma_start(out=d_gt[:, :], in_=wtm[:].rearrange("p a b c -> p (a b c)")),
                      r=["wtm"], dma="dbg_gt")

                if STOP_AFTER == "G":
                    continue
                for hp in range(2):
                    P.barrier()
                    qT = carve(0, 128, [2, NHALF], BF16)
                    kT = carve(4096, 128, [2, NHALF], BF16)
                    zT = carve(8192, 128, [2, NHALF], BF16)
                    vext = carve(12288, 128, [8, 2, 130], BF16)
                    osb = carve(16448, 128, [8, 256], BF16)
                    pre = [carve(20544 + i * 2064, 128, [3 + NHALF + 5], BF16) for i in range(2)]
                    dg4 = carve(24672, 128, [16, 128], BF16)
                    bbc = carve(28768, 128, [512], F32)
                    tbase = 30816
                    kp = [carve(tbase + i * 256, 128, [128], BF16) for i in range(2)]
                    spb = [carve(tbase + 512 + i * 256, 128, [128], BF16) for i in range(2)]
                    cbf = [carve(tbase + 1024 + i * 264, 128, [130], BF16) for i in range(2)]
                    hhf = [carve(tbase + 1552 + i * 512, 128, [128], F32) for i in range(2)]
                    hnb = [carve(tbase + 2576 + i * 256, 128, [128], BF16) for i in range(2)]
                    osig = carve(tbase + 3088, 128, [256], F32)
                    stt = [carve(tbase + 4112 + i * 64, 128, [16], F32) for i in range(2)]
                    for ci in range(4):
                        chunk = (0 if ci < 2 else 4) + hp * 2 + (ci % 2)
                        for j in range(4):
                            A("dve", lambda e, ci=ci, j=j, chunk=chunk: e.tensor_scalar(
                                out=dg4[:, ci * 4 + j, :], in0=ident_b[:],
                                scalar1=vecs[:, V_CW + j * 8 + chunk:V_CW + j * 8 + chunk + 1], scalar2=None,
                                op0=ALU.mult), r=["ident_b", "vecs"], w=[("dg4", ci)])
                    A("sp", lambda e, hp=hp: e.dma_start(out=bbc[:, 0:256],
                                                         in_=bin_d[l, :, C_MV + hp * 256:C_MV + hp * 256 + 256].partition_broadcast(128)),
                      w=["bbc"], r=["bbc"], dma="bbc")
                    A("sp", lambda e, hp=hp: e.dma_start(out=bbc[:, 256:512],
                                                         in_=bin_d[l, :, C_MO + hp * 256:C_MO + hp * 256 + 256].partition_broadcast(128)),
                      w=["bbc"], r=["bbc"], dma="bbc")
                    A("dve", lambda e: e.memset(vext, 1.0), w=["vext"], r=["vext"])
                    slot, offs = jobs.get()
                    for ci in range(4):
                        chunk = (0 if ci < 2 else 4) + hp * 2 + (ci % 2)
                        pr = pre[ci % 2]
                        dstT = qT if ci < 2 else kT
                        bcol = (V_BQ if ci < 2 else V_BK) + hp * 2 + (ci % 2)
                        A("act", lambda e, pr=pr, chunk=chunk: e.copy(out=pr[:, 0:3], in_=halo[:, chunk, 0:3]),
                          r=["halo"], w=[("pre", ci % 2, 0)])

                        def ev(bank, tt, pr=pr, bcol=bcol, ci=ci):
                            A("act", lambda e: e.activation(out=pr[:, 3 + tt * 512:3 + (tt + 1) * 512], in_=pb[bank][:],
                                                            func=AF.Identity, bias=vecs[:, bcol:bcol + 1], scale=1.0),
                              r=[("pb", bank), "vecs"], w=[("pre", ci % 2, tt)])
                        proj_fm(slot, ci * 128, 128, ev)
                        A("act", lambda e, pr=pr, chunk=chunk: e.copy(out=halo[:, chunk, 0:3], in_=pr[:, NHALF:NHALF + 3]),
                          r=[("pre", ci % 2, 1)], w=["halo"])
                        for tt in range(2):
                            bank = 2 + (tt % 2)
                            for j in range(4):
                                A("pe", lambda e, j=j, tt=tt, bank=bank, pr=pr, ci=ci: e.matmul(
                                    pb[bank][:], lhsT=dg4[:, ci * 4 + j, :], rhs=pr[:, tt * 512 + j:tt * 512 + j + 512],
                                    start=(j == 0), stop=(j == 3)),
                                  r=[("dg4", ci), ("pre", ci % 2, 0), ("pre", ci % 2, 1)], w=[("pb", bank)])
                            A("act", lambda e, tt=tt, bank=bank, dstT=dstT, ci=ci, chunk=chunk: e.activation(
                                out=dstT[:, ci % 2, tt * 512:(tt + 1) * 512], in_=pb[bank][:], func=AF.Silu,
                                bias=vecs[:, V_CB + chunk:V_CB + chunk + 1], scale=1.0),
                              r=[("pb", bank), "vecs"], w=[("qk", ci, tt)])
                    if dbg and first_layer_dbg and half == 0 and hp == 0:
                        A("sp", lambda e: e.dma_start(out=d_q[:, :, :], in_=qT), r=[("qk", 0, 0), ("qk", 0, 1), ("qk", 1, 0), ("qk", 1, 1)], dma="dbg_q")
                    slot, offs = jobs.get()
                    for i in range(2):
                        def ev(bank, tt, i=i):
                            A("act", lambda e: e.activation(out=zT[:, i, tt * 512:(tt + 1) * 512], in_=pb[bank][:],
                                                            func=AF.Silu, bias=vecs[:, V_BZ + hp * 2 + i:V_BZ + hp * 2 + i + 1],
                                                            scale=1.0), r=[("pb", bank), "vecs"], w=[("zT", i, tt)])
                        proj_fm(slot, i * 128, 128, ev)
                    slot, offs = jobs.get()

                    def ev_vo(bank, t8):
                        A("dve", lambda e: e.tensor_tensor(
                            out=vext[:, t8, :, 0:128], in0=pb[bank][:, 0:256].rearrange("p (a b) -> p a b", b=128),
                            in1=bbc[:, 0:256].rearrange("p (a b) -> p a b", b=128), op=ALU.add),
                          r=[("pb", bank), "bbc", "vext"], w=[("vext", t8)])
                        A("dve", lambda e: e.tensor_tensor(out=osig, in0=pb[bank][:, 256:512], in1=bbc[:, 256:512], op=ALU.add),
                          r=[("pb", bank), "bbc"], w=["osig"])
                        A("act", lambda e: e.activation(out=osb[:, t8, :], in_=osig, func=AF.Sigmoid),
                          r=["osig"], w=[("osb", t8)])
                    proj_tm(slot, 0, 512, ev_vo)
                    P.barrier()
                    def stA(it, hh, c8):
                        h = hp * 2 + hh
                        sset = it % 2
                        bT, bSt, bN, bC = sset, 2 + sset, 4 + sset, 6
                        tok = slice(c8 * 128, (c8 + 1) * 128)
                        wcol = wtm[:, c8, 0, h:h + 1]
                        dcol = dbc[:, h, c8:c8 + 1]
                        qk_r = [("qk", hh, c8 // 4), ("qk", 2 + hh, c8 // 4)]
                        tpv = pbb(bT)[:, 0:128]
                        stv = pb[bSt][:, 0:128]
                        A("pe", lambda e: e.transpose(out=tpv, in_=kT[:, hh, tok], identity=ident_b[:]),
                          r=qk_r + ["ident_b"], w=[("pb", bT)])
                        A("act", lambda e: e.activation(out=kp[sset], in_=tpv, func=AF.Identity, scale=wcol),
                          r=[("pb", bT), "wtm"], w=[("kp", sset)])
                        A("pe", lambda e: e.matmul(stv, lhsT=kT[:, hh, tok], rhs=qT[:, hh, tok], start=True, stop=True),
                          r=qk_r, w=[("pb", bSt)])
                        A("dve", lambda e: e.scalar_tensor_tensor(
                            out=spb[sset], in0=stv, scalar=wcol, in1=maskT[:], op0=ALU.mult, op1=ALU.mult),
                          r=[("pb", bSt), "wtm", "maskT"], w=[("spb", sset)])
                        A("dve", lambda e: e.tensor_scalar(
                            out=cbf[sset], in0=Cst[:, h, :], scalar1=dcol, scalar2=None, op0=ALU.mult),
                          r=["Cst", "dbc"], w=[("cbf", sset)])
                        A("pe", lambda e: e.matmul(
                            pb[bN][:, 0:130], lhsT=spb[sset], rhs=vext[:, c8, hh, :], start=True, stop=False),
                          r=[("spb", sset), ("vext", c8), "vext"], w=[("pb", bN)])
                        A("pe", lambda e: e.matmul(
                            pb[bN][:, 0:130], lhsT=qT[:, hh, tok], rhs=cbf[sset], start=False, stop=True),
                          r=qk_r + [("cbf", sset)], w=[("pb", bN)])
                        A("pe", lambda e: e.matmul(
                            pb[bC][:, 0:130], lhsT=kp[sset], rhs=vext[:, c8, hh, :], start=True, stop=True),
                          r=[("kp", sset), ("vext", c8), "vext"], w=[("pb", bC)])
                        A("dve", lambda e: e.scalar_tensor_tensor(
                            out=Cst[:, h, :], in0=Cst[:, h, :], scalar=dcol, in1=pb[bC][:, 0:130],
                            op0=ALU.mult, op1=ALU.add), r=["Cst", "dbc", ("pb", bC)], w=["Cst"])

                    def stB(it, hh, c8):
                        h = hp * 2 + hh
                        sset = it % 2
                        bN = 4 + sset
                        fcol = wtm[:, c8, 1, h:h + 1]
                        sv = stt[sset]
                        A("dve", lambda e: e.tensor_scalar(
                            out=sv[:, 12:13], in0=pb[bN][:, 128:129], scalar1=-1.0, scalar2=fcol,
                            op0=ALU.mult, op1=ALU.max), r=[("pb", bN), "wtm"], w=[("stt", sset)])
                        A("dve", lambda e: e.tensor_tensor(
                            out=sv[:, 0:1], in0=sv[:, 12:13], in1=pb[bN][:, 128:129], op=ALU.max),
                          r=[("pb", bN), ("stt", sset)], w=[("stt", sset)])
                        A("dve", lambda e: e.reciprocal(out=sv[:, 1:2], in_=sv[:, 0:1]),
                          r=[("stt", sset)], w=[("stt", sset)])
                        A("dve", lambda e: e.scalar_tensor_tensor(
                            out=hhf[sset], in0=pb[bN][:, 0:128], scalar=sv[:, 1:2], in1=osb[:, c8, hh * 128:(hh + 1) * 128],
                            op0=ALU.mult, op1=ALU.mult), r=[("pb", bN), ("stt", sset), ("osb", c8)], w=[("hhf", sset)])
                        A("dve", lambda e: e.bn_stats(out=sv[:, 2:8], in_=hhf[sset]),
                          r=[("hhf", sset)], w=[("stt", sset)])
                        A("dve", lambda e: e.bn_aggr(out=sv[:, 8:10], in_=sv[:, 2:8]),
                          r=[("stt", sset)], w=[("stt", sset)])
                        A("act", lambda e: e.activation(out=sv[:, 10:11], in_=sv[:, 9:10], func=AF.Ln,
                                                        bias=cst[:, CE5:CE5 + 1], scale=1.0),
                          r=[("stt", sset), "cst"], w=[("stt", sset)])
                        A("act", lambda e: e.activation(out=sv[:, 11:12], in_=sv[:, 10:11], func=AF.Exp, scale=-0.5),
                          r=[("stt", sset)], w=[("stt", sset)])

                    def stC(it, hh, c8):
                        sset = it % 2
                        sv = stt[sset]
                        tp2v = pbb(7)[:, 0:128]
                        A("dve", lambda e: e.tensor_scalar(
                            out=hnb[sset], in0=hhf[sset], scalar1=sv[:, 8:9], scalar2=sv[:, 11:12],
                            op0=ALU.subtract, op1=ALU.mult), r=[("hhf", sset), ("stt", sset)], w=[("hnb", sset)])
                        A("pe", lambda e: e.transpose(out=tp2v, in_=hnb[sset], identity=ident_b[:]),
                          r=[("hnb", sset), "ident_b"], w=[("pb", 7)])

                    def stD(it, hh, c8):
                        h = hp * 2 + hh
                        sset = it % 2
                        tok = slice(c8 * 128, (c8 + 1) * 128)
                        tp2v = pbb(7)[:, 0:128]
                        A("dve", lambda e: e.scalar_tensor_tensor(
                            out=yTa[:, h, tok], in0=tp2v, scalar=vecs[:, V_HG + h:V_HG + h + 1], in1=zT[:, hh, tok],
                            op0=ALU.mult, op1=ALU.mult),
                          r=[("pb", 7), "vecs", ("zT", hh, c8 // 4)], w=[("yTa", h)])

                    its = [(i, i // 8, i % 8) for i in range(16)]
                    for step in range(16 + 3):
                        if step < 16:
                            stA(*its[step])
                        if 0 <= step - 1 < 16:
                            stB(*its[step - 1])
                        if 0 <= step - 3 < 16:
                            stD(*its[step - 3])
                        if 0 <= step - 2 < 16:
                            stC(*its[step - 2])
                if dbg and first_layer_dbg and half == 0:
                    A("sp", lambda e: e.dma_start(out=d_ya[:, :, :], in_=yTa[:]), r=[("yTa", i) for i in range(4)], dma="dbg_ya")

                if STOP_AFTER == "A":
                    continue
                P.barrier()
                fqT = carve(0, 128, [2, NHALF], BF16)
                fzT = carve(8192, 64, [4, NHALF], BF16)
                ptb = [carve(16384 + i * 1024, 128, [512], BF16) for i in range(2)]
                rden = carve(18432, 128, [512], F32)
                t1 = carve(20480, 64, [512], F32)
                ytmp = carve(22528, 64, [512], BF16)
                bbf = carve(23552, 128, [256], F32)
                A("sp", lambda e: e.dma_start(out=bbf, in_=bin_d[l, :, C_FV:C_FV + 256].partition_broadcast(128)),
                  w=["bbf"], dma="bbf")
                slot, offs = jobs.get()
                for i in range(2):
                    def evq(bank, tt, i=i):
                        A("dve", lambda e: e.tensor_scalar(out=fqT[:, i, tt * 512:(tt + 1) * 512], in0=pb[bank][:],
                                                           scalar1=vecs[:, V_BFQ + i:V_BFQ + i + 1], scalar2=0.125,
                                                           op0=ALU.add, op1=ALU.mult),
                          r=[("pb", bank), "vecs"], w=[("fqT", tt)])
                    proj_fm(slot, i * 128, 128, evq)

                    def evk(bank, tt, i=i):
                        A("act", lambda e: e.activation(out=fkT[:, i, t0 + tt * 512:t0 + (tt + 1) * 512], in_=pb[bank][:],
                                                        func=AF.Identity, bias=vecs[:, V_BFK + i:V_BFK + i + 1], scale=1.0),
                          r=[("pb", bank), "vecs"], w=["fkT"])
                    proj_fm(slot, 256 + i * 128, 128, evk)
                slot, offs = jobs.get()

                def ev_fv(bank, t8):
                    A("dve", lambda e: e.tensor_tensor(
                        out=Vext[:, half * 8 + t8, :, 0:64], in0=pb[bank][:, 0:256].rearrange("p (a b) -> p a b", b=64),
                        in1=bbf.rearrange("p (a b) -> p a b", b=64), op=ALU.add),
                      r=[("pb", bank), "bbf"], w=["Vext"])
                proj_tm(slot, 0, 256, ev_fv)
                for h in range(4):
                    def evz(bank, tt, h=h):
                        A("act", lambda e: e.activation(out=fzT[:, h, tt * 512:(tt + 1) * 512], in_=pb[bank][0:64, :],
                                                        func=AF.Silu, bias=vec64[:, h:h + 1], scale=1.0),
                          r=[("pb", bank), "vec64"], w=[("fzT", tt)])
                    proj_fm(slot, 256 + h * 64, 64, evz)
                P.barrier()
                fblocks = []
                nO = 0
                for h in range(4):
                    for Q in range(2):
                        q0t = (t0 + Q * 512) // 128
                        nkb = q0t + 4
                        bO = 2 + (nO % 2)
                        nO += 1
                        for kb in range(nkb):
                            fblocks.append((len(fblocks), h, Q, kb, nkb, q0t, bO))

                def foxS(nS, h, Q, kb, nkb, q0t, bO):
                    kc, pbase = h // 2, (h % 2) * 64
                    j = kb - q0t
                    nc0 = max(0, j) * 128
                    N = 512 - nc0
                    bS = nS % 2
                    pt = ptb[nS % 2]
                    regs = [(0, N, False)] if j < 0 else ([(0, 128, True)] + ([(128, N, False)] if N > 128 else []))
                    for (ra, rb, dg) in regs:
                        qsr = slice(Q * 512 + nc0 + ra, Q * 512 + nc0 + rb)
                        A("pe", lambda e: e.matmul(
                            pb[bS][:, ra:rb], lhsT=fkT[pbase:pbase + 64, kc, kb * 128:(kb + 1) * 128],
                            rhs=fqT[pbase:pbase + 64, kc, qsr], start=True, stop=False),
                          r=["fkT", ("fqT", Q)], w=[("pb", bS)])
                        A("pe", lambda e: e.matmul(
                            pb[bS][:, ra:rb], lhsT=selB[:, h, :], rhs=cumq_bf[:, qsr], start=False, stop=(not dg)),
                          r=["selB", "cumq_bf"], w=[("pb", bS)])
                        if dg:
                            A("pe", lambda e: e.matmul(
                                pb[bS][:, ra:rb], lhsT=ident_b[:], rhs=negmask[:], start=False, stop=True),
                              r=["ident_b", "negmask"], w=[("pb", bS)])
                    A("act", lambda e: e.activation(
                        out=pt[:, 0:N], in_=pb[bS][:, 0:N], func=AF.Exp, bias=cumspT[:, kb, h:h + 1], scale=1.0),
                      r=[("pb", bS), "cumspT"], w=[("pt", bS)])

                def foxPV(nS, h, Q, kb, nkb, q0t, bO):
                    kc, pbase = h // 2, (h % 2) * 64
                    j = kb - q0t
                    nc0 = max(0, j) * 128
                    N = 512 - nc0
                    bS = nS % 2
                    pt = ptb[nS % 2]
                    A("pe", lambda e: e.matmul(
                        pb[bO][0:65, nc0:512], lhsT=Vext[:, kb, h, :], rhs=pt[:, 0:N],
                        start=(kb == 0), stop=(kb == nkb - 1)),
                      r=["Vext", ("pt", bS)], w=[("pb", bO)])
                    if kb != nkb - 1:
                        return
                    A("act", lambda e: e.copy(out=rden[0:1, :], in_=pb[bO][64:65, :]),
                      r=[("pb", bO)], w=["rden"])
                    A("dve", lambda e: e.reciprocal(out=rden[0:1, :], in_=rden[0:1, :]), r=["rden"], w=["rden"])
                    A("pe", lambda e: e.matmul(pb[4][0:64, :], lhsT=onesF[0:1, 0:64], rhs=rden[0:1, :],
                                               start=True, stop=True), r=["rden", "onesF"], w=[("pb", 4)])
                    A("dve", lambda e: e.tensor_tensor(
                        out=t1, in0=pb[bO][0:64, :], in1=fzT[:, h, Q * 512:(Q + 1) * 512], op=ALU.mult),
                      r=[("pb", bO), ("fzT", Q)], w=["t1"])
                    if pbase == 0:
                        A("dve", lambda e: e.tensor_tensor(
                            out=yTb[0:64, kc, Q * 512:(Q + 1) * 512], in0=t1, in1=pb[4][0:64, :], op=ALU.mult),
                          r=["t1", ("pb", 4)], w=[("yTb", kc)])
                    else:
                        A("dve", lambda e: e.tensor_tensor(out=ytmp, in0=t1, in1=pb[4][0:64, :], op=ALU.mult),
                          r=["t1", ("pb", 4)], w=["ytmp"])
                        A("act", lambda e: e.copy(out=yTb[64:128, kc, Q * 512:(Q + 1) * 512], in_=ytmp),
                          r=["ytmp"], w=[("yTb", kc)])

                foxS(*fblocks[0])
                for bi in range(len(fblocks)):
                    if bi + 1 < len(fblocks):
                        foxS(*fblocks[bi + 1])
                    foxPV(*fblocks[bi])
                if dbg and first_layer_dbg and half == 0:
                    A("sp", lambda e: e.dma_start(out=d_yb[:, :, :], in_=yTb[:]), r=[("yTb", 0), ("yTb", 1)], dma="dbg_yb")

                if STOP_AFTER == "B":
                    continue
                P.barrier()
                sg = [carve(i * 2048, 128, [512], F32) for i in range(2)]
                zcT = carve(4096, 128, [2, NHALF], BF16)
                dg31 = carve(8192, 128, [31, 128], BF16)
                yf = carve(16384, 128, [512], F32)
                ybf = carve(18432, 128, [512], BF16)
                ysq = carve(19456, 128, [512], BF16)
                msq = carve(20480, 128, [512], F32)
                var = carve(22528, 128, [512], F32)
                tt_ = carve(24576, 128, [512], F32)
                ss_ = carve(26624, 128, [512], F32)
                if half == 0:
                    A("dve", lambda e: e.memset(ubuf[:, :, 0:30], 0.0), w=["ubuf"], r=["ubuf"])
                else:
                    A("act", lambda e: e.copy(out=ubuf[:, :, 0:30], in_=ubuf[:, :, NHALF:NHALF + 30]), w=["ubuf"], r=["ubuf"])
                slot, offs = jobs.get()
                for c in range(2):
                    for tt in range(2):
                        bank = next_acc()
                        for k in range(8):
                            A("pe", lambda e, k=k, tt=tt, bank=bank, c=c: e.matmul(
                                pb[bank][:], lhsT=wbuf[slot][:, k, 256 + c * 128:256 + (c + 1) * 128],
                                rhs=hT[:, k, tt * 512:(tt + 1) * 512], start=(k == 0), stop=(k == 7)),
                              r=[("wb", slot), ("hT", tt)], w=[("pb", bank)])
                        A("act", lambda e, bank=bank, c=c, tt=tt: e.activation(
                            out=sg[tt % 2], in_=pb[bank][:], func=AF.Sigmoid, bias=vecs[:, V_BCG + c:V_BCG + c + 1], scale=1.0),
                          r=[("pb", bank), "vecs"], w=[("sg", tt % 2)])
                        bank = next_acc()
                        for k in range(8):
                            A("pe", lambda e, k=k, tt=tt, bank=bank, c=c: e.matmul(
                                pb[bank][:], lhsT=wbuf[slot][:, k, c * 128:(c + 1) * 128],
                                rhs=hT[:, k, tt * 512:(tt + 1) * 512], start=(k == 0), stop=(k == 7)),
                              r=[("wb", slot), ("hT", tt)], w=[("pb", bank)])
                        A("dve", lambda e, bank=bank, c=c, tt=tt: e.scalar_tensor_tensor(
                            out=ubuf[:, c, 30 + tt * 512:30 + (tt + 1) * 512], in0=pb[bank][:],
                            scalar=vecs[:, V_BCA + c:V_BCA + c + 1], in1=sg[tt % 2], op0=ALU.add, op1=ALU.mult),
                          r=[("pb", bank), "vecs", ("sg", tt % 2)], w=["ubuf"])
                slot, offs = jobs.get()
                for c in range(2):
                    def evcz(bank, tt, c=c):
                        A("act", lambda e: e.activation(out=zcT[:, c, tt * 512:(tt + 1) * 512], in_=pb[bank][:], func=AF.Silu,
                                                        bias=vecs[:, V_BCZ + c:V_BCZ + c + 1], scale=1.0),
                          r=[("pb", bank), "vecs"], w=[("zcT", c)])
                    proj_fm(slot, c * 128, 128, evcz)
                for c in range(2):
                    for j in range(31):
                        A("dve", lambda e, c=c, j=j: e.tensor_scalar(
                            out=dg31[:, j, :], in0=ident_b[:], scalar1=vecs[:, V_DW + j * 2 + c:V_DW + j * 2 + c + 1],
                            scalar2=None, op0=ALU.mult), r=["ident_b", "vecs"], w=["dg31"])
                    for tt in range(2):
                        bank = 2 + tt
                        for j in range(31):
                            A("pe", lambda e, c=c, j=j, tt=tt, bank=bank: e.matmul(
                                pb[bank][:], lhsT=dg31[:, j, :], rhs=ubuf[:, c, tt * 512 + j:tt * 512 + j + 512],
                                start=(j == 0), stop=(j == 30)), r=["dg31", "ubuf"], w=[("pb", bank)])
                        bcol = vecs[:, V_DWB + c:V_DWB + c + 1]
                        A("act", lambda e, bank=bank, bcol=bcol: e.activation(out=yf, in_=pb[bank][:], func=AF.Identity,
                                                                              bias=bcol, scale=1.0),
                          r=[("pb", bank), "vecs"], w=["yf"])
                        A("act", lambda e, bank=bank, bcol=bcol: e.activation(out=ysq, in_=pb[bank][:], func=AF.Square,
                                                                              bias=bcol, scale=1.0),
                          r=[("pb", bank), "vecs"], w=["ysq"])
                        A("dve", lambda e: e.tensor_copy(out=ybf, in_=yf), r=["yf"], w=["ybf"])
                        A("pe", lambda e: e.matmul(pb[4][:], lhsT=blk64[:], rhs=ybf, start=True, stop=True),
                          r=["blk64", "ybf"], w=[("pb", 4)])
                        A("pe", lambda e: e.matmul(pb[5][:], lhsT=blk64[:], rhs=ysq, start=True, stop=True),
                          r=["blk64", "ysq"], w=[("pb", 5)])
                        A("act", lambda e: e.activation(out=msq, in_=pb[4][:], func=AF.Square), r=[("pb", 4)], w=["msq"])
                        A("dve", lambda e: e.tensor_tensor(out=var, in0=pb[5][:], in1=msq, op=ALU.subtract),
                          r=[("pb", 5), "msq"], w=["var"])
                        A("act", lambda e: e.activation(out=var, in_=var, func=AF.Ln, bias=cst[:, CE5:CE5 + 1], scale=1.0),
                          r=["var", "cst"], w=["var"])
                        A("act", lambda e: e.activation(out=var, in_=var, func=AF.Exp, scale=-0.5), r=["var"], w=["var"])
                        A("dve", lambda e: e.tensor_tensor(out=tt_, in0=yf, in1=pb[4][:], op=ALU.subtract),
                          r=["yf", ("pb", 4)], w=["tt_"])
                        A("dve", lambda e: e.tensor_tensor(out=tt_, in0=tt_, in1=var, op=ALU.mult), r=["tt_", "var"], w=["tt_"])
                        A("act", lambda e, c=c: e.activation(out=ss_, in_=tt_, func=AF.Silu,
                                                             scale=vecs[:, V_LNG + c:V_LNG + c + 1],
                                                             bias=vecs[:, V_LNB + c:V_LNB + c + 1]),
                          r=["tt_", "vecs"], w=["ss_"])
                        A("dve", lambda e, c=c, tt=tt: e.tensor_tensor(out=yTc[:, c, tt * 512:(tt + 1) * 512], in0=ss_,
                                                                         in1=zcT[:, c, tt * 512:(tt + 1) * 512], op=ALU.mult),
                          r=["ss_", ("zcT", c)], w=[("yTc", c)])
                if dbg and first_layer_dbg and half == 0:
                    A("sp", lambda e: e.dma_start(out=d_yc[:, :, :], in_=yTc[:]), r=[("yTc", 0), ("yTc", 1)], dma="dbg_yc")

                if STOP_AFTER == "C":
                    continue
                P.barrier()
                ycat = [yTa[:, 0, :], yTa[:, 1, :], yTa[:, 2, :], yTa[:, 3, :], yTb[:, 0, :], yTb[:, 1, :],
                        yTc[:, 0, :], yTc[:, 1, :]]
                for jb in range(2):
                    slot, offs = jobs.get()
                    for d4 in range(4):
                        dc = jb * 4 + d4
                        for tt in range(2):
                            bank = next_acc()
                            for k in range(8):
                                A("pe", lambda e, k=k, tt=tt, bank=bank, d4=d4, slot=slot: e.matmul(
                                    pb[bank][:], lhsT=wbuf[slot][:, k, d4 * 128:(d4 + 1) * 128],
                                    rhs=ycat[k][:, tt * 512:(tt + 1) * 512], start=(k == 0), stop=(k == 7)),
                                  r=[("wb", slot), "ycat"], w=[("pb", bank)])
                            tok = slice(t0 + tt * 512, t0 + (tt + 1) * 512)
                            A("dve", lambda e, bank=bank, dc=dc, tok=tok: e.scalar_tensor_tensor(
                                out=xT[:, dc, tok], in0=pb[bank][:], scalar=modv[:, l, 16 + dc, s:s + 1], in1=xT[:, dc, tok],
                                op0=ALU.mult, op1=ALU.add),
                              r=[("pb", bank), "modv", ("xT", (t0 + tt * 512) // 512)], w=[("xT", (t0 + tt * 512) // 512)])

        for s in range(NS):
            load_x(s)
            for l in range(L):
                if s == 0:
                    load_vecs(l)
                    ada(l)
                layer(l, s, first_layer_dbg=(s == 0 and l == 0))
            store_x(s)
        P.finalize_and_emit()
    return nc


def _chunks(v):
    return np.ascontiguousarray(v.reshape(-1, 128).T)


def _host_vecs(inp, l):
    b_in = inp["b_in"][l]
    cols = [
        _chunks(inp["norm_g"][l]), _chunks(inp["b_ada"][l]),
        _chunks(b_in[C_MQ:C_MQ + 512]), _chunks(b_in[C_MK:C_MK + 512]), _chunks(b_in[C_MZ:C_MZ + 512]),
        _chunks(b_in[C_FQ:C_FQ + 256]), _chunks(b_in[C_FK:C_FK + 256]),
        _chunks(b_in[C_CA:C_CA + 256]), _chunks(b_in[C_CG:C_CG + 256]), _chunks(b_in[C_CZ:C_CZ + 256]),
        _chunks(inp["m_conv_b"][l]), _chunks(inp["m_hn_g"][l]),
        _chunks(inp["c_dw_b"][l]), _chunks(inp["c_ln_g"][l]), _chunks(inp["c_ln_b"][l]),
    ]
    cw = inp["m_conv_w"][l]
    cols.append(np.concatenate([_chunks(cw[j]) for j in range(4)], axis=1))
    dw = inp["c_dw_w"][l]
    cols.append(np.concatenate([_chunks(dw[j]) for j in range(31)], axis=1))
    v = np.concatenate(cols, axis=1).astype(np.float32)
    assert v.shape == (128, NV), v.shape
    vec64 = np.ascontiguousarray(np.concatenate([b_in[C_FZ:C_FZ + 256].reshape(4, 64).T, b_in[C_FQ:C_FQ + 256].reshape(4, 64).T,
                                                 b_in[C_FK:C_FK + 256].reshape(4, 64).T], axis=1)).astype(np.float32)
    gbv = np.stack([b_in[C_MI:C_MI + 4], b_in[C_MF:C_MF + 4], b_in[C_FF:C_FF + 4]], axis=1).astype(np.float32)
    return v, vec64, gbv


_NC_CACHE = {}


def _get_nc(L, NS, final_norm, dbg=False):
    key = (L, NS, final_norm, dbg)
    if key not in _NC_CACHE:
        _NC_CACHE[key] = build(L, NS, final_norm, dbg)
    return _NC_CACHE[key]


FUSED = True


def kernel(x, c, norm_g, w_ada, b_ada, w_in, b_in, m_conv_w, m_conv_b, m_hn_g,
           c_dw_w, c_dw_b, c_ln_g, c_ln_b, w_out, final_g):
    inp = dict(norm_g=np.asarray(norm_g), b_ada=np.asarray(b_ada), b_in=np.asarray(b_in),
               m_conv_w=np.asarray(m_conv_w), m_conv_b=np.asarray(m_conv_b), m_hn_g=np.asarray(m_hn_g),
               c_dw_w=np.asarray(c_dw_w), c_dw_b=np.asarray(c_dw_b), c_ln_g=np.asarray(c_ln_g),
               c_ln_b=np.asarray(c_ln_b))
    x = np.asarray(x, dtype=np.float32)
    c = np.asarray(c, dtype=np.float32)
    w_ada = np.asarray(w_ada, dtype=np.float32)
    w_in = np.asarray(w_in, dtype=np.float32)
    w_out = np.asarray(w_out, dtype=np.float32)
    b_in_a = np.asarray(b_in, dtype=np.float32)
    fg = np.asarray(final_g, dtype=np.float32).reshape(1, D)
    DEPTH = w_in.shape[0]
    hv = [_host_vecs(inp, l) for l in range(DEPTH)]
    n = 8
    if FUSED:
        nc = _get_nc(DEPTH, 2, True)
        in_maps = []
        for i in range(n):
            cs = c[2 * i:2 * i + 2]
            cT = np.ascontiguousarray(cs.reshape(2, 8, 128).transpose(2, 1, 0))
            in_maps.append({
                "x": np.ascontiguousarray(x[2 * i:2 * i + 2]), "cT": cT,
                "vecs": np.stack([h[0] for h in hv]), "vec64": np.stack([h[1] for h in hv]),
                "gb": np.stack([h[2] for h in hv]),
                "w_ada": w_ada, "w_in": w_in, "b_in": np.ascontiguousarray(b_in_a.reshape(DEPTH, 1, DIN)),
                "w_out": w_out, "final_g": fg})
        res = run_bass_kernel_spmd(nc, in_maps, core_ids=list(range(n)))
        return np.concatenate([r["out"] for r in res.results], axis=0)
    cur = x.copy()
    for l in range(DEPTH):
        nc = _get_nc(1, 1, l == DEPTH - 1)
        for sidx in range(2):
            in_maps = []
            for i in range(n):
                b = 2 * i + sidx
                cT = np.ascontiguousarray(c[b:b + 1].reshape(1, 8, 128).transpose(2, 1, 0))
                in_maps.append({
                    "x": np.ascontiguousarray(cur[b:b + 1]), "cT": cT,
                    "vecs": hv[l][0][None], "vec64": hv[l][1][None], "gb": hv[l][2][None],
                    "w_ada": w_ada[l:l + 1], "w_in": w_in[l:l + 1],
                    "b_in": np.ascontiguousarray(b_in_a[l].reshape(1, 1, DIN)),
                    "w_out": w_out[l:l + 1], "final_g": fg})
            res = run_bass_kernel_spmd(nc, in_maps, core_ids=list(range(n)))
            for i in range(n):
                cur[2 * i + sidx] = res.results[i]["out"][0]
    return cur
```

```python
import math
import numpy as np
import concourse.bass as bass
import concourse.mybir as mybir
from concourse.bass_utils import run_bass_kernel_spmd
from contextlib import ExitStack

AF = mybir.ActivationFunctionType
ALU = mybir.AluOpType
F32 = mybir.dt.float32
BF16 = mybir.dt.bfloat16

ENGS = ["pe", "act", "dve", "pool", "sp"]
SEM_EPOCH = 30000
SAME_ENG_SYNC = True
STOP_AFTER = None

S = 2048
D = 1024
NHALF = 1024
DIN = 4364
C_MQ, C_MK, C_MV, C_MO, C_MZ, C_MI, C_MF, C_FQ, C_FK, C_FV, C_FZ, C_FF, C_CA, C_CG, C_CZ = (
    0, 512, 1024, 1536, 2048, 2560, 2564, 2568, 2824, 3080, 3336, 3592, 3596, 3852, 4108)
V_NG, V_BA, V_BQ, V_BK, V_BZ, V_BFQ, V_BFK, V_BCA, V_BCG, V_BCZ, V_CB, V_HG, V_DWB, V_LNG, V_LNB, V_CW, V_DW, NV = (
    0, 8, 32, 36, 40, 44, 46, 48, 50, 52, 54, 62, 66, 68, 70, 72, 104, 166)


class Op:
    __slots__ = ("eng", "idx", "fn", "dma", "deps", "signal", "sig", "dcount", "waits")

    def __init__(self, eng, idx, fn, dma):
        self.eng = eng
        self.idx = idx
        self.fn = fn
        self.dma = dma
        self.deps = None
        self.signal = False
        self.sig = None
        self.dcount = None
        self.waits = []


class _Call:
    __slots__ = ("name", "a", "kw")

    def __init__(self, name, a, kw):
        self.name = name
        self.a = a
        self.kw = kw

    def __call__(self, e):
        return getattr(e, self.name)(*self.a, **self.kw)


class _Rec:
    def __getattr__(self, name):
        return lambda *a, **kw: _Call(name, a, kw)


_REC = _Rec()


class Prog:
    def __init__(self, nc, stack):
        self.nc = nc
        self.stack = stack
        self.ops = {e: [] for e in ENGS}
        self.reg = {}
        self.dma_cnt = {}
        self.dma_sem = {}
        self.pend = {e: None for e in ENGS}

    def barrier(self):
        snap_c = {e: len(self.ops[e]) - 1 for e in ENGS if e != "sp" and len(self.ops[e]) > 0}
        snap_c = {e: i for e, i in snap_c.items()}
        snap_d = {k: c for k, c in self.dma_cnt.items() if not (isinstance(k, tuple) and k[0] == "wb")}
        for e in ENGS:
            self.pend[e] = (dict(snap_c), dict(snap_d))

    def op(self, eng, fn, r=(), w=(), dma=None):
        call = fn(_REC)
        assert isinstance(call, _Call), "op lambda must return e.<instr>(...)"
        fn = call
        o = Op(eng, len(self.ops[eng]), fn, dma)
        deps = []
        for k in r:
            st = self.reg.get(k)
            if st is not None and st[0] is not None:
                deps.append(st[0])
        for k in w:
            st = self.reg.get(k)
            if st is not None:
                if st[0] is not None:
                    deps.append(st[0])
                deps.extend(st[1])
        cdeps = {}
        ddeps = {}
        for d in deps:
            if d.dma is not None:
                ddeps[d.dma] = self.dma_cnt[d.dma]
            else:
                if d.eng == eng and (eng == "pe" or not SAME_ENG_SYNC):
                    continue
                if cdeps.get(d.eng, -1) < d.idx:
                    cdeps[d.eng] = d.idx
        pb = self.pend[eng]
        if pb is not None:
            self.pend[eng] = None
            for e2, i2 in pb[0].items():
                if e2 == eng:
                    continue
                tgt = self.ops[e2][i2]
                j = i2
                while j >= 0 and self.ops[e2][j].dma is not None:
                    j -= 1
                if j >= 0 and cdeps.get(e2, -1) < j:
                    cdeps[e2] = j
            for k, c in pb[1].items():
                if ddeps.get(k, 0) < c:
                    ddeps[k] = c
        o.deps = (cdeps, ddeps)
        for k in r:
            st = self.reg.get(k)
            if st is None:
                st = [None, []]
                self.reg[k] = st
            st[1].append(o)
        for k in w:
            self.reg[k] = [o, []]
        if dma is not None:
            self.dma_cnt[dma] = self.dma_cnt.get(dma, 0) + 16
            o.dcount = self.dma_cnt[dma]
        self.ops[eng].append(o)
        return o

    def finalize_and_emit(self):
        nc = self.nc
        for e in ENGS:
            for o in self.ops[e]:
                for de, di in o.deps[0].items():
                    self.ops[de][di].signal = True
        nsig = {}
        for e in ENGS:
            c = 0
            for o in self.ops[e]:
                if o.signal and o.dma is None:
                    o.sig = c
                    c += 1
            nsig[e] = c
        sems = {}
        for e in ENGS:
            n = (nsig[e] + SEM_EPOCH - 1) // SEM_EPOCH
            sems[e] = [self.stack.enter_context(nc.semaphore(f"s_{e}_{i}")) for i in range(n)]
        for k in self.dma_cnt:
            self.dma_sem[k] = self.stack.enter_context(nc.semaphore(f"d_{len(self.dma_sem)}"))
        for e in ENGS:
            seen = {}
            dseen = {}
            for o in self.ops[e]:
                for de, di in o.deps[0].items():
                    s = self.ops[de][di].sig
                    if seen.get(de, -1) >= s:
                        continue
                    seen[de] = s
                    o.waits.append((sems[de][s // SEM_EPOCH], s % SEM_EPOCH + 1))
                for k, cnt in o.deps[1].items():
                    if dseen.get(k, 0) >= cnt:
                        continue
                    dseen[k] = cnt
                    o.waits.append((self.dma_sem[k], cnt))
        blk = self.stack.enter_context(nc.Block())
        prog = self

        def emit(e, name):
            for o in prog.ops[name]:
                for (s, v) in o.waits:
                    e.wait_ge(s, v)
                ins = o.fn(e)
                if o.dma is not None:
                    ins.then_inc(prog.dma_sem[o.dma], 16)
                elif o.signal:
                    ins.then_inc(sems[name][o.sig // SEM_EPOCH], 1)
            for k, cnt in prog.dma_cnt.items():
                if any(o.dma == k for o in prog.ops[name]):
                    e.wait_ge(prog.dma_sem[k], cnt)

        @blk.tensor
        def _(e):
            emit(e, "pe")

        @blk.scalar
        def _(e):
            emit(e, "act")

        @blk.vector
        def _(e):
            emit(e, "dve")

        @blk.gpsimd
        def _(e):
            emit(e, "pool")

        @blk.sync
        def _(e):
            emit(e, "sp")


def build(L, NS, final_norm, dbg=False):
    nc = bass.Bass("TRN2", target_bir_lowering=False)

    def din(n, s, d=F32):
        return nc.dram_tensor(n, s, d, kind="ExternalInput").ap()

    x_d = din("x", [NS, S, D])
    cT_d = din("cT", [128, 8, NS])
    vecs_d = din("vecs", [L, 128, NV])
    vec64_d = din("vec64", [L, 64, 12])
    gb_d = din("gb", [L, 4, 3])
    wada_d = din("w_ada", [L, D, 3 * D])
    win_d = din("w_in", [L, D, DIN])
    bin_d = din("b_in", [L, 1, DIN])
    wout_d = din("w_out", [L, D, D])
    fg_d = din("final_g", [1, D])
    out_d = nc.dram_tensor("out", [NS, S, D], F32, kind="ExternalOutput").ap()
    cq_scr = nc.dram_tensor("cq_scr", [4, NHALF], BF16).ap()
    if dbg:
        d_h = nc.dram_tensor("d_h", [128, 8, NHALF], BF16, kind="ExternalOutput").ap()
        d_ya = nc.dram_tensor("d_ya", [128, 4, NHALF], BF16, kind="ExternalOutput").ap()
        d_yb = nc.dram_tensor("d_yb", [128, 2, NHALF], BF16, kind="ExternalOutput").ap()
        d_yc = nc.dram_tensor("d_yc", [128, 2, NHALF], BF16, kind="ExternalOutput").ap()
        d_q = nc.dram_tensor("d_q", [128, 2, NHALF], BF16, kind="ExternalOutput").ap()
        d_gt = nc.dram_tensor("d_gt", [128, 64], F32, kind="ExternalOutput").ap()
        d_O = nc.dram_tensor("d_O", [128, 512], F32, kind="ExternalOutput").ap()
        d_pt = nc.dram_tensor("d_pt", [128, 512], BF16, kind="ExternalOutput").ap()
        d_fq = nc.dram_tensor("d_fq", [128, 4, NHALF], BF16, kind="ExternalOutput").ap()
        d_fk = nc.dram_tensor("d_fk", [128, 4, S], BF16, kind="ExternalOutput").ap()
        d_fz = nc.dram_tensor("d_fz", [64, 4, NHALF], BF16, kind="ExternalOutput").ap()
        d_V = nc.dram_tensor("d_V", [128, 16 * 4 * 65], BF16, kind="ExternalOutput").ap()
        d_cs = nc.dram_tensor("d_cs", [128, 64], F32, kind="ExternalOutput").ap()

    with ExitStack() as st:
        P = Prog(nc, st)
        A = P.op

        def sb(n, s, d):
            return st.enter_context(nc.sbuf_tensor(n, s, d))

        xT = sb("xT", [128, 8, S], F32)
        hT = sb("hT", [128, 8, NHALF], BF16)
        yTa = sb("yTa", [128, 4, NHALF], BF16)
        yTb = sb("yTb", [128, 2, NHALF], BF16)
        yTc = sb("yTc", [128, 2, NHALF], BF16)
        NWB = 3
        wbuf = [sb(f"wbuf{i}", [128, 8, 512], BF16) for i in range(NWB)]
        fkT = sb("fkT", [128, 2, S], BF16)
        Vext = sb("Vext", [128, 16, 4, 65], BF16)
        ubuf = sb("ubuf", [128, 2, 30 + NHALF], BF16)
        Cst = sb("Cst", [128, 4, 130], F32)
        cumspT = sb("cumspT", [128, 16, 4], F32)
        cumq_bf = sb("cumq_bf", [4, NHALF], BF16)
        carry = sb("carry", [4, 2], F32)
        wtm = sb("wtm", [128, 8, 2, 4], F32)
        dbc = sb("dbc", [128, 4, 8], F32)
        halo = sb("halo", [128, 8, 4], BF16)
        ident_f = sb("ident_f", [128, 128], F32)
        ident_b = sb("ident_b", [128, 128], BF16)
        ones_b = sb("ones_b", [128, 128], BF16)
        maskT = sb("maskT", [128, 128], BF16)
        blk64 = sb("blk64", [128, 128], BF16)
        negmask = sb("negmask", [128, 128], BF16)
        selF = sb("selF", [4, 4, 128], F32)
        selB = sb("selB", [4, 4, 128], BF16)
        onesF = sb("onesF", [128, 64], F32)
        cst = sb("cst", [128, 8], F32)
        vecs = sb("vecs_sb", [128, NV], F32)
        vec64 = sb("vec64_sb", [64, 12], F32)
        gb = sb("gb_sb", [4, 4], F32)
        cact = sb("cact", [128, 8, NS], BF16)
        cin = sb("cin", [128, 8, NS], F32)
        modv = sb("modv", [128, L, 24, NS], F32)
        Gv = sb("Gv", [128, 8], F32)
        SCRB = 36 * 1024
        scr = sb("scr", [128, SCRB // 2], BF16)
        pb = [st.enter_context(nc.psum_tensor(f"pb{i}", [128, 512], F32)) for i in range(8)]

        def carve(off, npart, shape, dt):
            n = 1
            for v in shape:
                n *= v
            nb = n * (4 if dt == F32 else 2)
            assert off % 4 == 0 and off + nb <= SCRB, (off, nb)
            ap = scr[0:npart, off // 2:(off + nb) // 2]
            if dt == F32:
                ap = ap.bitcast(F32)
            if len(shape) == 2:
                ap = ap.rearrange("p (a b) -> p a b", b=shape[1])
            elif len(shape) == 3:
                ap = ap.rearrange("p (a b c) -> p a b c", b=shape[1], c=shape[2])
            return ap

        def pbb(i):
            return pb[i][:].bitcast(BF16)

        CE6, CE5, C1, CLN, C0 = 0, 1, 2, 3, 4
        for i, v in enumerate([1e-6, 1e-5, 1.0, -0.5 * math.log(128.0), 0.0]):
            A("dve", lambda e, i=i, v=v: e.memset(cst[:, i:i + 1], v), w=["cst"], r=["cst"])
        A("pool", lambda e: e.memset(ident_f[:], 0.0), w=["ident_f"])
        A("pool", lambda e: e.affine_select(out=ident_f[:], in_=ident_f[:], pattern=[[-1, 128]],
                                            compare_op=ALU.not_equal, fill=1.0, base=0, channel_multiplier=1),
          r=["ident_f"], w=["ident_f"])
        A("dve", lambda e: e.tensor_copy(out=ident_b[:], in_=ident_f[:]), r=["ident_f"], w=["ident_b"])
        A("dve", lambda e: e.memset(ones_b[:], 1.0), w=["ones_b"])
        A("pool", lambda e: e.memset(maskT[:], 1.0), w=["maskT"])
        A("pool", lambda e: e.affine_select(out=maskT[:], in_=maskT[:], pattern=[[1, 128]],
                                            compare_op=ALU.is_ge, fill=0.0, base=0, channel_multiplier=-1),
          r=["maskT"], w=["maskT"])
        A("pool", lambda e: e.memset(negmask[:], 0.0), w=["negmask"])
        A("pool", lambda e: e.affine_select(out=negmask[:], in_=negmask[:], pattern=[[1, 128]],
                                            compare_op=ALU.is_ge, fill=-30000.0, base=0, channel_multiplier=-1),
          r=["negmask"], w=["negmask"])
        A("dve", lambda e: e.memset(blk64[:], 0.0), w=["blk64"])
        A("dve", lambda e: e.memset(blk64[0:64, 0:64], 1.0 / 64), r=["blk64"], w=["blk64"])
        A("dve", lambda e: e.memset(blk64[64:128, 64:128], 1.0 / 64), r=["blk64"], w=["blk64"])
        A("pool", lambda e: e.memset(selF[:], 0.0), w=["selF"])
        A("pool", lambda e: e.affine_select(out=selF[:], in_=selF[:], pattern=[[1, 4], [0, 128]],
                                            compare_op=ALU.not_equal, fill=1.0, base=0, channel_multiplier=-1),
          r=["selF"], w=["selF"])
        A("dve", lambda e: e.tensor_copy(out=selB[:], in_=selF[:]), r=["selF"], w=["selB"])
        A("dve", lambda e: e.memset(onesF[:], 1.0), w=["onesF"])
        A("dve", lambda e: e.memset(halo[:], 0.0), w=["halo"])
        A("sp", lambda e: e.dma_start(out=cin[:], in_=cT_d[:, :, :]), w=["cin"], dma="cin")
        A("act", lambda e: e.activation(out=cact[:], in_=cin[:], func=AF.Silu), r=["cin"], w=["cact"])

        wstate = {"n": 0}

        def wload(src, segs):
            slot = wstate["n"] % NWB
            wstate["n"] += 1
            offs = []
            o = 0
            for (c0, ncol) in segs:
                A("pool", lambda e, o=o, c0=c0, ncol=ncol, slot=slot: e.dma_start(
                    out=wbuf[slot][:, :, o:o + ncol],
                    in_=src[:, c0:c0 + ncol].rearrange("(k p) n -> p k n", p=128)),
                  w=[("wb", slot)], r=[("wb", slot)], dma=("wb", slot))
                offs.append(o)
                o += ncol
            return slot, offs

        class Jobs:
            def __init__(self, lst):
                self.lst = lst
                self.i = 0
                self.q = []

            def get(self):
                while len(self.q) < len(self.lst) and len(self.q) < self.i + NWB:
                    self.q.append(wload(*self.lst[len(self.q)]))
                cur = self.q[self.i]
                self.i += 1
                return cur

        def load_x(s):
            P.barrier()
            xin = [carve(i * 4096, 128, [D], F32) for i in range(2)]
            for t in range(16):
                A("sp", lambda e, t=t: e.dma_start(out=xin[t % 2], in_=x_d[s, t * 128:(t + 1) * 128, :]),
                  w=[("xin", t % 2)], dma=("xin", t % 2))
                for hb in range(2):
                    bank = 2 * (t % 2) + hb
                    for c4 in range(4):
                        c = hb * 4 + c4
                        A("pe", lambda e, t=t, c=c, c4=c4, bank=bank: e.transpose(
                            out=pb[bank][:, c4 * 128:(c4 + 1) * 128], in_=xin[t % 2][:, c * 128:(c + 1) * 128],
                            identity=ident_f[:]), r=[("xin", t % 2), "ident_f"], w=[("pb", bank)])
                    eng = "act" if hb == 0 else "dve"
                    if eng == "act":
                        A("act", lambda e, t=t, hb=hb, bank=bank: e.copy(
                            out=xT[:, hb * 4:hb * 4 + 4, t * 128:(t + 1) * 128],
                            in_=pb[bank][:].rearrange("p (a b) -> p a b", b=128)),
                          r=[("pb", bank)], w=[("xT", t // 4)])
                    else:
                        A("dve", lambda e, t=t, hb=hb, bank=bank: e.tensor_copy(
                            out=xT[:, hb * 4:hb * 4 + 4, t * 128:(t + 1) * 128],
                            in_=pb[bank][:].rearrange("p (a b) -> p a b", b=128)),
                          r=[("pb", bank)], w=[("xT", t // 4)])

        def store_x(s):
            P.barrier()
            xo = [carve(i * 4096, 128, [D], F32) for i in range(2)]
            fgb = carve(8192, 128, [D], F32)
            junk = carve(12288, 128, [D], F32)
            stat = carve(16384, 128, [8], F32)
            if final_norm:
                A("sp", lambda e: e.dma_start(out=fgb, in_=fg_d.partition_broadcast(128)), w=["fgb"], dma="fgb")
            for t in range(16):
                for hb in range(2):
                    bank = 2 * (t % 2) + hb
                    for c4 in range(4):
                        c = hb * 4 + c4
                        A("pe", lambda e, t=t, c=c, c4=c4, bank=bank: e.transpose(
                            out=pb[bank][:, c4 * 128:(c4 + 1) * 128], in_=xT[:, c, t * 128:(t + 1) * 128],
                            identity=ident_f[:]), r=[("xT", t // 4), "ident_f"], w=[("pb", bank)])
                    if hb == 0:
                        A("act", lambda e, t=t, bank=bank: e.copy(out=xo[t % 2][:, 0:512], in_=pb[bank][:]),
                          r=[("pb", bank)], w=[("xo", t % 2)])
                    else:
                        A("dve", lambda e, t=t, bank=bank: e.tensor_copy(out=xo[t % 2][:, 512:1024], in_=pb[bank][:]),
                          r=[("pb", bank)], w=[("xo", t % 2)])
                if final_norm:
                    A("act", lambda e, t=t: e.activation(out=junk, in_=xo[t % 2], func=AF.Square,
                                                          accum_out=stat[:, 0:1]),
                      r=[("xo", t % 2)], w=["junk", "stat"])
                    A("act", lambda e: e.activation(out=stat[:, 1:2], in_=stat[:, 0:1], func=AF.Ln,
                                                    scale=1.0 / D, bias=cst[:, CE6:CE6 + 1]),
                      r=["stat", "cst"], w=["stat"])
                    A("act", lambda e: e.activation(out=stat[:, 2:3], in_=stat[:, 1:2], func=AF.Exp, scale=-0.5),
                      r=["stat"], w=["stat"])
                    A("dve", lambda e, t=t: e.scalar_tensor_tensor(out=xo[t % 2], in0=xo[t % 2], scalar=stat[:, 2:3],
                                                                   in1=fgb, op0=ALU.mult, op1=ALU.mult),
                      r=[("xo", t % 2), "stat", "fgb"], w=[("xo", t % 2)])
                A("sp", lambda e, t=t: e.dma_start(out=out_d[s, t * 128:(t + 1) * 128, :], in_=xo[t % 2]),
                  r=[("xo", t % 2)], dma=("xo", t % 2))

        def ada(l):
            P.barrier()
            jobs = Jobs([(wada_d[l], [(g * 512, 512)]) for g in range(6)])
            for g in range(6):
                slot, offs = jobs.get()
                for fc4 in range(4):
                    fc = g * 4 + fc4
                    for k in range(8):
                        A("pe", lambda e, slot=slot, fc4=fc4, fc=fc, k=k: e.matmul(
                            pb[4][:, fc * NS:(fc + 1) * NS], lhsT=wbuf[slot][:, k, fc4 * 128:(fc4 + 1) * 128],
                            rhs=cact[:, k, :], start=(k == 0), stop=(k == 7)),
                          r=[("wb", slot), "cact"], w=[("pb", 4)])
            A("dve", lambda e: e.tensor_tensor(
                out=modv[:, l, :, :], in0=pb[4][:, 0:24 * NS].rearrange("p (a b) -> p a b", b=NS),
                in1=vecs[:, V_BA:V_BA + 24].unsqueeze(2).to_broadcast([128, 24, NS]), op=ALU.add),
              r=[("pb", 4), "vecs"], w=["modv"])

        def load_vecs(l):
            P.barrier()
            A("sp", lambda e: e.dma_start(out=vecs[:], in_=vecs_d[l, :, :]), w=["vecs"], dma="vecs")
            A("sp", lambda e: e.dma_start(out=vec64[:], in_=vec64_d[l, :, :]), w=["vec64"], dma="vec64")
            A("sp", lambda e: e.dma_start(out=gb[:, 0:3], in_=gb_d[l, :, :]), w=["gb"], dma="gb")

        acc_state = {"n": 0}

        def next_acc():
            b = acc_state["n"] % 2
            acc_state["n"] += 1
            return b

        def proj_fm(slot, col0, M, evac, tts=(0, 1)):
            for tt in tts:
                bank = next_acc()
                for k in range(8):
                    A("pe", lambda e, k=k, tt=tt, bank=bank: e.matmul(
                        pb[bank][0:M, :], lhsT=wbuf[slot][:, k, col0:col0 + M], rhs=hT[:, k, tt * 512:(tt + 1) * 512],
                        start=(k == 0), stop=(k == 7)),
                      r=[("wb", slot), ("hT", tt)], w=[("pb", bank)])
                evac(bank, tt)

        def proj_tm(slot, col0, N, evac):
            for t8 in range(8):
                bank = next_acc()
                for k in range(8):
                    A("pe", lambda e, k=k, t8=t8, bank=bank: e.matmul(
                        pb[bank][:, 0:N], lhsT=hT[:, k, t8 * 128:(t8 + 1) * 128], rhs=wbuf[slot][:, k, col0:col0 + N],
                        start=(k == 0), stop=(k == 7)),
                      r=[("wb", slot), ("hT", t8 // 4)], w=[("pb", bank)])
                evac(bank, t8)

        def layer(l, s, first_layer_dbg):
            load_vecs(l)
            A("dve", lambda e: e.scalar_tensor_tensor(out=Gv[:], in0=modv[:, l, 8:16, s], scalar=1.0,
                                                      in1=vecs[:, V_NG:V_NG + 8], op0=ALU.add, op1=ALU.mult),
              r=["modv", "vecs"], w=["Gv"])
            A("dve", lambda e: e.memset(Cst[:], 0.0), w=["Cst"], r=["Cst"])
            A("dve", lambda e: e.memset(carry[:], 0.0), w=["carry"], r=["carry"])
            A("dve", lambda e: e.memset(halo[:], 0.0), w=["halo"], r=["halo"])
            A("dve", lambda e: e.memset(Vext[:, :, :, 64:65], 1.0), w=["Vext"], r=["Vext"])
            win = win_d[l]
            wout = wout_d[l]
            for half in range(2):
                t0 = half * NHALF
                joblist = [(win, [(C_MI, 8), (C_FF, 4)])]
                for hp in range(2):
                    joblist.append((win, [(C_MQ + hp * 256, 256), (C_MK + hp * 256, 256)]))
                    joblist.append((win, [(C_MZ + hp * 256, 256)]))
                    joblist.append((win, [(C_MV + hp * 256, 256), (C_MO + hp * 256, 256)]))
                joblist.append((win, [(C_FQ, 256), (C_FK, 256)]))
                joblist.append((win, [(C_FV, 256), (C_FZ, 256)]))
                joblist.append((win, [(C_CA, 256), (C_CG, 256)]))
                joblist.append((win, [(C_CZ, 256)]))
                joblist.append((wout, [(0, 512)]))
                joblist.append((wout, [(512, 512)]))
                jobs = Jobs(joblist)

                P.barrier()
                sq = carve(0, 128, [8, 512], BF16)
                tmpf = [carve(8192 + i * 2048, 128, [512], F32) for i in range(2)]
                lnv = carve(12288, 128, [512], F32)
                for tt in range(2):
                    tok = slice(t0 + tt * 512, t0 + (tt + 1) * 512)
                    for c in range(8):
                        A("act", lambda e, c=c, tok=tok: e.activation(out=sq[:, c, :], in_=xT[:, c, tok], func=AF.Square),
                          r=[("xT", (t0 + tt * 512) // 512)], w=[("sq", c)])
                        A("pe", lambda e, c=c: e.matmul(pb[4][:], lhsT=ones_b[:], rhs=sq[:, c, :],
                                                        start=(c == 0), stop=(c == 7)),
                          r=[("sq", c), "ones_b"], w=[("pb", 4)])
                    A("act", lambda e: e.activation(out=lnv, in_=pb[4][:], func=AF.Ln, scale=1.0 / D,
                                                    bias=cst[:, CE6:CE6 + 1]), r=[("pb", 4), "cst"], w=["lnv"])
                    A("act", lambda e: e.activation(out=pb[5][:], in_=lnv, func=AF.Exp, scale=-0.5),
                      r=["lnv"], w=[("pb", 5)])
                    for c in range(8):
                        A("dve", lambda e, c=c, tok=tok: e.scalar_tensor_tensor(
                            out=tmpf[c % 2], in0=xT[:, c, tok], scalar=Gv[:, c:c + 1], in1=pb[5][:],
                            op0=ALU.mult, op1=ALU.mult),
                          r=[("xT", (t0 + tt * 512) // 512), "Gv", ("pb", 5)], w=[("tmpf", c % 2)])
                        A("act", lambda e, c=c, tt=tt: e.activation(
                            out=hT[:, c, tt * 512:(tt + 1) * 512], in_=tmpf[c % 2], func=AF.Identity,
                            bias=modv[:, l, c, s:s + 1], scale=1.0),
                          r=[("tmpf", c % 2), "modv"], w=[("hT", tt)])
                if dbg and first_layer_dbg and half == 0:
                    A("sp", lambda e: e.dma_start(out=d_h[:, :, :], in_=hT[:]), r=[("hT", 0), ("hT", 1)], dma="dbg_h")

                if STOP_AFTER == "H":
                    continue
                P.barrier()
                g1 = carve(0, 4, [NHALF], F32)
                g2 = carve(4096, 4, [NHALF], F32)
                g3 = carve(8192, 4, [NHALF], F32)
                g4 = carve(12288, 4, [NHALF], F32)
                g5 = carve(16384, 4, [NHALF], F32)
                msk = carve(20480, 4, [NHALF], F32)
                dsm = carve(24576, 4, [8], F32)
                slot, offs = jobs.get()

                gname = {id(g1): "g1", id(g2): "g2", id(g3): "g3", id(g4): "g4", id(g5): "g5"}

                def gate_proj(gi, dst):
                    def ev(bank, tt):
                        A("act", lambda e, bank=bank, tt=tt: e.activation(
                            out=dst[:, tt * 512:(tt + 1) * 512], in_=pb[bank][0:4, :], func=AF.Identity,
                            bias=gb[:, gi:gi + 1], scale=1.0), r=[("pb", bank), "gb"], w=[("g", gname[id(dst)], tt)])
                    proj_fm(slot, 4 * gi, 4, ev)

                gate_proj(0, g1)
                gate_proj(1, g2)
                gate_proj(2, g4)
                gk = lambda t: [("g", gname[id(t)], 0), ("g", gname[id(t)], 1)]
                for gt in (g2, g4):
                    A("act", lambda e, gt=gt: e.activation(out=gt, in_=gt, func=AF.Exp, scale=-1.0), r=gk(gt), w=gk(gt))
                    A("act", lambda e, gt=gt: e.activation(out=gt, in_=gt, func=AF.Ln, bias=cst[0:4, C1:C1 + 1], scale=1.0),
                      r=gk(gt) + ["cst"], w=gk(gt))
                A("dve", lambda e: e.memset(msk, 1.0), w=["msk"])
                A("dve", lambda e: e.memset(msk.rearrange("p (a b) -> p a b", b=128)[:, :, 0:1], 0.0), r=["msk"], w=["msk"])
                A("dve", lambda e: e.tensor_tensor_scan(out=g3, data0=msk, data1=g2, initial=0.0,
                                                        op0=ALU.mult, op1=ALU.add), r=["msk"] + gk(g2), w=gk(g3))
                g3v = g3.rearrange("p (a b) -> p a b", b=128)
                A("act", lambda e: e.activation(out=dsm, in_=g3v[:, :, 127], func=AF.Exp, scale=-1.0), r=gk(g3), w=["dsm"])
                A("dve", lambda e: e.tensor_tensor(out=g2.rearrange("p (a b) -> p a b", b=128), in0=g3v,
                                                   in1=g3v[:, :, 127:128].to_broadcast([4, 8, 128]), op=ALU.subtract),
                  r=gk(g3), w=gk(g2))
                A("dve", lambda e: e.tensor_tensor(out=g1, in0=g1, in1=g2, op=ALU.add), r=gk(g1) + gk(g2), w=gk(g1))
                A("act", lambda e: e.activation(out=g1, in_=g1, func=AF.Exp, bias=cst[0:4, CLN:CLN + 1], scale=1.0),
                  r=gk(g1) + ["cst"], w=gk(g1))
                A("act", lambda e: e.activation(out=g2, in_=g2, func=AF.Exp), r=gk(g2), w=gk(g2))
                A("dve", lambda e: e.memset(msk, 1.0), r=["msk"] + gk(g3), w=["msk"])
                A("dve", lambda e: e.tensor_tensor_scan(out=g5, data0=msk, data1=g4, initial=carry[:, 0:1],
                                                        op0=ALU.mult, op1=ALU.add),
                  r=["msk", "carry"] + gk(g4), w=gk(g5))
                A("dve", lambda e: e.tensor_copy(out=carry[:, 0:1], in_=g5[:, NHALF - 1:NHALF]), r=gk(g5), w=["carry"])
                A("act", lambda e: e.activation(out=cumq_bf[:], in_=g5, func=AF.Identity, scale=-1.0),
                  r=gk(g5), w=["cumq_bf"])
                for t8 in range(8):
                    for qi, gt in enumerate((g1, g2)):
                        A("pe", lambda e, t8=t8, qi=qi, gt=gt: e.transpose(
                            out=pb[6][:, (t8 * 2 + qi) * 4:(t8 * 2 + qi) * 4 + 4], in_=gt[:, t8 * 128:(t8 + 1) * 128],
                            identity=ident_f[0:4, 0:4]), r=gk(gt) + ["ident_f"], w=[("pb", 6)])
                    A("pe", lambda e, t8=t8: e.transpose(
                        out=pb[7][:, t8 * 4:t8 * 4 + 4], in_=g5[:, t8 * 128:(t8 + 1) * 128],
                        identity=ident_f[0:4, 0:4]), r=gk(g5) + ["ident_f"], w=[("pb", 7)])
                A("dve", lambda e: e.tensor_copy(out=wtm[:].rearrange("p a b c -> p (a b c)"), in_=pb[6][:, 0:64]),
                  r=[("pb", 6)], w=["wtm"])
                A("dve", lambda e: e.tensor_copy(out=cumspT[:, half * 8:half * 8 + 8, :].rearrange("p a b -> p (a b)"),
                                                 in_=pb[7][:, 0:32]), r=[("pb", 7)], w=["cumspT"])
                for h in range(4):
                    A("pe", lambda e, h=h: e.matmul(pb[5][:, h * 8:(h + 1) * 8], lhsT=selF[:, h, :], rhs=dsm,
                                                    start=True, stop=True), r=["selF", "dsm"], w=[("pb", 5)])
                A("dve", lambda e: e.tensor_copy(out=dbc[:].rearrange("p a b -> p (a b)"), in_=pb[5][:, 0:32]),
                  r=[("pb", 5)], w=["dbc"])
                if dbg and first_layer_dbg and half == 0:
                    A("sp", lambda e: e.dma_start(out=d_gt[:, :], in_=wtm[:].rearrange("p a b c -> p (a b c)")),
                      r=["wtm"], dma="dbg_gt")

                if STOP_AFTER == "G":
                    continue
                for hp in range(2):
                    P.barrier()
                    qT = carve(0, 128, [2, NHALF], BF16)
                    kT = carve(4096, 128, [2, NHALF], BF16)
                    zT = carve(8192, 128, [2, NHALF], BF16)
                    vext = carve(12288, 128, [8, 2, 130], BF16)
                    osb = carve(16448, 128, [8, 256], BF16)
                    pre = [carve(20544 + i * 2064, 128, [3 + NHALF + 5], BF16) for i in range(2)]
                    dg4 = carve(24672, 128, [16, 128], BF16)
                    bbc = carve(28768, 128, [512], F32)
                    tbase = 30816
                    kp = [carve(tbase + i * 256, 128, [128], BF16) for i in range(2)]
                    spb = [carve(tbase + 512 + i * 256, 128, [128], BF16) for i in range(2)]
                    cbf = [carve(tbase + 1024 + i * 264, 128, [130], BF16) for i in range(2)]
                    hhf = [carve(tbase + 1552 + i * 512, 128, [128], F32) for i in range(2)]
                    hnb = [carve(tbase + 2576 + i * 256, 128, [128], BF16) for i in range(2)]
                    osig = carve(tbase + 3088, 128, [256], F32)
                    stt = [carve(tbase + 4112 + i * 64, 128, [16], F32) for i in range(2)]
                    for ci in range(4):
                        chunk = (0 if ci < 2 else 4) + hp * 2 + (ci % 2)
                        for j in range(4):
                            A("dve", lambda e, ci=ci, j=j, chunk=chunk: e.tensor_scalar(
                                out=dg4[:, ci * 4 + j, :], in0=ident_b[:],
                                scalar1=vecs[:, V_CW + j * 8 + chunk:V_CW + j * 8 + chunk + 1], scalar2=None,
                                op0=ALU.mult), r=["ident_b", "vecs"], w=[("dg4", ci)])
                    A("sp", lambda e, hp=hp: e.dma_start(out=bbc[:, 0:256],
                                                         in_=bin_d[l, :, C_MV + hp * 256:C_MV + hp * 256 + 256].partition_broadcast(128)),
                      w=["bbc"], r=["bbc"], dma="bbc")
                    A("sp", lambda e, hp=hp: e.dma_start(out=bbc[:, 256:512],
                                                         in_=bin_d[l, :, C_MO + hp * 256:C_MO + hp * 256 + 256].partition_broadcast(128)),
                      w=["bbc"], r=["bbc"], dma="bbc")
                    A("dve", lambda e: e.memset(vext, 1.0), w=["vext"], r=["vext"])
                    slot, offs = jobs.get()
                    for ci in range(4):
                        chunk = (0 if ci < 2 else 4) + hp * 2 + (ci % 2)
                        pr = pre[ci % 2]
                        dstT = qT if ci < 2 else kT
                        bcol = (V_BQ if ci < 2 else V_BK) + hp * 2 + (ci % 2)
                        A("act", lambda e, pr=pr, chunk=chunk: e.copy(out=pr[:, 0:3], in_=halo[:, chunk, 0:3]),
                          r=["halo"], w=[("pre", ci % 2, 0)])

                        def ev(bank, tt, pr=pr, bcol=bcol, ci=ci):
                            A("act", lambda e: e.activation(out=pr[:, 3 + tt * 512:3 + (tt + 1) * 512], in_=pb[bank][:],
                                                            func=AF.Identity, bias=vecs[:, bcol:bcol + 1], scale=1.0),
                              r=[("pb", bank), "vecs"], w=[("pre", ci % 2, tt)])
                        proj_fm(slot, ci * 128, 128, ev)
                        A("act", lambda e, pr=pr, chunk=chunk: e.copy(out=halo[:, chunk, 0:3], in_=pr[:, NHALF:NHALF + 3]),
                          r=[("pre", ci % 2, 1)], w=["halo"])
                        for tt in range(2):
                            bank = 2 + (tt % 2)
                            for j in range(4):
                                A("pe", lambda e, j=j, tt=tt, bank=bank, pr=pr, ci=ci: e.matmul(
                                    pb[bank][:], lhsT=dg4[:, ci * 4 + j, :], rhs=pr[:, tt * 512 + j:tt * 512 + j + 512],
                                    start=(j == 0), stop=(j == 3)),
                                  r=[("dg4", ci), ("pre", ci % 2, 0), ("pre", ci % 2, 1)], w=[("pb", bank)])
                            A("act", lambda e, tt=tt, bank=bank, dstT=dstT, ci=ci, chunk=chunk: e.activation(
                                out=dstT[:, ci % 2, tt * 512:(tt + 1) * 512], in_=pb[bank][:], func=AF.Silu,
                                bias=vecs[:, V_CB + chunk:V_CB + chunk + 1], scale=1.0),
                              r=[("pb", bank), "vecs"], w=[("qk", ci, tt)])
                    if dbg and first_layer_dbg and half == 0 and hp == 0:
                        A("sp", lambda e: e.dma_start(out=d_q[:, :, :], in_=qT), r=[("qk", 0, 0), ("qk", 0, 1), ("qk", 1, 0), ("qk", 1, 1)], dma="dbg_q")
                    slot, offs = jobs.get()
                    for i in range(2):
                        def ev(bank, tt, i=i):
                            A("act", lambda e: e.activation(out=zT[:, i, tt * 512:(tt + 1) * 512], in_=pb[bank][:],
                                                            func=AF.Silu, bias=vecs[:, V_BZ + hp * 2 + i:V_BZ + hp * 2 + i + 1],
                                                            scale=1.0), r=[("pb", bank), "vecs"], w=[("zT", i, tt)])
                        proj_fm(slot, i * 128, 128, ev)
                    slot, offs = jobs.get()

                    def ev_vo(bank, t8):
                        A("dve", lambda e: e.tensor_tensor(
                            out=vext[:, t8, :, 0:128], in0=pb[bank][:, 0:256].rearrange("p (a b) -> p a b", b=128),
                            in1=bbc[:, 0:256].rearrange("p (a b) -> p a b", b=128), op=ALU.add),
                          r=[("pb", bank), "bbc", "vext"], w=[("vext", t8)])
                        A("dve", lambda e: e.tensor_tensor(out=osig, in0=pb[bank][:, 256:512], in1=bbc[:, 256:512], op=ALU.add),
                          r=[("pb", bank), "bbc"], w=["osig"])
                        A("act", lambda e: e.activation(out=osb[:, t8, :], in_=osig, func=AF.Sigmoid),
                          r=["osig"], w=[("osb", t8)])
                    proj_tm(slot, 0, 512, ev_vo)
                    P.barrier()
                    def stA(it, hh, c8):
                        h = hp * 2 + hh
                        sset = it % 2
                        bT, bSt, bN, bC = sset, 2 + sset, 4 + sset, 6
                        tok = slice(c8 * 128, (c8 + 1) * 128)
                        wcol = wtm[:, c8, 0, h:h + 1]
                        dcol = dbc[:, h, c8:c8 + 1]
                        qk_r = [("qk", hh, c8 // 4), ("qk", 2 + hh, c8 // 4)]
                        tpv = pbb(bT)[:, 0:128]
                        stv = pb[bSt][:, 0:128]
                        A("pe", lambda e: e.transpose(out=tpv, in_=kT[:, hh, tok], identity=ident_b[:]),
                          r=qk_r + ["ident_b"], w=[("pb", bT)])
                        A("act", lambda e: e.activation(out=kp[sset], in_=tpv, func=AF.Identity, scale=wcol),
                          r=[("pb", bT), "wtm"], w=[("kp", sset)])
                        A("pe", lambda e: e.matmul(stv, lhsT=kT[:, hh, tok], rhs=qT[:, hh, tok], start=True, stop=True),
                          r=qk_r, w=[("pb", bSt)])
                        A("dve", lambda e: e.scalar_tensor_tensor(
                            out=spb[sset], in0=stv, scalar=wcol, in1=maskT[:], op0=ALU.mult, op1=ALU.mult),
                          r=[("pb", bSt), "wtm", "maskT"], w=[("spb", sset)])
                        A("dve", lambda e: e.tensor_scalar(
                            out=cbf[sset], in0=Cst[:, h, :], scalar1=dcol, scalar2=None, op0=ALU.mult),
                          r=["Cst", "dbc"], w=[("cbf", sset)])
                        A("pe", lambda e: e.matmul(
                            pb[bN][:, 0:130], lhsT=spb[sset], rhs=vext[:, c8, hh, :], start=True, stop=False),
                          r=[("spb", sset), ("vext", c8), "vext"], w=[("pb", bN)])
                        A("pe", lambda e: e.matmul(
                            pb[bN][:, 0:130], lhsT=qT[:, hh, tok], rhs=cbf[sset], start=False, stop=True),
                          r=qk_r + [("cbf", sset)], w=[("pb", bN)])
                        A("pe", lambda e: e.matmul(
                            pb[bC][:, 0:130], lhsT=kp[sset], rhs=vext[:, c8, hh, :], start=True, stop=True),
                          r=[("kp", sset), ("vext", c8), "vext"], w=[("pb", bC)])
                        A("dve", lambda e: e.scalar_tensor_tensor(
                            out=Cst[:, h, :], in0=Cst[:, h, :], scalar=dcol, in1=pb[bC][:, 0:130],
                            op0=ALU.mult, op1=ALU.add), r=["Cst", "dbc", ("pb", bC)], w=["Cst"])

                    def stB(it, hh, c8):
                        h = hp * 2 + hh
                        sset = it % 2
                        bN = 4 + sset
                        fcol = wtm[:, c8, 1, h:h + 1]
                        sv = stt[sset]
                        A("dve", lambda e: e.tensor_scalar(
                            out=sv[:, 12:13], in0=pb[bN][:, 128:129], scalar1=-1.0, scalar2=fcol,
                            op0=ALU.mult, op1=ALU.max), r=[("pb", bN), "wtm"], w=[("stt", sset)])
                        A("dve", lambda e: e.tensor_tensor(
                            out=sv[:, 0:1], in0=sv[:, 12:13], in1=pb[bN][:, 128:129], op=ALU.max),
                          r=[("pb", bN), ("stt", sset)], w=[("stt", sset)])
                        A("dve", lambda e: e.reciprocal(out=sv[:, 1:2], in_=sv[:, 0:1]),
                          r=[("stt", sset)], w=[("stt", sset)])
                        A("dve", lambda e: e.scalar_tensor_tensor(
                            out=hhf[sset], in0=pb[bN][:, 0:128], scalar=sv[:, 1:2], in1=osb[:, c8, hh * 128:(hh + 1) * 128],
                            op0=ALU.mult, op1=ALU.mult), r=[("pb", bN), ("stt", sset), ("osb", c8)], w=[("hhf", sset)])
                        A("dve", lambda e: e.bn_stats(out=sv[:, 2:8], in_=hhf[sset]),
                          r=[("hhf", sset)], w=[("stt", sset)])
                        A("dve", lambda e: e.bn_aggr(out=sv[:, 8:10], in_=sv[:, 2:8]),
                          r=[("stt", sset)], w=[("stt", sset)])
                        A("act", lambda e: e.activation(out=sv[:, 10:11], in_=sv[:, 9:10], func=AF.Ln,
                                                        bias=cst[:, CE5:CE5 + 1], scale=1.0),
                          r=[("stt", sset), "cst"], w=[("stt", sset)])
                        A("act", lambda e: e.activation(out=sv[:, 11:12], in_=sv[:, 10:11], func=AF.Exp, scale=-0.5),
                          r=[("stt", sset)], w=[("stt", sset)])

                    def stC(it, hh, c8):
                        sset = it % 2
                        sv = stt[sset]
                        tp2v = pbb(7)[:, 0:128]
                        A("dve", lambda e: e.tensor_scalar(
                            out=hnb[sset], in0=hhf[sset], scalar1=sv[:, 8:9], scalar2=sv[:, 11:12],
                            op0=ALU.subtract, op1=ALU.mult), r=[("hhf", sset), ("stt", sset)], w=[("hnb", sset)])
                        A("pe", lambda e: e.transpose(out=tp2v, in_=hnb[sset], identity=ident_b[:]),
                          r=[("hnb", sset), "ident_b"], w=[("pb", 7)])

                    def stD(it, hh, c8):
                        h = hp * 2 + hh
                        sset = it % 2
                        tok = slice(c8 * 128, (c8 + 1) * 128)
                        tp2v = pbb(7)[:, 0:128]
                        A("dve", lambda e: e.scalar_tensor_tensor(
                            out=yTa[:, h, tok], in0=tp2v, scalar=vecs[:, V_HG + h:V_HG + h + 1], in1=zT[:, hh, tok],
                            op0=ALU.mult, op1=ALU.mult),
                          r=[("pb", 7), "vecs", ("zT", hh, c8 // 4)], w=[("yTa", h)])

                    its = [(i, i // 8, i % 8) for i in range(16)]
                    for step in range(16 + 3):
                        if step < 16:
                            stA(*its[step])
                        if 0 <= step - 1 < 16:
                            stB(*its[step - 1])
                        if 0 <= step - 3 < 16:
                            stD(*its[step - 3])
                        if 0 <= step - 2 < 16:
                            stC(*its[step - 2])
                if dbg and first_layer_dbg and half == 0:
                    A("sp", lambda e: e.dma_start(out=d_ya[:, :, :], in_=yTa[:]), r=[("yTa", i) for i in range(4)], dma="dbg_ya")

                if STOP_AFTER == "A":
                    continue
                P.barrier()
                fqT = carve(0, 128, [2, NHALF], BF16)
                fzT = carve(8192, 64, [4, NHALF], BF16)
                ptb = [carve(24576 + i * 1024, 128, [512], BF16) for i in range(4)]
                SBK = [0, 1, 5, 6]
                rden = carve(18432, 128, [512], F32)
                t1 = carve(20480, 64, [512], F32)
                ytmp = carve(22528, 64, [512], BF16)
                bbf = carve(23552, 128, [256], F32)
                A("sp", lambda e: e.dma_start(out=bbf, in_=bin_d[l, :, C_FV:C_FV + 256].partition_broadcast(128)),
                  w=["bbf"], dma="bbf")
                slot, offs = jobs.get()
                for i in range(2):
                    def evq(bank, tt, i=i):
                        A("dve", lambda e: e.tensor_scalar(out=fqT[:, i, tt * 512:(tt + 1) * 512], in0=pb[bank][:],
                                                           scalar1=vecs[:, V_BFQ + i:V_BFQ + i + 1], scalar2=0.125,
                                                           op0=ALU.add, op1=ALU.mult),
                          r=[("pb", bank), "vecs"], w=[("fqT", tt)])
                    proj_fm(slot, i * 128, 128, evq)

                    def evk(bank, tt, i=i):
                        A("act", lambda e: e.activation(out=fkT[:, i, t0 + tt * 512:t0 + (tt + 1) * 512], in_=pb[bank][:],
                                                        func=AF.Identity, bias=vecs[:, V_BFK + i:V_BFK + i + 1], scale=1.0),
                          r=[("pb", bank), "vecs"], w=["fkT"])
                    proj_fm(slot, 256 + i * 128, 128, evk)
                slot, offs = jobs.get()

                def ev_fv(bank, t8):
                    A("dve", lambda e: e.tensor_tensor(
                        out=Vext[:, half * 8 + t8, :, 0:64], in0=pb[bank][:, 0:256].rearrange("p (a b) -> p a b", b=64),
                        in1=bbf.rearrange("p (a b) -> p a b", b=64), op=ALU.add),
                      r=[("pb", bank), "bbf"], w=["Vext"])
                proj_tm(slot, 0, 256, ev_fv)
                for h in range(4):
                    def evz(bank, tt, h=h):
                        A("act", lambda e: e.activation(out=fzT[:, h, tt * 512:(tt + 1) * 512], in_=pb[bank][0:64, :],
                                                        func=AF.Silu, bias=vec64[:, h:h + 1], scale=1.0),
                          r=[("pb", bank), "vec64"], w=[("fzT", tt)])
                    proj_fm(slot, 256 + h * 64, 64, evz)
                P.barrier()
                fblocks = []
                nO = 0
                for h in range(4):
                    for Q in range(2):
                        q0t = (t0 + Q * 512) // 128
                        nkb = q0t + 4
                        bO = 2 + (nO % 2)
                        nO += 1
                        for kb in range(nkb):
                            fblocks.append((len(fblocks), h, Q, kb, nkb, q0t, bO))

                def foxS(nS, h, Q, kb, nkb, q0t, bO):
                    kc, pbase = h // 2, (h % 2) * 64
                    j = kb - q0t
                    nc0 = max(0, j) * 128
                    N = 512 - nc0
                    bS = SBK[nS % 4]
                    pt = ptb[nS % 4]
                    regs = [(0, N, False)] if j < 0 else ([(0, 128, True)] + ([(128, N, False)] if N > 128 else []))
                    for (ra, rb, dg) in regs:
                        qsr = slice(Q * 512 + nc0 + ra, Q * 512 + nc0 + rb)
                        A("pe", lambda e: e.matmul(
                            pb[bS][:, ra:rb], lhsT=fkT[pbase:pbase + 64, kc, kb * 128:(kb + 1) * 128],
                            rhs=fqT[pbase:pbase + 64, kc, qsr], start=True, stop=False),
                          r=["fkT", ("fqT", Q)], w=[("pb", bS)])
                        A("pe", lambda e: e.matmul(
                            pb[bS][:, ra:rb], lhsT=selB[:, h, :], rhs=cumq_bf[:, qsr], start=False, stop=(not dg)),
                          r=["selB", "cumq_bf"], w=[("pb", bS)])
                        if dg:
                            A("pe", lambda e: e.matmul(
                                pb[bS][:, ra:rb], lhsT=ident_b[:], rhs=negmask[:], start=False, stop=True),
                              r=["ident_b", "negmask"], w=[("pb", bS)])
                    A("act", lambda e: e.activation(
                        out=pt[:, 0:N], in_=pb[bS][:, 0:N], func=AF.Exp, bias=cumspT[:, kb, h:h + 1], scale=1.0),
                      r=[("pb", bS), "cumspT"], w=[("pt", bS)])

                def foxPV(nS, h, Q, kb, nkb, q0t, bO):
                    kc, pbase = h // 2, (h % 2) * 64
                    j = kb - q0t
                    nc0 = max(0, j) * 128
                    N = 512 - nc0
                    bS = SBK[nS % 4]
                    pt = ptb[nS % 4]
                    A("pe", lambda e: e.matmul(
                        pb[bO][0:65, nc0:512], lhsT=Vext[:, kb, h, :], rhs=pt[:, 0:N],
                        start=(kb == 0), stop=(kb == nkb - 1)),
                      r=["Vext", ("pt", bS)], w=[("pb", bO)])
                    if kb != nkb - 1:
                        return
                    A("act", lambda e: e.copy(out=rden[0:1, :], in_=pb[bO][64:65, :]),
                      r=[("pb", bO)], w=["rden"])
                    A("dve", lambda e: e.reciprocal(out=rden[0:1, :], in_=rden[0:1, :]), r=["rden"], w=["rden"])
                    A("pe", lambda e: e.matmul(pb[4][0:64, :], lhsT=onesF[0:1, 0:64], rhs=rden[0:1, :],
                                               start=True, stop=True), r=["rden", "onesF"], w=[("pb", 4)])
                    A("dve", lambda e: e.tensor_tensor(
                        out=t1, in0=pb[bO][0:64, :], in1=fzT[:, h, Q * 512:(Q + 1) * 512], op=ALU.mult),
                      r=[("pb", bO), ("fzT", Q)], w=["t1"])
                    if pbase == 0:
                        A("dve", lambda e: e.tensor_tensor(
                            out=yTb[0:64, kc, Q * 512:(Q + 1) * 512], in0=t1, in1=pb[4][0:64, :], op=ALU.mult),
                          r=["t1", ("pb", 4)], w=[("yTb", kc)])
                    else:
                        A("dve", lambda e: e.tensor_tensor(out=ytmp, in0=t1, in1=pb[4][0:64, :], op=ALU.mult),
                          r=["t1", ("pb", 4)], w=["ytmp"])
                        A("act", lambda e: e.copy(out=yTb[64:128, kc, Q * 512:(Q + 1) * 512], in_=ytmp),
                          r=["ytmp"], w=[("yTb", kc)])

                for bi in range(min(3, len(fblocks))):
                    foxS(*fblocks[bi])
                for bi in range(len(fblocks)):
                    if bi + 3 < len(fblocks):
                        foxS(*fblocks[bi + 3])
                    foxPV(*fblocks[bi])
                if dbg and first_layer_dbg and half == 0:
                    A("sp", lambda e: e.dma_start(out=d_yb[:, :, :], in_=yTb[:]), r=[("yTb", 0), ("yTb", 1)], dma="dbg_yb")

                if STOP_AFTER == "B":
                    continue
                P.barrier()
                sg = [carve(i * 2048, 128, [512], F32) for i in range(2)]
                zcT = carve(4096, 128, [2, NHALF], BF16)
                dg31 = carve(8192, 128, [31, 128], BF16)
                yf = carve(16384, 128, [512], F32)
                ybf = carve(18432, 128, [512], BF16)
                ysq = carve(19456, 128, [512], BF16)
                msq = carve(20480, 128, [512], F32)
                var = carve(22528, 128, [512], F32)
                tt_ = carve(24576, 128, [512], F32)
                ss_ = carve(26624, 128, [512], F32)
                if half == 0:
                    A("dve", lambda e: e.memset(ubuf[:, :, 0:30], 0.0), w=["ubuf"], r=["ubuf"])
                else:
                    A("act", lambda e: e.copy(out=ubuf[:, :, 0:30], in_=ubuf[:, :, NHALF:NHALF + 30]), w=["ubuf"], r=["ubuf"])
                slot, offs = jobs.get()
                for c in range(2):
                    for tt in range(2):
                        bank = next_acc()
                        for k in range(8):
                            A("pe", lambda e, k=k, tt=tt, bank=bank, c=c: e.matmul(
                                pb[bank][:], lhsT=wbuf[slot][:, k, 256 + c * 128:256 + (c + 1) * 128],
                                rhs=hT[:, k, tt * 512:(tt + 1) * 512], start=(k == 0), stop=(k == 7)),
                              r=[("wb", slot), ("hT", tt)], w=[("pb", bank)])
                        A("act", lambda e, bank=bank, c=c, tt=tt: e.activation(
                            out=sg[tt % 2], in_=pb[bank][:], func=AF.Sigmoid, bias=vecs[:, V_BCG + c:V_BCG + c + 1], scale=1.0),
                          r=[("pb", bank), "vecs"], w=[("sg", tt % 2)])
                        bank = next_acc()
                        for k in range(8):
                            A("pe", lambda e, k=k, tt=tt, bank=bank, c=c: e.matmul(
                                pb[bank][:], lhsT=wbuf[slot][:, k, c * 128:(c + 1) * 128],
                                rhs=hT[:, k, tt * 512:(tt + 1) * 512], start=(k == 0), stop=(k == 7)),
                              r=[("wb", slot), ("hT", tt)], w=[("pb", bank)])
                        A("dve", lambda e, bank=bank, c=c, tt=tt: e.scalar_tensor_tensor(
                            out=ubuf[:, c, 30 + tt * 512:30 + (tt + 1) * 512], in0=pb[bank][:],
                            scalar=vecs[:, V_BCA + c:V_BCA + c + 1], in1=sg[tt % 2], op0=ALU.add, op1=ALU.mult),
                          r=[("pb", bank), "vecs", ("sg", tt % 2)], w=["ubuf"])
                slot, offs = jobs.get()
                for c in range(2):
                    def evcz(bank, tt, c=c):
                        A("act", lambda e: e.activation(out=zcT[:, c, tt * 512:(tt + 1) * 512], in_=pb[bank][:], func=AF.Silu,
                                                        bias=vecs[:, V_BCZ + c:V_BCZ + c + 1], scale=1.0),
                          r=[("pb", bank), "vecs"], w=[("zcT", c)])
                    proj_fm(slot, c * 128, 128, evcz)
                for c in range(2):
                    for j in range(31):
                        A("dve", lambda e, c=c, j=j: e.tensor_scalar(
                            out=dg31[:, j, :], in0=ident_b[:], scalar1=vecs[:, V_DW + j * 2 + c:V_DW + j * 2 + c + 1],
                            scalar2=None, op0=ALU.mult), r=["ident_b", "vecs"], w=["dg31"])
                    for tt in range(2):
                        bank = 2 + tt
                        for j in range(31):
                            A("pe", lambda e, c=c, j=j, tt=tt, bank=bank: e.matmul(
                                pb[bank][:], lhsT=dg31[:, j, :], rhs=ubuf[:, c, tt * 512 + j:tt * 512 + j + 512],
                                start=(j == 0), stop=(j == 30)), r=["dg31", "ubuf"], w=[("pb", bank)])
                        bcol = vecs[:, V_DWB + c:V_DWB + c + 1]
                        A("act", lambda e, bank=bank, bcol=bcol: e.activation(out=yf, in_=pb[bank][:], func=AF.Identity,
                                                                              bias=bcol, scale=1.0),
                          r=[("pb", bank), "vecs"], w=["yf"])
                        A("act", lambda e, bank=bank, bcol=bcol: e.activation(out=ysq, in_=pb[bank][:], func=AF.Square,
                                                                              bias=bcol, scale=1.0),
                          r=[("pb", bank), "vecs"], w=["ysq"])
                        A("dve", lambda e: e.tensor_copy(out=ybf, in_=yf), r=["yf"], w=["ybf"])
                        A("pe", lambda e: e.matmul(pb[4][:], lhsT=blk64[:], rhs=ybf, start=True, stop=True),
                          r=["blk64", "ybf"], w=[("pb", 4)])
                        A("pe", lambda e: e.matmul(pb[5][:], lhsT=blk64[:], rhs=ysq, start=True, stop=True),
                          r=["blk64", "ysq"], w=[("pb", 5)])
                        A("act", lambda e: e.activation(out=msq, in_=pb[4][:], func=AF.Square), r=[("pb", 4)], w=["msq"])
                        A("dve", lambda e: e.tensor_tensor(out=var, in0=pb[5][:], in1=msq, op=ALU.subtract),
                          r=[("pb", 5), "msq"], w=["var"])
                        A("act", lambda e: e.activation(out=var, in_=var, func=AF.Ln, bias=cst[:, CE5:CE5 + 1], scale=1.0),
                          r=["var", "cst"], w=["var"])
                        A("act", lambda e: e.activation(out=var, in_=var, func=AF.Exp, scale=-0.5), r=["var"], w=["var"])
                        A("dve", lambda e: e.tensor_tensor(out=tt_, in0=yf, in1=pb[4][:], op=ALU.subtract),
                          r=["yf", ("pb", 4)], w=["tt_"])
                        A("dve", lambda e: e.tensor_tensor(out=tt_, in0=tt_, in1=var, op=ALU.mult), r=["tt_", "var"], w=["tt_"])
                        A("act", lambda e, c=c: e.activation(out=ss_, in_=tt_, func=AF.Silu,
                                                             scale=vecs[:, V_LNG + c:V_LNG + c + 1],
                                                             bias=vecs[:, V_LNB + c:V_LNB + c + 1]),
                          r=["tt_", "vecs"], w=["ss_"])
                        A("dve", lambda e, c=c, tt=tt: e.tensor_tensor(out=yTc[:, c, tt * 512:(tt + 1) * 512], in0=ss_,
                                                                         in1=zcT[:, c, tt * 512:(tt + 1) * 512], op=ALU.mult),
                          r=["ss_", ("zcT", c)], w=[("yTc", c)])
                if dbg and first_layer_dbg and half == 0:
                    A("sp", lambda e: e.dma_start(out=d_yc[:, :, :], in_=yTc[:]), r=[("yTc", 0), ("yTc", 1)], dma="dbg_yc")

                if STOP_AFTER == "C":
                    continue
                P.barrier()
                ycat = [yTa[:, 0, :], yTa[:, 1, :], yTa[:, 2, :], yTa[:, 3, :], yTb[:, 0, :], yTb[:, 1, :],
                        yTc[:, 0, :], yTc[:, 1, :]]
                for jb in range(2):
                    slot, offs = jobs.get()
                    for d4 in range(4):
                        dc = jb * 4 + d4
                        for tt in range(2):
                            bank = next_acc()
                            for k in range(8):
                                A("pe", lambda e, k=k, tt=tt, bank=bank, d4=d4, slot=slot: e.matmul(
                                    pb[bank][:], lhsT=wbuf[slot][:, k, d4 * 128:(d4 + 1) * 128],
                                    rhs=ycat[k][:, tt * 512:(tt + 1) * 512], start=(k == 0), stop=(k == 7)),
                                  r=[("wb", slot), "ycat"], w=[("pb", bank)])
                            tok = slice(t0 + tt * 512, t0 + (tt + 1) * 512)
                            A("dve", lambda e, bank=bank, dc=dc, tok=tok: e.scalar_tensor_tensor(
                                out=xT[:, dc, tok], in0=pb[bank][:], scalar=modv[:, l, 16 + dc, s:s + 1], in1=xT[:, dc, tok],
                                op0=ALU.mult, op1=ALU.add),
                              r=[("pb", bank), "modv", ("xT", (t0 + tt * 512) // 512)], w=[("xT", (t0 + tt * 512) // 512)])

        for s in range(NS):
            load_x(s)
            for l in range(L):
                if s == 0:
                    load_vecs(l)
                    ada(l)
                layer(l, s, first_layer_dbg=(s == 0 and l == 0))
            store_x(s)
        P.finalize_and_emit()
    return nc


def _chunks(v):
    return np.ascontiguousarray(v.reshape(-1, 128).T)


def _host_vecs(inp, l):
    b_in = inp["b_in"][l]
    cols = [
        _chunks(inp["norm_g"][l]), _chunks(inp["b_ada"][l]),
        _chunks(b_in[C_MQ:C_MQ + 512]), _chunks(b_in[C_MK:C_MK + 512]), _chunks(b_in[C_MZ:C_MZ + 512]),
        _chunks(b_in[C_FQ:C_FQ + 256]), _chunks(b_in[C_FK:C_FK + 256]),
        _chunks(b_in[C_CA:C_CA + 256]), _chunks(b_in[C_CG:C_CG + 256]), _chunks(b_in[C_CZ:C_CZ + 256]),
        _chunks(inp["m_conv_b"][l]), _chunks(inp["m_hn_g"][l]),
        _chunks(inp["c_dw_b"][l]), _chunks(inp["c_ln_g"][l]), _chunks(inp["c_ln_b"][l]),
    ]
    cw = inp["m_conv_w"][l]
    cols.append(np.concatenate([_chunks(cw[j]) for j in range(4)], axis=1))
    dw = inp["c_dw_w"][l]
    cols.append(np.concatenate([_chunks(dw[j]) for j in range(31)], axis=1))
    v = np.concatenate(cols, axis=1).astype(np.float32)
    assert v.shape == (128, NV), v.shape
    vec64 = np.ascontiguousarray(np.concatenate([b_in[C_FZ:C_FZ + 256].reshape(4, 64).T, b_in[C_FQ:C_FQ + 256].reshape(4, 64).T,
                                                 b_in[C_FK:C_FK + 256].reshape(4, 64).T], axis=1)).astype(np.float32)
    gbv = np.stack([b_in[C_MI:C_MI + 4], b_in[C_MF:C_MF + 4], b_in[C_FF:C_FF + 4]], axis=1).astype(np.float32)
    return v, vec64, gbv


_NC_CACHE = {}


def _get_nc(L, NS, final_norm, dbg=False):
    key = (L, NS, final_norm, dbg)
    if key not in _NC_CACHE:
        _NC_CACHE[key] = build(L, NS, final_norm, dbg)
    return _NC_CACHE[key]


FUSED = True


def kernel(x, c, norm_g, w_ada, b_ada, w_in, b_in, m_conv_w, m_conv_b, m_hn_g,
           c_dw_w, c_dw_b, c_ln_g, c_ln_b, w_out, final_g):
    inp = dict(norm_g=np.asarray(norm_g), b_ada=np.asarray(b_ada), b_in=np.asarray(b_in),
               m_conv_w=np.asarray(m_conv_w), m_conv_b=np.asarray(m_conv_b), m_hn_g=np.asarray(m_hn_g),
               c_dw_w=np.asarray(c_dw_w), c_dw_b=np.asarray(c_dw_b), c_ln_g=np.asarray(c_ln_g),
               c_ln_b=np.asarray(c_ln_b))
    x = np.asarray(x, dtype=np.float32)
    c = np.asarray(c, dtype=np.float32)
    w_ada = np.asarray(w_ada, dtype=np.float32)
    w_in = np.asarray(w_in, dtype=np.float32)
    w_out = np.asarray(w_out, dtype=np.float32)
    b_in_a = np.asarray(b_in, dtype=np.float32)
    fg = np.asarray(final_g, dtype=np.float32).reshape(1, D)
    DEPTH = w_in.shape[0]
    hv = [_host_vecs(inp, l) for l in range(DEPTH)]
    n = 8
    if FUSED:
        nc = _get_nc(DEPTH, 2, True)
        in_maps = []
        for i in range(n):
            cs = c[2 * i:2 * i + 2]
            cT = np.ascontiguousarray(cs.reshape(2, 8, 128).transpose(2, 1, 0))
            in_maps.append({
                "x": np.ascontiguousarray(x[2 * i:2 * i + 2]), "cT": cT,
                "vecs": np.stack([h[0] for h in hv]), "vec64": np.stack([h[1] for h in hv]),
                "gb": np.stack([h[2] for h in hv]),
                "w_ada": w_ada, "w_in": w_in, "b_in": np.ascontiguousarray(b_in_a.reshape(DEPTH, 1, DIN)),
                "w_out": w_out, "final_g": fg})
        res = run_bass_kernel_spmd(nc, in_maps, core_ids=list(range(n)))
        return np.concatenate([r["out"] for r in res.results], axis=0)
    cur = x.copy()
    for l in range(DEPTH):
        nc = _get_nc(1, 1, l == DEPTH - 1)
        for sidx in range(2):
            in_maps = []
            for i in range(n):
                b = 2 * i + sidx
                cT = np.ascontiguousarray(c[b:b + 1].reshape(1, 8, 128).transpose(2, 1, 0))
                in_maps.append({
                    "x": np.ascontiguousarray(cur[b:b + 1]), "cT": cT,
                    "vecs": hv[l][0][None], "vec64": hv[l][1][None], "gb": hv[l][2][None],
                    "w_ada": w_ada[l:l + 1], "w_in": w_in[l:l + 1],
                    "b_in": np.ascontiguousarray(b_in_a[l].reshape(1, 1, DIN)),
                    "w_out": w_out[l:l + 1], "final_g": fg})
            res = run_bass_kernel_spmd(nc, in_maps, core_ids=list(range(n)))
            for i in range(n):
                cur[2 * i + sidx] = res.results[i]["out"][0]
    return cur
```
